# Optimizing a Trainium2 kernel written in Bass

```python
import jax
import jax.numpy as jnp
from jax import lax
import numpy as np

D_MODEL = 2048
BATCH = 4
SEQ = 8192
DEPTH = 1
DEC_BATCH = 8
DEC_SEQ = 16
PAST_LEN = 4096

CHUNK = 64
D_MIX = D_MODEL
C_CONV = D_MIX // 2
C_RWKV = D_MIX - C_CONV
HEAD_RWKV = 64
H_RWKV = C_RWKV // HEAD_RWKV
CONV_WIDTH = 31
R_DECAY = 64
R_ICLR = 64
R_GATE = 160
N_SHIFT = 3 * C_RWKV + R_DECAY + R_ICLR + R_GATE
P_IN = 2 * C_CONV + N_SHIFT
N_EXPERTS = 32
TOP_K = 4
D_FF = D_MODEL
SWIGLU_LIMIT = 7.0
SWIGLU_ALPHA = 1.702
MOE_BLOCK = 128
LN_EPS = 1e-5
GN_EPS = 64e-5
ALPHA = (2.0 * DEPTH) ** 0.25
BETA = (8.0 * DEPTH) ** -0.25

kernel_name = 'hybrid_conv_rwkv7_moe_stream_step'


def _layer_norm(x, g, b, eps=LN_EPS):
    xf = x.astype(jnp.float32)
    mu = jnp.mean(xf, axis=-1, keepdims=True)
    var = jnp.mean(jnp.square(xf - mu), axis=-1, keepdims=True)
    return ((xf - mu) * lax.rsqrt(var + eps) * g + b).astype(x.dtype)


def _wkv7_scan(state, r, decay, k, v, kk, a):
    def step(S, inp):
        r_t, w_t, k_t, v_t, kk_t, a_t = inp
        sa = jnp.einsum('bhvk,bhk->bhv', S, -kk_t)
        S = (S * w_t[:, :, None, :] + sa[..., None] * (kk_t * a_t)[:, :, None, :]
             + v_t[..., None] * k_t[:, :, None, :])
        return S, jnp.einsum('bhvk,bhk->bhv', S, r_t)
    xs = tuple(jnp.moveaxis(t, 1, 0) for t in (r, decay, k, v, kk, a))
    state, y = lax.scan(step, state, xs)
    return jnp.moveaxis(y, 0, 1), state


def _rwkv7_group(zs, state, lp):
    B, T, _ = zs.shape
    zf = zs.astype(jnp.float32)
    r, k, v, xw, xa, xg = jnp.split(
        zf, [C_RWKV, 2 * C_RWKV, 3 * C_RWKV, 3 * C_RWKV + R_DECAY, 3 * C_RWKV + R_DECAY + R_ICLR], axis=-1)
    w_log = -jax.nn.softplus(-(lp['rwkv_w0'] + jnp.tanh(xw) @ lp['rwkv_w2'])) - 0.5
    decay = jnp.exp(-jnp.exp(w_log))
    a = jax.nn.sigmoid(lp['rwkv_a0'] + xa @ lp['rwkv_a2'])
    g = jax.nn.sigmoid(xg) @ lp['rwkv_g2']
    heads = lambda t: t.reshape(B, T, H_RWKV, HEAD_RWKV)
    kk = heads(k * lp['rwkv_k_k'])
    kk = kk / jnp.maximum(jnp.sqrt(jnp.sum(kk * kk, axis=-1, keepdims=True)), 1e-12)
    k = k * (1.0 + (a - 1.0) * lp['rwkv_k_a'])
    r_h, k_h, v_h = heads(r), heads(k), heads(v)
    y, state_new = _wkv7_scan(state.astype(jnp.float32), r_h, heads(decay), k_h, v_h, kk, heads(a))
    mu = jnp.mean(y, axis=-1, keepdims=True)
    var = jnp.mean(jnp.square(y - mu), axis=-1, keepdims=True)
    y = ((y - mu) * lax.rsqrt(var + GN_EPS)).reshape(B, T, C_RWKV) * lp['rwkv_ln_g'] + lp['rwkv_ln_b']
    bonus = jnp.sum(r_h * k_h * lp['rwkv_r_k'], axis=-1, keepdims=True) * v_h
    y = (y + bonus.reshape(B, T, C_RWKV)) * g
    return y.astype(zs.dtype), state_new.astype(state.dtype)


def _mixer(x, conv_buf, shift_buf, wkv_state, lp):
    proj = jnp.einsum('btd,dp->btp', x, lp['w_in']) + lp['b_in']
    u_val, u_gate, z = jnp.split(proj, [C_CONV, 2 * C_CONV], axis=-1)
    u = u_val * jax.nn.sigmoid(u_gate)
    u_ext = jnp.concatenate([conv_buf.astype(u.dtype), u], axis=1)
    c = lax.conv_general_dilated(
        u_ext, lp['conv_w'][:, None, :].astype(u_ext.dtype), (1,), 'VALID',
        dimension_numbers=('NWC', 'WIO', 'NWC'), feature_group_count=C_CONV) + lp['conv_b']
    c = jax.nn.silu(_layer_norm(c, lp['conv_ln_g'], lp['conv_ln_b']))
    new_conv = u_ext[:, -(CONV_WIDTH - 1):]
    z_prev = jnp.concatenate([shift_buf.astype(z.dtype), z[:, :-1]], axis=1)
    zs = z + lp['mu_shift'] * (z_prev - z)
    new_shift = z[:, -1:]
    y_b, new_wkv = _rwkv7_group(zs, wkv_state, lp)
    mix = jnp.einsum('btc,cd->btd', jnp.concatenate([c, y_b], axis=-1), lp['w_out'])
    return mix, new_conv, new_shift, new_wkv


def _moe(x, lp):
    B, T, D = x.shape
    xt = x.reshape(B * T, D)
    n_tok = B * T
    logits = (xt @ lp['router_w'] + lp['router_b']).astype(jnp.float32)
    top_logit, top_idx = lax.top_k(logits, TOP_K)
    gates = jax.nn.softmax(top_logit, axis=-1)
    n_assign = n_tok * TOP_K
    flat_e = top_idx.reshape(-1)
    order = jnp.argsort(flat_e)
    sorted_e = flat_e[order]
    counts = jnp.bincount(flat_e, length=N_EXPERTS)
    padded = (counts + MOE_BLOCK - 1) // MOE_BLOCK * MOE_BLOCK
    seg_end = jnp.cumsum(padded)
    seg_start = seg_end - padded
    start = jnp.cumsum(counts) - counts
    dest = seg_start[sorted_e] + jnp.arange(n_assign) - start[sorted_e]
    n_rows = (n_assign + N_EXPERTS * (MOE_BLOCK - 1) + MOE_BLOCK - 1) // MOE_BLOCK * MOE_BLOCK
    n_blocks = n_rows // MOE_BLOCK
    row_tok = jnp.full((n_rows,), n_tok, jnp.int32).at[dest].set((order // TOP_K).astype(jnp.int32))
    row_gate = jnp.zeros((n_rows,), jnp.float32).at[dest].set(gates.reshape(-1)[order])
    block_exp = jnp.minimum(
        jnp.searchsorted(seg_end, jnp.arange(n_blocks) * MOE_BLOCK, side='right'), N_EXPERTS - 1)
    x_pad = jnp.concatenate([xt, jnp.zeros((1, D), xt.dtype)], axis=0)
    xb = x_pad[row_tok].reshape(n_blocks, MOE_BLOCK, D)
    w_gate, b_gate, w_up, b_up = lp['w_gate'], lp['b_gate'], lp['w_up'], lp['b_up']
    w_down, b_down = lp['w_down'], lp['b_down']

    def expert_block(args):
        xblk, e = args
        gate = jnp.minimum(xblk @ w_gate[e] + b_gate[e], SWIGLU_LIMIT)
        up = jnp.clip(xblk @ w_up[e] + b_up[e], -SWIGLU_LIMIT, SWIGLU_LIMIT)
        h = (up + 1.0) * gate * jax.nn.sigmoid(SWIGLU_ALPHA * gate)
        return h @ w_down[e] + b_down[e]

    yb = lax.map(expert_block, (xb, block_exp)).reshape(n_rows, D)
    y = jax.ops.segment_sum(yb * row_gate[:, None], row_tok, num_segments=n_tok + 1)[:n_tok]
    return y.reshape(B, T, D).astype(x.dtype)


def _layer(x, conv_buf, shift_buf, wkv_state, lp):
    mix, new_conv, new_shift, new_wkv = _mixer(x, conv_buf, shift_buf, wkv_state, lp)
    x = _layer_norm(ALPHA * x + mix, lp['ln1_g'], lp['ln1_b'])
    x = _layer_norm(ALPHA * x + _moe(x, lp), lp['ln2_g'], lp['ln2_b'])
    return x, new_conv, new_shift, new_wkv


def setup_inputs(seed: int = 0) -> dict:
    key = jax.random.key(seed)
    ks = jax.random.split(key, 40)
    nrm = lambda k, shape, s: s * jax.random.normal(k, shape, jnp.float32)
    L = DEPTH
    return {
        'x_prompt': nrm(ks[0], (BATCH, SEQ, D_MODEL), 1.0),
        'x_sample': nrm(ks[1], (DEC_BATCH, DEC_SEQ, D_MODEL), 1.0),
        'state_conv': nrm(ks[2], (L, DEC_BATCH, CONV_WIDTH - 1, C_CONV), 0.5),
        'state_shift': nrm(ks[3], (L, DEC_BATCH, 1, N_SHIFT), 1.0),
        'state_wkv': nrm(ks[4], (L, DEC_BATCH, H_RWKV, HEAD_RWKV, HEAD_RWKV), 0.3),
        'w_in': nrm(ks[5], (L, D_MODEL, P_IN), D_MODEL ** -0.5),
        'b_in': nrm(ks[6], (L, P_IN), 0.02),
        'mu_shift': jax.random.uniform(ks[7], (L, N_SHIFT), jnp.float32),
        'conv_w': nrm(ks[8], (L, CONV_WIDTH, C_CONV), CONV_WIDTH ** -0.5),
        'conv_b': nrm(ks[9], (L, C_CONV), 0.02),
        'conv_ln_g': 1.0 + nrm(ks[10], (L, C_CONV), 0.05),
        'conv_ln_b': nrm(ks[11], (L, C_CONV), 0.02),
        'rwkv_w0': jax.random.uniform(ks[12], (L, C_RWKV), jnp.float32, -5.0, 1.0),
        'rwkv_w2': nrm(ks[13], (L, R_DECAY, C_RWKV), 0.1),
        'rwkv_a0': nrm(ks[14], (L, C_RWKV), 0.1),
        'rwkv_a2': nrm(ks[15], (L, R_ICLR, C_RWKV), 0.1),
        'rwkv_g2': nrm(ks[16], (L, R_GATE, C_RWKV), R_GATE ** -0.5),
        'rwkv_k_k': 0.85 + nrm(ks[17], (L, C_RWKV), 0.05),
        'rwkv_k_a': 1.0 + nrm(ks[18], (L, C_RWKV), 0.05),
        'rwkv_r_k': nrm(ks[19], (L, H_RWKV, HEAD_RWKV), 0.1),
        'rwkv_ln_g': 1.0 + nrm(ks[20], (L, C_RWKV), 0.05),
        'rwkv_ln_b': nrm(ks[21], (L, C_RWKV), 0.02),
        'w_out': nrm(ks[22], (L, D_MIX, D_MODEL), BETA * D_MIX ** -0.5),
        'ln1_g': 1.0 + nrm(ks[23], (L, D_MODEL), 0.05),
        'ln1_b': nrm(ks[24], (L, D_MODEL), 0.02),
        'router_w': nrm(ks[25], (L, D_MODEL, N_EXPERTS), D_MODEL ** -0.5),
        'router_b': nrm(ks[26], (L, N_EXPERTS), 0.01),
        'w_gate': nrm(ks[27], (L, N_EXPERTS, D_MODEL, D_FF), D_MODEL ** -0.5),
        'b_gate': nrm(ks[28], (L, N_EXPERTS, D_FF), 0.01),
        'w_up': nrm(ks[29], (L, N_EXPERTS, D_MODEL, D_FF), D_MODEL ** -0.5),
        'b_up': nrm(ks[30], (L, N_EXPERTS, D_FF), 0.01),
        'w_down': nrm(ks[31], (L, N_EXPERTS, D_FF, D_MODEL), BETA * D_FF ** -0.5),
        'b_down': nrm(ks[32], (L, N_EXPERTS, D_MODEL), 0.01),
        'ln2_g': 1.0 + nrm(ks[33], (L, D_MODEL), 0.05),
        'ln2_b': nrm(ks[34], (L, D_MODEL), 0.02),
    }


def reference(x_prompt, x_sample, state_conv, state_shift, state_wkv, w_in, b_in, mu_shift,
              conv_w, conv_b, conv_ln_g, conv_ln_b, rwkv_w0, rwkv_w2, rwkv_a0, rwkv_a2, rwkv_g2,
              rwkv_k_k, rwkv_k_a, rwkv_r_k, rwkv_ln_g, rwkv_ln_b, w_out, ln1_g, ln1_b,
              router_w, router_b, w_gate, b_gate, w_up, b_up, w_down, b_down, ln2_g, ln2_b):
    n_p = x_prompt.shape[0]
    y_p, y_s = x_prompt, x_sample
    conv_p, shift_p, wkv_p, conv_s, shift_s, wkv_s = [], [], [], [], [], []
    for d in range(DEPTH):
        lp = {
            'w_in': w_in[d], 'b_in': b_in[d], 'mu_shift': mu_shift[d],
            'conv_w': conv_w[d], 'conv_b': conv_b[d], 'conv_ln_g': conv_ln_g[d], 'conv_ln_b': conv_ln_b[d],
            'rwkv_w0': rwkv_w0[d], 'rwkv_w2': rwkv_w2[d], 'rwkv_a0': rwkv_a0[d], 'rwkv_a2': rwkv_a2[d],
            'rwkv_g2': rwkv_g2[d], 'rwkv_k_k': rwkv_k_k[d], 'rwkv_k_a': rwkv_k_a[d], 'rwkv_r_k': rwkv_r_k[d],
            'rwkv_ln_g': rwkv_ln_g[d], 'rwkv_ln_b': rwkv_ln_b[d], 'w_out': w_out[d],
            'ln1_g': ln1_g[d], 'ln1_b': ln1_b[d], 'router_w': router_w[d], 'router_b': router_b[d],
            'w_gate': w_gate[d], 'b_gate': b_gate[d], 'w_up': w_up[d], 'b_up': b_up[d],
            'w_down': w_down[d], 'b_down': b_down[d], 'ln2_g': ln2_g[d], 'ln2_b': ln2_b[d],
        }
        zero_conv = jnp.zeros((n_p, CONV_WIDTH - 1, C_CONV), x_prompt.dtype)
        zero_shift = jnp.zeros((n_p, 1, N_SHIFT), x_prompt.dtype)
        zero_wkv = jnp.zeros((n_p, H_RWKV, HEAD_RWKV, HEAD_RWKV), state_wkv.dtype)
        y_p, cp, sp, wp = _layer(y_p, zero_conv, zero_shift, zero_wkv, lp)
        y_s, cs, ss, ws = _layer(y_s, state_conv[d], state_shift[d], state_wkv[d], lp)
        conv_p.append(cp); shift_p.append(sp); wkv_p.append(wp)
        conv_s.append(cs); shift_s.append(ss); wkv_s.append(ws)
    return (y_p, y_s, jnp.stack(conv_p), jnp.stack(shift_p), jnp.stack(wkv_p),
            jnp.stack(conv_s), jnp.stack(shift_s), jnp.stack(wkv_s))
```

```python
import os
import numpy as np
from contextlib import ExitStack
import concourse.bass as bass
import concourse.mybir as mybir
from concourse.bass_utils import run_bass_kernel_spmd

F32 = mybir.dt.float32
BF16 = mybir.dt.bfloat16
I32 = mybir.dt.int32
AF = mybir.ActivationFunctionType
ALU = mybir.AluOpType

D = 2048
C_CONV = 1024
NSH = 3360
P_IN = 5408
NEXP = 32
ALPHA = 2.0 ** 0.25
LN_EPS = 1e-5
GN_EPS = 64e-5
DEC = 0.6065306597126334
CH = 64
T = 256
TS = 128
NOWN = 4096
NTILES = NOWN // T
CAP = 768
NROWS = NEXP * CAP
NSUB = 33
XROW = 2050


class _Op:
    __slots__ = ("eng", "fn", "reads", "writes", "semkey", "idx", "waits", "inc", "tick", "is_dma")


class Prog:
    ISSUE = {"pe": "pe", "act": "act", "dve": "dve", "pool": "pool", "sp": "sp", "aq": "act", "gq": "pool"}
    ENG = {"pe": "tensor", "act": "scalar", "dve": "vector", "pool": "gpsimd", "sp": "sync"}

    def __init__(self, nc, stack):
        self.nc = nc
        self.stack = stack
        self.ops = []
        self.last_w = {}
        self.readers = {}
        self.cnt = {}
        self.waited = {}
        self.sems = {}
        self.pending_barrier = None
        self.barrier_done = {}
        self.n_emitted = 0

    def add(self, eng, fn, reads=(), writes=(), semkey=None):
        op = _Op()
        op.eng = eng
        op.fn = fn
        op.reads = tuple(reads)
        op.writes = tuple(writes)
        op.is_dma = eng in ("sp", "aq", "gq")
        op.semkey = semkey if semkey is not None else (("dma_" + eng) if op.is_dma else None)
        op.waits = {}
        op.inc = False
        op.tick = 0
        op.idx = len(self.ops)
        self.ops.append(op)
        return op

    def _chan(self, op):
        return op.semkey if op.is_dma else op.eng

    def _sem(self, c):
        if c not in self.sems:
            self.sems[c] = self.stack.enter_context(self.nc.semaphore("s_" + str(c)))
        return self.sems[c]

    def flush(self):
        nc = self.nc
        ops = self.ops
        if not ops:
            return
        n = len(ops)
        deps = [None] * n
        last_w, readers = {}, {}
        for op in ops:
            d = set()
            for k in op.reads:
                if k in last_w:
                    d.add(last_w[k])
            for k in op.writes:
                if k in last_w:
                    d.add(last_w[k])
                for r in readers.get(k, ()):
                    d.add(r)
            d.discard(op.idx)
            best = {}
            keep = set()
            for di in d:
                dop = ops[di]
                if dop.is_dma:
                    keep.add(di)
                else:
                    if dop.eng not in best or di > best[dop.eng]:
                        best[dop.eng] = di
            keep.update(best.values())
            deps[op.idx] = keep
            for k in op.reads:
                readers.setdefault(k, []).append(op.idx)
            for k in op.writes:
                last_w[k] = op.idx
                readers[k] = []

        def skip(dop, op):
            return (not dop.is_dma) and (not op.is_dma) and dop.eng == "pe" and op.eng == "pe"

        needed = [False] * n
        for op in ops:
            for d in deps[op.idx]:
                if not skip(ops[d], op):
                    needed[d] = True
        lastc = {}
        for op in ops:
            if not op.is_dma:
                lastc[op.eng] = op.idx
        for e, i in lastc.items():
            needed[i] = True
        for op in ops:
            if op.is_dma or needed[op.idx]:
                c = self._chan(op)
                self.cnt[c] = self.cnt.get(c, 0) + (16 if op.is_dma else 1)
                op.tick = self.cnt[c]
                op.inc = True
                self._sem(c)
        bar = self.pending_barrier
        grp_final = {}
        for op in ops:
            if op.is_dma and str(op.semkey).startswith("ld"):
                grp_final[op.semkey] = op.tick
        streams = {"pe": [], "act": [], "dve": [], "pool": [], "sp": []}
        for op in ops:
            ie = self.ISSUE[op.eng]
            w = {}
            for d in deps[op.idx]:
                dop = ops[d]
                if skip(dop, op):
                    continue
                c = self._chan(dop)
                if dop.is_dma and c in grp_final and op.is_dma and str(op.semkey).startswith("ld"):
                    continue
                w[c] = max(w.get(c, 0), grp_final.get(c, dop.tick) if dop.is_dma else dop.tick)
            if bar is not None and not self.barrier_done.get(ie, False):
                for c, t in bar.items():
                    w[c] = max(w.get(c, 0), t)
                self.barrier_done[ie] = True
            for c, t in list(w.items()):
                if self.waited.get((ie, c), 0) >= t:
                    del w[c]
                else:
                    self.waited[(ie, c)] = t
            op.waits = w
            streams[ie].append(op)
        sems = self.sems
        chan = self._chan
        with nc.Block() as block:
            def make(lst):
                def body(eng):
                    for op in lst:
                        for c, t in op.waits.items():
                            eng.wait_ge(sems[c], t)
                        ins = op.fn(eng)
                        if op.inc:
                            ins.then_inc(sems[chan(op)], 16 if op.is_dma else 1)
                return body
            for ename, lst in streams.items():
                if lst:
                    getattr(block, self.ENG[ename])(make(lst))
        self.n_emitted += n
        self.ops = []
        self.pending_barrier = dict(self.cnt)
        self.barrier_done = {}

    def finish(self):
        self.flush()
        nc = self.nc
        cnt = dict(self.cnt)
        sems = self.sems
        with nc.Block() as block:
            @block.sync
            def _(eng):
                for c, t in cnt.items():
                    eng.wait_ge(sems[c], t)


def fap(base, dims, off=0):
    return bass.AP(base.tensor, base.offset + off, [list(base.ap[0])] + [list(d) for d in dims])


def build_nc(stage=99):
    nc = bass.Bass("TRN2", target_bir_lowering=False)
    dI = lambda n, s, dt=F32: nc.dram_tensor(n, list(s), dt, kind="ExternalInput").ap()
    dO = lambda n, s, dt=F32: nc.dram_tensor(n, list(s), dt, kind="ExternalOutput").ap()
    dS = lambda n, s, dt=F32: nc.dram_tensor(n, list(s), dt, kind="Internal").ap()
    xo = dI("xo", [NOWN, D]); xp = dI("xp", [NOWN, D]); xs = dI("xs", [16, D]); flag_d = dI("flag", [128, 1])
    w_in = dI("w_in", [D, P_IN]); binT_d = dI("binT", [128, 43]); muT_d = dI("muT", [128, 27])
    cwT_d = dI("cwT", [128, 8, 31]); cvec_d = dI("cvec", [128, 3, 8])
    pvec_d = dI("pvec", [128, 7, 8])
    w2_d = dI("w2", [64, 1024]); a2_d = dI("a2", [64, 1024]); g2_d = dI("g2", [160, 1024])
    w_out = dI("w_out", [D, D]); lnv_d = dI("lnv", [4, D])
    rw_d = dI("rw", [D, NEXP]); rb_d = dI("rb", [1, NEXP])
    if stage >= 4:
        w_gate = dI("w_gate", [NEXP, D, D]); w_up = dI("w_up", [NEXP, D, D]); w_down = dI("w_down", [NEXP, D, D])
        bgu_d = dI("bgu", [128, 2, NEXP, 16]); bdn_d = dI("bdn", [NEXP, D])
    sconv_d = dI("sconv", [30, C_CONV]); sshT_d = dI("sshT", [128, 27]); swkv_d = dI("swkv", [8, 128, 64])
    ident_d = dI("ident", [128, 128]); maskh_d = dI("maskh", [128, 2]); m1_d = dI("m1", [128, 256]); m2_d = dI("m2", [128, 256])
    onesbd_d = dI("onesbd", [128, 128]); tri_d = dI("tri", [128, 128]); ecap_d = dI("ecap", [128, NEXP]); rvt_d = dI("rvt", [128, 2])
    y_own = dO("y_own", [NSUB * 128, D])
    conv_o = dO("conv_o", [30, C_CONV]); conv_so = dO("conv_so", [30, C_CONV])
    shift_o = dO("shift_o", [128, 27]); shift_so = dO("shift_so", [128, 27])
    wkv_o = dO("wkv_o", [8, 128, 64]); wkv_so = dO("wkv_so", [8, 128, 64])
    x1s = dS("x1s", [NSUB * 128, D])
    xsorted = dS("xsorted", [NROWS + 128, XROW], BF16)
    ysorted = dS("ysorted", [NROWS, D])

    with ExitStack() as top:
        P = Prog(nc, top)
        A = P.add
        sbt = lambda st, n, s, dt=F32: st.enter_context(nc.sbuf_tensor("sb_" + n, list(s), dt))
        pbank = [top.enter_context(nc.psum_tensor(f"pb{i}", [128, 512], F32)) for i in range(8)]
        PS = {}
        for i_, n_ in enumerate(("pj0", "pj1", "m0", "m1", "fb0", "fb1", "c0", "c1")):
            PS[n_] = pbank[i_][:, :]
        rot = {}
        done_subs = []

        def nxt(prefix, n):
            i = rot.get(prefix, 0)
            rot[prefix] = (i + 1) % n
            return f"{prefix}{i}"

        cst = top
        ident = sbt(cst, "ident", [128, 128]); identb = sbt(cst, "identb", [128, 128], BF16)
        maskh = sbt(cst, "maskh", [128, 2]); m1 = sbt(cst, "m1", [128, 256]); m2 = sbt(cst, "m2", [128, 256])
        onesbd = sbt(cst, "onesbd", [128, 128]); flag = sbt(cst, "flag", [128, 1])
        idx_all = sbt(top, "idx_all", [128, NSUB, 4], I32)
        rvt = sbt(top, "rvt", [128, 2])
        A("sp", lambda e: e.dma_start(out=rvt[:], in_=rvt_d), writes=["rvt"], semkey="ld0")
        epsc = sbt(top, "epsc", [128, 3])
        A("pool", lambda e: e.memset(epsc[:, 0:1], LN_EPS), writes=["epsc"])
        A("pool", lambda e: e.memset(epsc[:, 1:2], GN_EPS), writes=["epsc"])
        A("pool", lambda e: e.memset(epsc[:, 2:3], 1e-24), writes=["epsc"])

        def rsqrt(dst, src, col, rk, wk):
            A("act", lambda e: e.activation(out=dst, in_=src, func=AF.Sqrt, bias=epsc[0:dst.shape[0], col:col + 1], scale=1.0), reads=rk + ["epsc"], writes=wk)
            A("dve", lambda e: e.reciprocal(out=dst, in_=dst), reads=wk, writes=wk)
        gd_all = sbt(top, "gd_all", [128, NSUB, NEXP])
        A("sp", lambda e: e.dma_start(out=ident[:], in_=ident_d), writes=["ident"], semkey="ld0")
        A("gq", lambda e: e.dma_start(out=identb[:], in_=ident_d), writes=["identb"], semkey="ld1")
        A("sp", lambda e: e.dma_start(out=maskh[:], in_=maskh_d), writes=["maskh"], semkey="ld0")
        A("sp", lambda e: e.dma_start(out=m1[:], in_=m1_d), writes=["m1"], semkey="ld0")
        A("sp", lambda e: e.dma_start(out=m2[:], in_=m2_d), writes=["m2"], semkey="ld0")
        A("sp", lambda e: e.dma_start(out=onesbd[:], in_=onesbd_d), writes=["onesbd"], semkey="ld0")
        A("sp", lambda e: e.dma_start(out=flag[:], in_=flag_d), writes=["flag"], semkey="ld0")

        w_in_bf = dS("w_in_bf", [D, P_IN], BF16)
        w_out_bf = dS("w_out_bf", [D, D], BF16)
        for q in range(8):
            A("gq", lambda e, q=q: e.dma_start(out=w_in_bf[q * 256:(q + 1) * 256, :], in_=w_in[q * 256:(q + 1) * 256, :]), writes=["w_in_bf"], semkey="ld1")
            A("gq", lambda e, q=q: e.dma_start(out=w_out_bf[q * 256:(q + 1) * 256, :], in_=w_out[q * 256:(q + 1) * 256, :]), writes=["w_out_bf"], semkey="ld1")
        with ExitStack() as pa:
            binT = sbt(pa, "binT", [128, 43]); muT = sbt(pa, "muT", [128, 27])
            cwT = sbt(pa, "cwT", [128, 8, 31]); cvec = sbt(pa, "cvec", [128, 3, 8]); pvec = sbt(pa, "pvec", [128, 7, 8])
            w2a2 = sbt(pa, "w2a2", [128, 1024]); g2a = sbt(pa, "g2a", [128, 1024]); g2b = sbt(pa, "g2b", [32, 1024])
            for (t_, d_, k_) in ((binT, binT_d, "binT"), (muT, muT_d, "muT"), (cwT, cwT_d, "cwT"), (cvec, cvec_d, "cvec"), (pvec, pvec_d, "pvec")):
                A("sp", lambda e, t_=t_, d_=d_: e.dma_start(out=t_[:], in_=d_), writes=[k_], semkey="ld0")
            A("sp", lambda e: e.dma_start(out=w2a2[0:64, :], in_=w2_d), writes=["w2a2"], semkey="ld0")
            A("sp", lambda e: e.dma_start(out=w2a2[64:128, :], in_=a2_d), writes=["w2a2"], semkey="ld0")
            A("sp", lambda e: e.dma_start(out=g2a[:], in_=g2_d[0:128, :]), writes=["g2a"], semkey="ld0")
            A("sp", lambda e: e.dma_start(out=g2b[:], in_=g2_d[128:160, :]), writes=["g2b"], semkey="ld0")
            lnv = sbt(pa, "lnv", [128, 2, D])
            A("sp", lambda e: e.dma_start(out=lnv[:, 0, :], in_=lnv_d[0:1, :].partition_broadcast(128)), writes=["lnv"], semkey="ld0")
            A("sp", lambda e: e.dma_start(out=lnv[:, 1, :], in_=lnv_d[1:2, :].partition_broadcast(128)), writes=["lnv"], semkey="ld0")
            rw = sbt(pa, "rw", [128, 16, NEXP]); rb = sbt(pa, "rb", [128, NEXP]); ecap = sbt(pa, "ecap", [128, NEXP])
            tri = sbt(pa, "tri", [128, 128], BF16); onesb = sbt(pa, "onesb", [128, 128], BF16)
            A("sp", lambda e: e.dma_start(out=rw[:], in_=rw_d.rearrange("(k p) n -> p k n", p=128)), writes=["rw"], semkey="ld0")
            A("sp", lambda e: e.dma_start(out=rb[:], in_=rb_d.partition_broadcast(128)), writes=["rb"], semkey="ld0")
            A("sp", lambda e: e.dma_start(out=ecap[:], in_=ecap_d), writes=["ecap"], semkey="ld0")
            A("gq", lambda e: e.dma_start(out=tri[:], in_=tri_d), writes=["tri"], semkey="ld1")
            A("pool", lambda e: e.memset(onesb[:], 1.0), writes=["onesb"])
            onesf = sbt(pa, "onesf", [128, 128]); A("pool", lambda e: e.memset(onesf[:], 1.0), writes=["onesf"])
            ones_t = sbt(pa, "ones_t", [128, CH]); A("pool", lambda e: e.memset(ones_t[:], 1.0), writes=["ones_t"])
            carry = sbt(pa, "carry", [128, 27]); A("pool", lambda e: e.memset(carry[:], 0.0), writes=["carry"])
            uhist = sbt(pa, "uhist", [128, 8, 30]); A("pool", lambda e: e.memset(uhist[:], 0.0), writes=["uhist"])
            S32 = [sbt(pa, f"S32_{p}", [128, 128]) for p in range(8)]
            Sbf = [sbt(pa, f"Sbf_{p}", [128, 128], BF16) for p in range(8)]
            for p in range(8):
                A("pool", lambda e, p=p: e.memset(S32[p][:], 0.0), writes=[f"S32_{p}"])
                A("pool", lambda e, p=p: e.memset(Sbf[p][:], 0.0), writes=[f"Sbf_{p}"])
            cntbase = sbt(pa, "cntbase", [128, NEXP]); A("pool", lambda e: e.memset(cntbase[:], 0.0), writes=["cntbase"])
            xt = sbt(pa, "xt", [128, D])
            xT = sbt(pa, "xT", [128, 16, T], BF16)
            wst = [sbt(pa, f"wst{i}", [128, 16, 128], BF16) for i in range(2)]
            zraw = [sbt(pa, f"zraw{i}", [128, T + 1]) for i in range(2)]
            zd = [sbt(pa, f"zd{i}", [128, T]) for i in range(2)]
            zg = sbt(pa, "zg", [128, 12, T])
            zl = sbt(pa, "zl", [128, 3, T]); lor = zl
            uT = sbt(pa, "uT", [128, 8, 30 + T])
            sgt = [sbt(pa, f"sgt{i}", [128, T]) for i in range(2)]
            cacc = [sbt(pa, f"cacc{i}", [128, T]) for i in range(2)]
            csq = [sbt(pa, f"csq{i}", [128, T]) for i in range(2)]
            cfull = sbt(pa, "cfull", [128, 8, T])
            lnm = sbt(pa, "lnm", [128, 3, T])
            catT = sbt(pa, "catT", [128, 16, T], BF16)
            NR = 4
            PBQ = ["m0", "m1", "fb0", "fb1"]
            def mk(n, s, dt=F32):
                return [sbt(pa, f"{n}{i}", s, dt) for i in range(NR)]
            e_sg = mk("e_sg", [128, TS]); e_a = mk("e_a", [128, TS]); e_kk0 = mk("e_kk0", [128, TS])
            e_t = mk("e_t", [128, TS]); e_kk = mk("e_kk", [128, TS]); e_km = mk("e_km", [128, TS])
            e_cs = mk("e_cs", [128, TS]); e_csm = mk("e_csm", [128, TS]); e_eg = mk("e_eg", [128, TS]); e_ieg = mk("e_ieg", [128, TS])
            e_egm = mk("e_egm", [128, TS]); e_n1 = mk("e_n1", [128, TS], BF16); e_n2 = mk("e_n2", [128, TS], BF16)
            e_n3 = mk("e_n3", [128, TS], BF16); e_t2 = mk("e_t2", [128, TS])
            NCS = TS // CH
            AR = [sbt(pa, f"AR{q}", [128, NCS, 192], BF16) for q in range(4)]
            BT = [sbt(pa, f"BT{q}", [128, NCS, 128], BF16) for q in range(4)]
            KT = [sbt(pa, f"KT{q}", [128, NCS, 128], BF16) for q in range(4)]
            VT = [sbt(pa, f"VT{q}", [128, NCS, 128], BF16) for q in range(4)]
            TOK = [sbt(pa, f"TOK{q}", [128, NCS, 384], BF16) for q in range(4)]
            GC = [sbt(pa, f"GC{q}", [128, NCS]) for q in range(4)]
            BON = [sbt(pa, f"BON{q}", [128, TS]) for q in range(4)]
            GG = [sbt(pa, f"GG{q}", [128, TS]) for q in range(4)]
            YS = [sbt(pa, f"YS{q}", [128, TS]) for q in range(4)]
            NU = 4
            Lt = [sbt(pa, f"Lt{i}", [128, 640], BF16) for i in range(NU)]
            Xa = [sbt(pa, f"Xa{i}", [128, 384], BF16) for i in range(NU)]
            Xb = [sbt(pa, f"Xb{i}", [128, 384], BF16) for i in range(NU)]
            for i_ in range(NU):
                A("pool", lambda e, i_=i_: e.tensor_copy(out=Lt[i_][:, 256:384], in_=identb[:]), reads=["identb"], writes=[f"Lt{i_}"])
            TT = [sbt(pa, f"TT{q}", [128, 128], BF16) for q in range(4)]
            Wb = [sbt(pa, f"Wb{q}", [128, 128], BF16) for q in range(4)]
            Ub = [sbt(pa, f"Ub{q}", [128, 128], BF16) for q in range(4)]
            stmp = [sbt(pa, f"stmp{i}", [128, 128]) for i in range(2)]
            xrow = [sbt(pa, f"xrow{i}", [128, XROW], BF16) for i in range(2)]
            x1Tb = [sbt(pa, "x1Tb0", [128, 4, 128])] * 2
            A("pool", lambda e: e.memset(xrow[0][:], 0.0), writes=["xrow0"])
            xs_v = xsorted.rearrange("(r p) c -> p r c", p=128)
            nblk_x = (NROWS + 128) // 128
            for q0 in range(0, nblk_x, 20):
                nb_ = min(20, nblk_x - q0)
                A("sp", lambda e, q0=q0, nb_=nb_: e.dma_start(out=xs_v[:, q0:q0 + nb_, :], in_=fap(xrow[0][:], [[0, nb_], [1, XROW]])), reads=["xrow0"], writes=["xsorted"], semkey="ld0")
            bst = sbt(pa, "bst", [128, 4, 6]); mv = sbt(pa, "mv", [128, 2]); rstd = sbt(pa, "rstd", [128, 1])
            lg = sbt(pa, "lg", [128, NEXP]); m8 = sbt(pa, "m8", [128, 8]); negm = sbt(pa, "negm", [128, 1])
            e4 = sbt(pa, "e4", [128, 4]); s4 = sbt(pa, "s4", [128, 1]); g4 = sbt(pa, "g4", [128, 4])
            oh = sbt(pa, "oh", [128, 4, NEXP]); msk = sbt(pa, "msk", [128, NEXP]); mskb = sbt(pa, "mskb", [128, NEXP], BF16)
            posf = sbt(pa, "posf", [128, NEXP]); pk = sbt(pa, "pk", [128, 4]); junk = sbt(pa, "junk", [128, NEXP])
            cT8 = sbt(pa, "cT8", [30, C_CONV])
            idx_sc = sbt(pa, "idx_sc", [128, 4], I32)

            def load_x(src_ap, rows):
                if rows < 128:
                    A("pool", lambda e: e.memset(xt[:], 0.0), writes=["xt"])
                A("sp", lambda e: e.dma_start(out=xt[0:rows, :], in_=src_ap), writes=["xt"], semkey="x0")

            def transpose_x(col0, ncol):
                for g in range(4):
                    pn = nxt("pj", 2)
                    for j in range(4):
                        kc = 4 * g + j
                        A("pe", lambda e, pn=pn, j=j, kc=kc: e.transpose(out=PS[pn][:, j * 128:(j + 1) * 128], in_=xt[:, kc * 128:(kc + 1) * 128], identity=ident[:]),
                          reads=["xt", "ident"], writes=[pn])
                    eng = "act" if g % 2 == 0 else "dve"
                    def ev(e, pn=pn, g=g, eng=eng):
                        src = PS[pn].rearrange("p (j t) -> p j t", j=4)[:, :, 0:ncol]
                        dst = xT[:, 4 * g:4 * g + 4, col0:col0 + ncol]
                        return e.copy(out=dst, in_=src) if eng == "act" else e.tensor_copy(out=dst, in_=src)
                    A(eng, ev, reads=[pn], writes=["xT"])

            wst_rot = [0]

            def proj_cols(cts, ncol, consume):
                for ct in cts:
                    wi = wst_rot[0]; wst_rot[0] ^= 1
                    width = min(128, P_IN - ct * 128)
                    A("sp", lambda e, ct=ct, width=width, wi=wi: e.dma_start(out=wst[wi][:, :, 0:width],
                                                                           in_=w_in_bf[:, ct * 128:ct * 128 + width].rearrange("(k p) n -> p k n", p=128)),
                      reads=["w_in_bf"], writes=[f"wst{wi}"], semkey=f"wst{wi}")
                    pn = nxt("pj", 2)
                    for kc in range(16):
                        A("pe", lambda e, pn=pn, kc=kc, width=width, wi=wi: e.matmul(PS[pn][0:width, 0:ncol], lhsT=wst[wi][:, kc, 0:width],
                                                                                  rhs=xT[:, kc, 0:ncol], start=(kc == 0), stop=(kc == 15)),
                          reads=[f"wst{wi}", "xT"], writes=[pn])
                    consume(ct, pn, width)

            zr_rot = [0]

            def z_consume(ncol, valid, dst, dstk, slot_of):
                def consume(ct, pn, width):
                    zi = ct - 16
                    sl = slot_of(ct)
                    ri = zr_rot[0]; zr_rot[0] = (ri + 1) % 2
                    zr, zk = zraw[ri], f"zraw{ri}"
                    di = ri % 2
                    A("act", lambda e: e.activation(out=zr[0:width, 1:ncol + 1], in_=PS[pn][0:width, 0:ncol], func=AF.Identity, bias=binT[0:width, ct:ct + 1], scale=1.0),
                      reads=[pn, "binT"], writes=[zk])
                    A("pool", lambda e: e.tensor_copy(out=zr[0:width, 0:1], in_=carry[0:width, zi:zi + 1]), reads=["carry"], writes=[zk])
                    A("pool", lambda e: e.tensor_copy(out=carry[0:width, zi:zi + 1], in_=zr[0:width, valid:valid + 1]), reads=[zk], writes=["carry"])
                    A("dve", lambda e: e.tensor_tensor(out=zd[di][0:width, 0:ncol], in0=zr[0:width, 0:ncol], in1=zr[0:width, 1:ncol + 1], op=ALU.subtract),
                      reads=[zk], writes=[f"zd{di}"])
                    A("dve", lambda e: e.scalar_tensor_tensor(out=dst[0:width, sl, 0:ncol], in0=zd[di][0:width, 0:ncol], scalar=muT[0:width, zi:zi + 1],
                                                              in1=zr[0:width, 1:ncol + 1], op0=ALU.mult, op1=ALU.add),
                      reads=[f"zd{di}", zk, "muT"], writes=[dstk])
                return consume

            def u_consume(ncol):
                def consume(ct, pn, width):
                    if ct >= 8:
                        gi = ct - 8
                        A("act", lambda e: e.activation(out=sgt[gi % 2][:, 0:ncol], in_=PS[pn][:, 0:ncol], func=AF.Sigmoid, bias=binT[:, ct:ct + 1], scale=1.0),
                          reads=[pn, "binT"], writes=[f"sgt{gi % 2}"])
                    else:
                        A("dve", lambda e: e.scalar_tensor_tensor(out=uT[:, ct, 30:30 + ncol], in0=PS[pn][:, 0:ncol], scalar=binT[:, ct:ct + 1],
                                                                  in1=sgt[ct % 2][:, 0:ncol], op0=ALU.add, op1=ALU.mult),
                          reads=[pn, "binT", f"sgt{ct % 2}"], writes=["uT"])
                return consume

            def glu_proj(ncol):
                for ci in range(8):
                    proj_cols([8 + ci, ci], ncol, u_consume(ncol))

            def conv_block(ncol):
                for ci in range(8):
                    ai = ci % 2
                    A("dve", lambda e, ci=ci, ai=ai: e.tensor_scalar(out=cacc[ai][:, 0:ncol], in0=uT[:, ci, 0:ncol], scalar1=cwT[:, ci, 0:1], scalar2=cvec[:, 0, ci:ci + 1],
                                                                    op0=ALU.mult, op1=ALU.add), reads=["uT", "cwT", "cvec"], writes=[f"cacc{ai}"])
                    for j in range(1, 31):
                        last = (j == 30)
                        A("dve", lambda e, ci=ci, ai=ai, j=j, last=last: e.scalar_tensor_tensor(
                            out=(cfull[:, ci, 0:ncol] if last else cacc[ai][:, 0:ncol]), in0=uT[:, ci, j:j + ncol], scalar=cwT[:, ci, j:j + 1],
                            in1=cacc[ai][:, 0:ncol], op0=ALU.mult, op1=ALU.add),
                          reads=["uT", "cwT", f"cacc{ai}"], writes=(["cfull"] if last else [f"cacc{ai}"]))
                    A("act", lambda e, ci=ci, ai=ai: e.activation(out=csq[ai][:, 0:ncol], in_=cfull[:, ci, 0:ncol], func=AF.Square), reads=["cfull"], writes=[f"csq{ai}"])
                    A("pe", lambda e, ci=ci: e.matmul(PS["m0"][:, 0:ncol], lhsT=onesf[:], rhs=cfull[:, ci, 0:ncol], start=(ci == 0), stop=(ci == 7)),
                      reads=["cfull", "onesf"], writes=["m0"])
                    A("pe", lambda e, ci=ci, ai=ai: e.matmul(PS["m1"][:, 0:ncol], lhsT=onesf[:], rhs=csq[ai][:, 0:ncol], start=(ci == 0), stop=(ci == 7)),
                      reads=[f"csq{ai}", "onesf"], writes=["m1"])
                A("act", lambda e: e.activation(out=lnm[:, 0, 0:ncol], in_=PS["m0"][:, 0:ncol], func=AF.Copy, scale=1.0 / C_CONV), reads=["m0"], writes=["lnm"])
                A("pool", lambda e: e.tensor_tensor(out=lnm[:, 1, 0:ncol], in0=lnm[:, 0, 0:ncol], in1=lnm[:, 0, 0:ncol], op=ALU.mult), reads=["lnm"], writes=["lnm"])
                A("dve", lambda e: e.scalar_tensor_tensor(out=lnm[:, 2, 0:ncol], in0=PS["m1"][:, 0:ncol], scalar=1.0 / C_CONV, in1=lnm[:, 1, 0:ncol], op0=ALU.mult, op1=ALU.subtract),
                  reads=["m1", "lnm"], writes=["lnm"])
                rsqrt(lnm[:, 2, 0:ncol], lnm[:, 2, 0:ncol], 0, ["lnm"], ["lnm"])
                for ci in range(8):
                    ai = ci % 2
                    A("pool", lambda e, ci=ci, ai=ai: e.tensor_tensor(out=csq[ai][:, 0:ncol], in0=cfull[:, ci, 0:ncol], in1=lnm[:, 0, 0:ncol], op=ALU.subtract), reads=["cfull", "lnm"], writes=[f"csq{ai}"])
                    A("dve", lambda e, ci=ci, ai=ai: e.tensor_tensor(out=csq[ai][:, 0:ncol], in0=csq[ai][:, 0:ncol], in1=lnm[:, 2, 0:ncol], op=ALU.mult), reads=[f"csq{ai}", "lnm"], writes=[f"csq{ai}"])
                    A("act", lambda e, ci=ci, ai=ai: e.activation(out=catT[:, ci, 0:ncol], in_=csq[ai][:, 0:ncol], func=AF.Silu, bias=cvec[:, 2, ci:ci + 1], scale=cvec[:, 1, ci:ci + 1]),
                      reads=[f"csq{ai}", "cvec"], writes=["catT"])

            def save_uhist(valid):
                A("pool", lambda e: e.tensor_copy(out=uhist[:], in_=uT[:, :, valid:valid + 30]), reads=["uT"], writes=["uhist"])

            def load_uhist():
                A("pool", lambda e: e.tensor_copy(out=uT[:, :, 0:30], in_=uhist[:]), reads=["uhist"], writes=["uT"])

            def lora_acts(ncol, full):
                A("act", lambda e: e.activation(out=lor[0:64, 0, 0:ncol], in_=zl[0:64, 0, 0:ncol], func=AF.Tanh), reads=["zl"], writes=["zl"])
                if full:
                    A("act", lambda e: e.activation(out=lor[:, 1, 0:ncol], in_=zl[:, 1, 0:ncol], func=AF.Sigmoid), reads=["zl"], writes=["zl"])
                    A("act", lambda e: e.activation(out=lor[0:32, 2, 0:ncol], in_=zl[0:32, 2, 0:ncol], func=AF.Sigmoid), reads=["zl"], writes=["zl"])

            def bd(eng_name, dst3, src2, nch, keyr, keyw):
                def f(e):
                    in0 = fap(src2, [[CH, nch], [0, 2], [1, CH]])
                    in1 = fap(maskh[:], [[0, nch], [1, 2], [0, CH]])
                    return e.tensor_tensor(out=dst3, in0=in0, in1=in1, op=ALU.mult)
                A(eng_name, f, reads=keyr + ["maskh"], writes=keyw)

            def pair_prep(p, col0, ncol, nch, full, valid):
                q = p % 4
                i = q % NR
                pv = lambda j: pvec[:, j, p:p + 1]
                cw = slice(col0, col0 + ncol)
                zr_ = zg[:, q, cw]; zk_ = zg[:, 4 + q, cw]; zv_ = zg[:, 8 + q, cw]
                K = lambda n: [f"{n}{i}"]
                cs_ = slice(p * 128, (p + 1) * 128)
                N = slice(0, ncol)
                if valid < ncol:
                    for sl in (q, 4 + q, 8 + q):
                        yield A("pool", lambda e, sl=sl: e.memset(zg[:, sl, col0 + valid:col0 + ncol], 0.0), writes=["zg"])
                pw = PBQ[q]
                yield A("pe", lambda e: e.matmul(PS[pw][:, N], lhsT=w2a2[0:64, cs_], rhs=lor[0:64, 0, cw], start=True, stop=True), reads=["w2a2", "zl"], writes=[pw])
                yield A("act", lambda e: e.activation(out=e_sg[i][:, N], in_=PS[pw][:, N], func=AF.Sigmoid, bias=pv(0), scale=1.0), reads=[pw, "pvec"], writes=K("e_sg"))
                if valid < ncol:
                    yield A("pool", lambda e: e.memset(e_sg[i][:, valid:ncol], 0.0), writes=K("e_sg"))
                pa_ = PBQ[q]
                yield A("pe", lambda e: e.matmul(PS[pa_][:, N], lhsT=w2a2[64:128, cs_], rhs=lor[64:128, 0, cw], start=True, stop=True), reads=["w2a2", "zl"], writes=[pa_])
                yield A("act", lambda e: e.activation(out=e_a[i][:, N], in_=PS[pa_][:, N], func=AF.Sigmoid, bias=pv(1), scale=1.0), reads=[pa_, "pvec"], writes=K("e_a"))
                if full:
                    pg = PBQ[q]
                    yield A("pe", lambda e: e.matmul(PS[pg][:, N], lhsT=g2a[:, cs_], rhs=lor[:, 1, cw], start=True, stop=False), reads=["g2a", "zl"], writes=[pg])
                    yield A("pe", lambda e: e.matmul(PS[pg][:, N], lhsT=g2b[0:32, cs_], rhs=lor[0:32, 2, cw], start=False, stop=True), reads=["g2b", "zl"], writes=[pg])
                    yield A("act", lambda e: e.copy(out=GG[q][:, N], in_=PS[pg][:, N]), reads=[pg], writes=[f"GG{q}"])
                yield A("dve", lambda e: e.tensor_scalar(out=e_kk0[i][:, N], in0=zk_, scalar1=pv(2), scalar2=None, op0=ALU.mult), reads=["zg", "pvec"], writes=K("e_kk0"))
                yield A("act", lambda e: e.activation(out=e_t[i][:, N], in_=e_kk0[i][:, N], func=AF.Square), reads=K("e_kk0"), writes=K("e_t"))
                pss = PBQ[q]
                yield A("pe", lambda e: e.matmul(PS[pss][:, N], lhsT=onesbd[:], rhs=e_t[i][:, N], start=True, stop=True), reads=["onesbd"] + K("e_t"), writes=[pss])
                yield rsqrt(e_t[i][:, N], PS[pss][:, N], 2, [pss], K("e_t"))
                yield A("dve", lambda e: e.tensor_tensor(out=e_kk[i][:, N], in0=e_kk0[i][:, N], in1=e_t[i][:, N], op=ALU.mult), reads=K("e_kk0") + K("e_t"), writes=K("e_kk"))
                yield A("dve", lambda e: e.tensor_scalar(out=e_t2[i][:, N], in0=e_a[i][:, N], scalar1=-1.0, scalar2=pv(3), op0=ALU.add, op1=ALU.mult), reads=K("e_a") + ["pvec"], writes=K("e_t2"))
                yield A("dve", lambda e: e.scalar_tensor_tensor(out=e_km[i][:, N], in0=e_t2[i][:, N], scalar=1.0, in1=zk_, op0=ALU.add, op1=ALU.mult), reads=K("e_t2") + ["zg"], writes=K("e_km"))
                if full:
                    yield A("dve", lambda e: e.scalar_tensor_tensor(out=e_t2[i][:, N], in0=zr_, scalar=pv(4), in1=e_km[i][:, N], op0=ALU.mult, op1=ALU.mult), reads=["zg", "pvec"] + K("e_km"), writes=K("e_t2"))
                    pb_ = PBQ[q]
                    yield A("pe", lambda e: e.matmul(PS[pb_][:, N], lhsT=onesbd[:], rhs=e_t2[i][:, N], start=True, stop=True), reads=["onesbd"] + K("e_t2"), writes=[pb_])
                    yield A("dve", lambda e: e.tensor_tensor(out=BON[q][:, N], in0=PS[pb_][:, N], in1=zv_, op=ALU.mult), reads=[pb_, "zg"], writes=[f"BON{q}"])
                for c in range(nch):
                    yield A("dve", lambda e, c=c: e.tensor_tensor_scan(out=e_cs[i][:, c * CH:(c + 1) * CH], data0=ones_t[:], data1=e_sg[i][:, c * CH:(c + 1) * CH], initial=0.0, op0=ALU.mult, op1=ALU.add),
                      reads=K("e_sg") + ["ones_t"], writes=K("e_cs"))
                yield A("pool", lambda e: e.tensor_tensor(out=e_csm[i][:, N], in0=e_cs[i][:, N], in1=e_sg[i][:, N], op=ALU.subtract), reads=K("e_cs") + K("e_sg"), writes=K("e_csm"))
                yield A("act", lambda e: e.activation(out=e_eg[i][:, N], in_=e_cs[i][:, N], func=AF.Exp, scale=-DEC), reads=K("e_cs"), writes=K("e_eg"))
                yield A("act", lambda e: e.activation(out=e_ieg[i][:, N], in_=e_cs[i][:, N], func=AF.Exp, scale=DEC), reads=K("e_cs"), writes=K("e_ieg"))
                yield A("act", lambda e: e.activation(out=e_egm[i][:, N], in_=e_csm[i][:, N], func=AF.Exp, scale=-DEC), reads=K("e_csm"), writes=K("e_egm"))
                yield A("pool", lambda e: e.tensor_copy(out=GC[q][:, 0:nch], in_=fap(e_eg[i][:, N], [[CH, nch]], off=CH - 1)), reads=K("e_eg"), writes=[f"GC{q}"])
                yield A("dve", lambda e: e.scalar_tensor_tensor(out=e_n1[i][:, N], in0=e_kk[i][:, N], scalar=-1.0, in1=e_egm[i][:, N], op0=ALU.mult, op1=ALU.mult), reads=K("e_kk") + K("e_egm"), writes=K("e_n1"))
                yield bd("pool", AR[q][:, 0:nch, 0:128].rearrange("p c (h t) -> p c h t", h=2), e_n1[i][:, N], nch, K("e_n1"), [f"AR{q}"])
                yield A("dve", lambda e: e.tensor_tensor(out=e_t[i][:, N], in0=e_kk[i][:, N], in1=e_a[i][:, N], op=ALU.mult), reads=K("e_kk") + K("e_a"), writes=K("e_t"))
                yield A("dve", lambda e: e.tensor_tensor(out=e_n2[i][:, N], in0=e_t[i][:, N], in1=e_ieg[i][:, N], op=ALU.mult), reads=K("e_t") + K("e_ieg"), writes=K("e_n2"))
                yield bd("pool", BT[q][:, 0:nch, :].rearrange("p c (h t) -> p c h t", h=2), e_n2[i][:, N], nch, K("e_n2"), [f"BT{q}"])
                yield A("dve", lambda e: e.tensor_tensor(out=e_n3[i][:, N], in0=e_km[i][:, N], in1=e_ieg[i][:, N], op=ALU.mult), reads=K("e_km") + K("e_ieg"), writes=K("e_n3"))
                yield bd("pool", KT[q][:, 0:nch, :].rearrange("p c (h t) -> p c h t", h=2), e_n3[i][:, N], nch, K("e_n3"), [f"KT{q}"])
                if full:
                    yield A("dve", lambda e: e.tensor_tensor(out=AR[q][:, 0:nch, 128:192], in0=zr_.rearrange("p (c t) -> p c t", t=CH), in1=e_eg[i][:, N].rearrange("p (c t) -> p c t", t=CH), op=ALU.mult),
                      reads=["zg"] + K("e_eg"), writes=[f"AR{q}"])
                yield bd("pool", VT[q][:, 0:nch, :].rearrange("p c (h t) -> p c h t", h=2), zv_, nch, ["zg"], [f"VT{q}"])
                for c in range(nch):
                    pt = PBQ[q]
                    ptb = PS[pt].bitcast(BF16)
                    for j, (src, sn) in enumerate(((BT, "BT"), (KT, "KT"), (VT, "VT"))):
                        yield A("pe", lambda e, j=j, src=src, c=c, ptb=ptb: e.transpose(out=ptb[:, j * 128:(j + 1) * 128], in_=src[q][:, c, :], identity=identb[:]),
                          reads=[f"{sn}{q}", "identb"], writes=[pt])
                    yield A("act", lambda e, c=c, ptb=ptb: e.copy(out=TOK[q][:, c, :], in_=ptb[:, 0:384]), reads=[pt], writes=[f"TOK{q}"])

            u_rot = [0]

            def unit_local(p, c, full):
                q = p % 4
                ui = q
                L, Lk = Lt[ui], f"Lt{ui}"
                nR = 192 if full else 128
                gb = PBQ[q]
                yield A("pe", lambda e: e.matmul(PS[gb][:, 0:128], lhsT=AR[q][:, c, 0:128], rhs=BT[q][:, c, :], start=True, stop=True), reads=[f"AR{q}", f"BT{q}"], writes=[gb])
                yield A("pe", lambda e: e.matmul(PS[gb][:, 128:128 + nR], lhsT=BT[q][:, c, :], rhs=AR[q][:, c, 0:nR], start=True, stop=True), reads=[f"AR{q}", f"BT{q}"], writes=[gb])
                yield A("pe", lambda e: e.matmul(PS[gb][:, 320:320 + nR], lhsT=KT[q][:, c, :], rhs=AR[q][:, c, 0:nR], start=True, stop=True), reads=[f"AR{q}", f"KT{q}"], writes=[gb])
                yield A("dve", lambda e: e.tensor_tensor(out=L[:, 0:256], in0=PS[gb][:, 0:256], in1=m1[:], op=ALU.mult), reads=[gb, "m1"], writes=[Lk])
                if full:
                    yield A("dve", lambda e: e.tensor_tensor(out=L[:, 384:640], in0=PS[gb][:, 256:512], in1=m2[:], op=ALU.mult), reads=[gb, "m2"], writes=[Lk])
                else:
                    yield A("dve", lambda e: e.tensor_tensor(out=L[:, 448:576], in0=PS[gb][:, 320:448], in1=m2[:, 64:192], op=ALU.mult), reads=[gb, "m2"], writes=[Lk])
                cur, curk = L, Lk
                bufs = [(Xa[ui], f"Xa{ui}"), (Xb[ui], f"Xb{ui}")]
                for lev in range(6):
                    fbn = PBQ[q]
                    if lev == 5:
                        yield A("pe", lambda e, cur=cur, fbn=fbn: e.matmul(PS[fbn][:, 256:384], lhsT=cur[:, 0:128], rhs=cur[:, 256:384], start=True, stop=True), reads=[curk], writes=[fbn])
                        yield A("dve", lambda e, cur=cur, fbn=fbn: e.tensor_tensor(out=TT[q][:], in0=PS[fbn][:, 256:384], in1=cur[:, 256:384], op=ALU.add), reads=[fbn, curk], writes=[f"TT{q}"])
                    else:
                        nx, nxk = bufs[lev % 2]
                        yield A("pe", lambda e, cur=cur, fbn=fbn: e.matmul(PS[fbn][:, 128:384], lhsT=cur[:, 0:128], rhs=cur[:, 128:384], start=True, stop=True), reads=[curk], writes=[fbn])
                        yield A("pe", lambda e, cur=cur, fbn=fbn: e.matmul(PS[fbn][:, 0:128], lhsT=cur[:, 128:256], rhs=cur[:, 0:128], start=True, stop=True), reads=[curk], writes=[fbn])
                        yield A("act", lambda e, nx=nx, fbn=fbn: e.copy(out=nx[:, 0:256], in_=PS[fbn][:, 0:256]), reads=[fbn], writes=[nxk])
                        yield A("dve", lambda e, nx=nx, cur=cur, fbn=fbn: e.tensor_tensor(out=nx[:, 256:384], in0=PS[fbn][:, 256:384], in1=cur[:, 256:384], op=ALU.add), reads=[fbn, curk], writes=[nxk])
                        cur, curk = nx, nxk

            def chain_stage_w(p, c, ui):
                q = p % 4
                cn = nxt("c", 2)
                A("pe", lambda e: e.matmul(PS[cn][:, 0:128], lhsT=AR[q][:, c, 0:128], rhs=Sbf[p][:], start=True, stop=False), reads=[f"AR{q}", f"Sbf_{p}"], writes=[cn])
                A("pe", lambda e: e.matmul(PS[cn][:, 0:128], lhsT=Lt[ui][:, 448:576], rhs=TOK[q][:, c, 256:384], start=False, stop=True), reads=[f"Lt{ui}", f"TOK{q}"], writes=[cn])
                A("act", lambda e: e.copy(out=Wb[q][:], in_=PS[cn][:, 0:128]), reads=[cn], writes=[f"Wb{q}"])

            def chain_stage_u(p, c):
                q = p % 4
                cn = nxt("c", 2)
                A("pe", lambda e: e.matmul(PS[cn][:, 0:128], lhsT=TT[q][:], rhs=Wb[q][:], start=True, stop=True), reads=[f"TT{q}", f"Wb{q}"], writes=[cn])
                A("dve", lambda e: e.tensor_copy(out=Ub[q][:], in_=PS[cn][:, 0:128]), reads=[cn], writes=[f"Ub{q}"])

            def chain_stage_y(p, c, ui):
                q = p % 4
                cn = nxt("c", 2)
                A("pe", lambda e: e.matmul(PS[cn][:, 0:CH], lhsT=Sbf[p][:], rhs=AR[q][:, c, 128:192], start=True, stop=False), reads=[f"Sbf_{p}", f"AR{q}"], writes=[cn])
                A("pe", lambda e: e.matmul(PS[cn][:, 0:CH], lhsT=Ub[q][:], rhs=Lt[ui][:, 384:448], start=False, stop=False), reads=[f"Ub{q}", f"Lt{ui}"], writes=[cn])
                A("pe", lambda e: e.matmul(PS[cn][:, 0:CH], lhsT=TOK[q][:, c, 256:384], rhs=Lt[ui][:, 576:640], start=False, stop=True), reads=[f"TOK{q}", f"Lt{ui}"], writes=[cn])
                A("act", lambda e: e.copy(out=YS[q][:, c * CH:(c + 1) * CH], in_=PS[cn][:, 0:CH]), reads=[cn], writes=[f"YS{q}"])

            def chain_stage_s(p, c):
                q = p % 4
                cn = nxt("c", 2)
                si = p % 2
                A("pe", lambda e: e.matmul(PS[cn][:, 0:128], lhsT=TOK[q][:, c, 0:128], rhs=Ub[q][:], start=True, stop=False), reads=[f"TOK{q}", f"Ub{q}"], writes=[cn])
                A("pe", lambda e: e.matmul(PS[cn][:, 0:128], lhsT=TOK[q][:, c, 128:256], rhs=TOK[q][:, c, 256:384], start=False, stop=True), reads=[f"TOK{q}"], writes=[cn])
                A("dve", lambda e: e.tensor_tensor(out=stmp[si][:], in0=PS[cn][:, 0:128], in1=S32[p][:], op=ALU.add), reads=[cn, f"S32_{p}"], writes=[f"stmp{si}"])
                A("act", lambda e: e.activation(out=S32[p][:], in_=stmp[si][:], func=AF.Copy, scale=GC[q][:, c:c + 1]), reads=[f"stmp{si}", f"GC{q}"], writes=[f"S32_{p}"])
                A("pool", lambda e: e.tensor_copy(out=Sbf[p][:], in_=S32[p][:]), reads=[f"S32_{p}"], writes=[f"Sbf_{p}"])

            def rwkv_finish(p, col0, ncol):
                q = p % 4
                i = q % NR
                K = lambda n: [f"{n}{i}"]
                pv = lambda j: pvec[:, j, p:p + 1]
                N = slice(0, ncol)
                yield A("act", lambda e: e.activation(out=e_t[i][:, N], in_=YS[q][:, N], func=AF.Square), reads=[f"YS{q}"], writes=K("e_t"))
                pm = PBQ[q]
                yield A("pe", lambda e: e.matmul(PS[pm][:, N], lhsT=onesbd[:], rhs=YS[q][:, N], start=True, stop=True), reads=["onesbd", f"YS{q}"], writes=[pm])
                yield A("act", lambda e: e.activation(out=e_kk0[i][:, N], in_=PS[pm][:, N], func=AF.Copy, scale=1.0 / 64), reads=[pm], writes=K("e_kk0"))
                pq = PBQ[q]
                yield A("pe", lambda e: e.matmul(PS[pq][:, N], lhsT=onesbd[:], rhs=e_t[i][:, N], start=True, stop=True), reads=["onesbd"] + K("e_t"), writes=[pq])
                yield A("pool", lambda e: e.tensor_tensor(out=e_kk[i][:, N], in0=e_kk0[i][:, N], in1=e_kk0[i][:, N], op=ALU.mult), reads=K("e_kk0"), writes=K("e_kk"))
                yield A("dve", lambda e: e.scalar_tensor_tensor(out=e_t[i][:, N], in0=PS[pq][:, N], scalar=1.0 / 64, in1=e_kk[i][:, N], op0=ALU.mult, op1=ALU.subtract), reads=[pq] + K("e_kk"), writes=K("e_t"))
                yield rsqrt(e_t[i][:, N], e_t[i][:, N], 1, K("e_t"), K("e_t"))
                yield A("pool", lambda e: e.tensor_tensor(out=e_km[i][:, N], in0=YS[q][:, N], in1=e_kk0[i][:, N], op=ALU.subtract), reads=[f"YS{q}"] + K("e_kk0"), writes=K("e_km"))
                yield A("dve", lambda e: e.tensor_tensor(out=e_km[i][:, N], in0=e_km[i][:, N], in1=e_t[i][:, N], op=ALU.mult), reads=K("e_km") + K("e_t"), writes=K("e_km"))
                yield A("dve", lambda e: e.tensor_scalar(out=e_km[i][:, N], in0=e_km[i][:, N], scalar1=pv(5), scalar2=pv(6), op0=ALU.mult, op1=ALU.add), reads=K("e_km") + ["pvec"], writes=K("e_km"))
                yield A("pool", lambda e: e.tensor_tensor(out=e_km[i][:, N], in0=e_km[i][:, N], in1=BON[q][:, N], op=ALU.add), reads=K("e_km") + [f"BON{q}"], writes=K("e_km"))
                yield A("pool", lambda e: e.tensor_tensor(out=catT[:, 8 + p, col0:col0 + ncol], in0=e_km[i][:, N], in1=GG[q][:, N], op=ALU.mult), reads=K("e_km") + [f"GG{q}"], writes=["catT"])

            def run_il(gens):
                gens = list(gens)
                while gens:
                    for g_ in list(gens):
                        try:
                            next(g_)
                        except StopIteration:
                            gens.remove(g_)

            def rwkv_group(g, ncolt, full, valid):
                nsubs = (ncolt + TS - 1) // TS
                for sub in range(nsubs):
                    col0 = sub * TS
                    ncol = min(TS, ncolt - col0)
                    nch = ncol // CH
                    v = max(0, min(ncol, valid - col0))
                    run_il([pair_prep(4 * g + q, col0, ncol, nch, full, v) for q in range(4)])
                    for c in range(nch):
                        run_il([unit_local(4 * g + q, c, full) for q in range(4)])
                        for q in range(4):
                            chain_stage_w(4 * g + q, c, q)
                        for q in range(4):
                            chain_stage_u(4 * g + q, c)
                        if full:
                            for q in range(4):
                                chain_stage_y(4 * g + q, c, q)
                        for q in range(4):
                            chain_stage_s(4 * g + q, c)
                    if full:
                        run_il([rwkv_finish(4 * g + q, col0, ncol) for q in range(4)])

            def apply_flag_state():
                for p in range(8):
                    A("dve", lambda e, p=p: e.tensor_scalar(out=S32[p][:], in0=S32[p][:], scalar1=flag[:, 0:1], scalar2=None, op0=ALU.mult), reads=[f"S32_{p}", "flag"], writes=[f"S32_{p}"])
                    A("pool", lambda e, p=p: e.tensor_copy(out=Sbf[p][:], in_=S32[p][:]), reads=[f"S32_{p}"], writes=[f"Sbf_{p}"])
                A("dve", lambda e: e.tensor_scalar(out=carry[:], in0=carry[:], scalar1=flag[:, 0:1], scalar2=None, op0=ALU.mult), reads=["carry", "flag"], writes=["carry"])
                A("dve", lambda e: e.tensor_scalar(out=uhist[:].rearrange("p a b -> p (a b)"), in0=uhist[:].rearrange("p a b -> p (a b)"), scalar1=flag[:, 0:1], scalar2=None, op0=ALU.mult),
                  reads=["uhist", "flag"], writes=["uhist"])

            def out_conv(dst):
                for ci in range(8):
                    pn = nxt("pj", 2)
                    A("pe", lambda e, ci=ci, pn=pn: e.transpose(out=PS[pn][0:30, 0:128], in_=uhist[:, ci, :], identity=ident[:]), reads=["uhist", "ident"], writes=[pn])
                    A("act", lambda e, ci=ci, pn=pn: e.copy(out=cT8[:, ci * 128:(ci + 1) * 128], in_=PS[pn][0:30, 0:128]), reads=[pn], writes=["cT8"])
                A("sp", lambda e: e.dma_start(out=dst, in_=cT8[:]), reads=["cT8"], semkey="out")

            def out_wkv(dst):
                for p in range(8):
                    A("sp", lambda e, p=p: e.dma_start(out=dst[p, 0:64, :], in_=S32[p][0:64, 0:64]), reads=[f"S32_{p}"], semkey="out")
                    A("sp", lambda e, p=p: e.dma_start(out=dst[p, 64:128, :], in_=S32[p][64:128, 64:128]), reads=[f"S32_{p}"], semkey="out")

            def layer_norm(buf, bufk, gb, gbk):
                for q in range(4):
                    A("dve", lambda e, q=q: e.bn_stats(out=bst[:, q, :], in_=buf[:, q * 512:(q + 1) * 512]), reads=[bufk], writes=["bst"])
                A("dve", lambda e: e.bn_aggr(out=mv[:], in_=bst[:].rearrange("p a b -> p (a b)")), reads=["bst"], writes=["mv"])
                rsqrt(rstd[:], mv[:, 1:2], 0, ["mv"], ["rstd"])
                A("dve", lambda e: e.tensor_scalar(out=buf[:], in0=buf[:], scalar1=mv[:, 0:1], scalar2=rstd[:, 0:1], op0=ALU.subtract, op1=ALU.mult), reads=[bufk, "mv", "rstd"], writes=[bufk])
                A("pool", lambda e: e.tensor_tensor(out=buf[:], in0=buf[:], in1=gb[:, 0, :], op=ALU.mult), reads=[bufk, gbk], writes=[bufk])
                A("pool", lambda e: e.tensor_tensor(out=buf[:], in0=buf[:], in1=gb[:, 1, :], op=ALU.add), reads=[bufk, gbk], writes=[bufk])

            def front_sub(src_ap, rows, col0, ncolm, sidx):
                done_subs.append(sidx)
                load_x(src_ap, rows)
                if ncolm < 128:
                    pass
                for piece in range(16):
                    wi = wst_rot[0]; wst_rot[0] ^= 1
                    A("sp", lambda e, wi=wi, piece=piece: e.dma_start(out=wst[wi][:], in_=w_out_bf[:, piece * 128:(piece + 1) * 128].rearrange("(k p) n -> p k n", p=128)),
                      reads=["w_out_bf"], writes=[f"wst{wi}"], semkey=f"wst{wi}")
                    pn = nxt("pj", 2)
                    for kc in range(16):
                        A("pe", lambda e, kc=kc, pn=pn, wi=wi: e.matmul(PS[pn][0:ncolm, 0:128], lhsT=catT[:, kc, col0:col0 + ncolm], rhs=wst[wi][:, kc, :], start=(kc == 0), stop=(kc == 15)),
                          reads=["catT", f"wst{wi}"], writes=[pn])
                    A("dve", lambda e, pn=pn, piece=piece: e.scalar_tensor_tensor(out=xt[0:ncolm, piece * 128:(piece + 1) * 128], in0=xt[0:ncolm, piece * 128:(piece + 1) * 128], scalar=ALPHA,
                                                                                  in1=PS[pn][0:ncolm, 0:128], op0=ALU.mult, op1=ALU.add), reads=[pn, "xt"], writes=["xt"])
                layer_norm(xt, "xt", lnv, "lnv")
                A("sp", lambda e: e.dma_start(out=x1s[sidx * 128:(sidx + 1) * 128, :], in_=xt[:]), reads=["xt"], writes=["x1s"], semkey="x1s")
                if stage < 3:
                    return
                pl = nxt("m", 2)
                for g in range(4):
                    pn = nxt("pj", 2)
                    bi = g % 2
                    for j in range(4):
                        kc = 4 * g + j
                        A("pe", lambda e, pn=pn, j=j, kc=kc: e.transpose(out=PS[pn][:, j * 128:(j + 1) * 128], in_=xt[:, kc * 128:(kc + 1) * 128], identity=ident[:]), reads=["xt", "ident"], writes=[pn])
                    if g % 2 == 0:
                        A("act", lambda e, pn=pn, bi=bi: e.copy(out=x1Tb[bi][:], in_=PS[pn].rearrange("p (j t) -> p j t", j=4)), reads=[pn], writes=["x1Tb0"])
                    else:
                        A("dve", lambda e, pn=pn, bi=bi: e.tensor_copy(out=x1Tb[bi][:], in_=PS[pn].rearrange("p (j t) -> p j t", j=4)), reads=[pn], writes=["x1Tb0"])
                    for j in range(4):
                        kc = 4 * g + j
                        A("pe", lambda e, kc=kc, j=j, bi=bi: e.matmul(PS[pl][:, 0:NEXP], lhsT=x1Tb[bi][:, j, :], rhs=rw[:, kc, :], start=(kc == 0), stop=(kc == 15)), reads=["x1Tb0", "rw"], writes=[pl])
                A("dve", lambda e: e.tensor_tensor(out=lg[:], in0=PS[pl][:, 0:NEXP], in1=rb[:], op=ALU.add), reads=[pl, "rb"], writes=["lg"])
                A("dve", lambda e: e.max(out=m8[:], in_=lg[:]), reads=["lg"], writes=["m8"])
                A("dve", lambda e: e.tensor_scalar(out=negm[:], in0=m8[:, 0:1], scalar1=-1.0, scalar2=None, op0=ALU.mult), reads=["m8"], writes=["negm"])
                A("act", lambda e: e.activation(out=e4[:], in_=m8[:, 0:4], func=AF.Exp, bias=negm[:, 0:1], scale=1.0), reads=["m8", "negm"], writes=["e4"])
                A("dve", lambda e: e.tensor_reduce(out=s4[:], in_=e4[:], axis=mybir.AxisListType.X, op=ALU.add), reads=["e4"], writes=["s4"])
                A("dve", lambda e: e.reciprocal(out=s4[:], in_=s4[:]), reads=["s4"], writes=["s4"])
                A("dve", lambda e: e.tensor_scalar(out=g4[:], in0=e4[:], scalar1=s4[:, 0:1], scalar2=None, op0=ALU.mult), reads=["e4", "s4"], writes=["g4"])
                for k in range(4):
                    A("dve", lambda e, k=k: e.tensor_scalar(out=oh[:, k, :], in0=lg[:], scalar1=m8[:, k:k + 1], scalar2=None, op0=ALU.is_equal), reads=["lg", "m8"], writes=["oh"])
                A("dve", lambda e: e.tensor_tensor(out=msk[:], in0=oh[:, 0, :], in1=oh[:, 1, :], op=ALU.add), reads=["oh"], writes=["msk"])
                A("dve", lambda e: e.tensor_tensor(out=msk[:], in0=msk[:], in1=oh[:, 2, :], op=ALU.add), reads=["oh", "msk"], writes=["msk"])
                A("dve", lambda e: e.tensor_tensor(out=msk[:], in0=msk[:], in1=oh[:, 3, :], op=ALU.add), reads=["oh", "msk"], writes=["msk"])
                if ncolm < 128:
                    A("dve", lambda e: e.tensor_scalar(out=msk[:], in0=msk[:], scalar1=rvt[:, 0:1], scalar2=None, op0=ALU.mult), reads=["msk", "rvt"], writes=["msk"])
                A("pool", lambda e: e.tensor_copy(out=mskb[:], in_=msk[:]), reads=["msk"], writes=["mskb"])
                A("dve", lambda e: e.tensor_scalar(out=gd_all[:, sidx, :], in0=oh[:, 0, :], scalar1=g4[:, 0:1], scalar2=None, op0=ALU.mult), reads=["oh", "g4"], writes=["gd_all"])
                for k in range(1, 4):
                    A("dve", lambda e, k=k: e.scalar_tensor_tensor(out=gd_all[:, sidx, :], in0=oh[:, k, :], scalar=g4[:, k:k + 1], in1=gd_all[:, sidx, :], op0=ALU.mult, op1=ALU.add), reads=["oh", "g4", "gd_all"], writes=["gd_all"])
                pc = nxt("m", 2)
                A("pe", lambda e: e.matmul(PS[pc][:, 0:NEXP], lhsT=tri[:], rhs=mskb[:], start=True, stop=True), reads=["tri", "mskb"], writes=[pc])
                A("pe", lambda e: e.matmul(PS[pc][:, NEXP:2 * NEXP], lhsT=onesb[:], rhs=mskb[:], start=True, stop=True), reads=["onesb", "mskb"], writes=[pc])
                A("dve", lambda e: e.tensor_tensor(out=posf[:], in0=PS[pc][:, 0:NEXP], in1=cntbase[:], op=ALU.add), reads=[pc, "cntbase"], writes=["posf"])
                A("dve", lambda e: e.tensor_tensor(out=posf[:], in0=posf[:], in1=ecap[:], op=ALU.add), reads=["posf", "ecap"], writes=["posf"])
                A("dve", lambda e: e.tensor_tensor(out=cntbase[:], in0=PS[pc][:, NEXP:2 * NEXP], in1=cntbase[:], op=ALU.add), reads=[pc, "cntbase"], writes=["cntbase"])
                for k in range(4):
                    A("dve", lambda e, k=k: e.tensor_tensor(out=junk[:], in0=oh[:, k, :], in1=posf[:], op=ALU.mult), reads=["oh", "posf"], writes=["junk"])
                    A("dve", lambda e, k=k: e.tensor_reduce(out=pk[:, k:k + 1], in_=junk[:], axis=mybir.AxisListType.X, op=ALU.add), reads=["junk"], writes=["pk"])
                A("dve", lambda e: e.tensor_scalar(out=pk[:], in0=pk[:], scalar1=float(NROWS - 1), scalar2=0.0, op0=ALU.min, op1=ALU.max), reads=["pk"], writes=["pk"])
                A("dve", lambda e: e.tensor_copy(out=idx_all[:, sidx, :], in_=pk[:]), reads=["pk"], writes=["idx_all"])
                if ncolm < 128:
                    A("dve", lambda e: e.tensor_scalar(out=pk[:], in0=pk[:], scalar1=rvt[:, 1:2], scalar2=rvt[:, 0:1], op0=ALU.subtract, op1=ALU.mult), reads=["pk", "rvt"], writes=["pk"])
                    A("dve", lambda e: e.tensor_scalar(out=pk[:], in0=pk[:], scalar1=rvt[:, 1:2], scalar2=None, op0=ALU.add), reads=["pk", "rvt"], writes=["pk"])
                A("dve", lambda e: e.tensor_copy(out=idx_sc[:], in_=pk[:]), reads=["pk"], writes=["idx_sc"])
                for k in range(4):
                    xi = k % 2
                    if xi == 0:
                        A("act", lambda e, xi=xi: e.copy(out=xrow[xi][:, 0:D], in_=xt[:]), reads=["xt"], writes=[f"xrow{xi}"])
                    else:
                        A("pool", lambda e, xi=xi: e.tensor_copy(out=xrow[xi][:, 0:D], in_=xt[:]), reads=["xt"], writes=[f"xrow{xi}"])
                    A("dve", lambda e, k=k, xi=xi: e.tensor_copy(out=xrow[xi][:, D:D + 1], in_=g4[:, k:k + 1]), reads=["g4"], writes=[f"xrow{xi}"])
                    A("dve", lambda e, xi=xi: e.tensor_copy(out=negm[:], in_=xrow[xi][:, D:D + 1]), reads=[f"xrow{xi}"], writes=["negm"])
                    A("dve", lambda e, k=k, xi=xi: e.tensor_tensor(out=xrow[xi][:, D + 1:D + 2], in0=g4[:, k:k + 1], in1=negm[:], op=ALU.subtract), reads=["g4", "negm"], writes=[f"xrow{xi}"])
                    A("gq", lambda e, k=k, xi=xi: e.indirect_dma_start(out=xsorted, out_offset=bass.IndirectOffsetOnAxis(ap=idx_sc[:, k:k + 1], axis=0), in_=xrow[xi][:], in_offset=None),
                      reads=[f"xrow{xi}", "idx_sc"], writes=["xsorted"], semkey=f"sc{xi}")

            def z_slot_g(g):
                return lambda ct: ((ct - 16) // 8) * 4 + (ct - 16) % 8 - 4 * g

            def rwkv_tile(src, row0, ncol, nrows, full, valid):
                nsub = (ncol + 127) // 128
                for s in range(nsub):
                    r = min(128, nrows - s * 128)
                    load_x(src[row0 + s * 128: row0 + s * 128 + r, :], r)
                    transpose_x(s * 128, min(128, ncol - s * 128))
                zc_l = z_consume(ncol, valid, zl, "zl", lambda ct: ct - 40)
                if full:
                    proj_cols([40, 41], ncol, zc_l)
                    proj_cols([42], ncol, zc_l)
                else:
                    proj_cols([40], ncol, zc_l)
                lora_acts(ncol, full)
                for g in range(2):
                    zc = z_consume(ncol, valid, zg, "zg", z_slot_g(g))
                    kinds = (0, 1, 2) if full else (1, 2)
                    for kind in kinds:
                        base = 16 + 8 * kind + 4 * g
                        proj_cols([base, base + 1], ncol, zc)
                        proj_cols([base + 2, base + 3], ncol, zc)
                    rwkv_group(g, ncol, full, valid)

            def do_conv(ncol, valid):
                load_uhist()
                glu_proj(ncol)
                conv_block(ncol)
                save_uhist(valid)

            n_pre = NTILES
            if os.environ.get("MK_NPRE") is not None:
                n_pre = int(os.environ["MK_NPRE"])
            n_own = NTILES
            if os.environ.get("MK_NOWN") is not None:
                n_own = int(os.environ["MK_NOWN"])
            for ti in range(n_pre):
                rwkv_tile(xp, ti * T, T, T, False, T)
                if ti == n_pre - 1:
                    glu_proj(T)
                    save_uhist(T)
            apply_flag_state()
            for ti in range(n_own):
                rwkv_tile(xo, ti * T, T, T, True, T)
                do_conv(T, T)
                if stage >= 2:
                    for s in range(2):
                        front_sub(xo[ti * T + s * 128: ti * T + (s + 1) * 128, :], 128, s * 128, 128, ti * 2 + s)
            A("sp", lambda e: e.dma_start(out=shift_o, in_=carry[:]), reads=["carry"], semkey="out")
            out_conv(conv_o)
            out_wkv(wkv_o)
            P.flush()
            A("sp", lambda e: e.dma_start(out=carry[:], in_=sshT_d), writes=["carry"], semkey="ld0")
            A("sp", lambda e: e.dma_start(out=cT8[:], in_=sconv_d), writes=["cT8"], semkey="ld0")
            for ci in range(8):
                pn = nxt("pj", 2)
                A("pe", lambda e, ci=ci, pn=pn: e.transpose(out=PS[pn][:, 0:30], in_=cT8[0:30, ci * 128:(ci + 1) * 128], identity=ident[0:30, 0:30]), reads=["cT8", "ident"], writes=[pn])
                A("act", lambda e, ci=ci, pn=pn: e.copy(out=uhist[:, ci, :], in_=PS[pn][:, 0:30]), reads=[pn], writes=["uhist"])
            for p in range(8):
                A("sp", lambda e, p=p: e.dma_start(out=stmp[p % 2][:, 0:64], in_=swkv_d[p]), writes=[f"stmp{p % 2}"], semkey=f"stl{p % 2}")
                def fbd(e, p=p):
                    in0 = fap(stmp[p % 2][:, 0:64], [[0, 2], [1, 64]])
                    in1 = fap(maskh[:], [[1, 2], [0, 64]])
                    return e.tensor_tensor(out=S32[p][:].rearrange("p (h v) -> p h v", h=2), in0=in0, in1=in1, op=ALU.mult)
                A("dve", fbd, reads=[f"stmp{p % 2}", "maskh"], writes=[f"S32_{p}"])
                A("pool", lambda e, p=p: e.tensor_copy(out=Sbf[p][:], in_=S32[p][:]), reads=[f"S32_{p}"], writes=[f"Sbf_{p}"])
            rwkv_tile(xs, 0, CH, 16, True, 16)
            do_conv(CH, 16)
            if stage >= 2:
                front_sub(xs[0:16, :], 16, 0, CH, 32)
            A("sp", lambda e: e.dma_start(out=shift_so, in_=carry[:]), reads=["carry"], semkey="out")
            out_conv(conv_so)
            out_wkv(wkv_so)
            P.flush()

        if stage >= 4:
            NB = CAP // 128
            HALF = CAP // 2
            with ExitStack() as pb:
                bgu = sbt(pb, "bgu", [128, 2, NEXP, 16])
                A("sp", lambda e: e.dma_start(out=bgu[:], in_=bgu_d), writes=["bgu"], semkey="ld0")
                xs_t = [sbt(pb, f"xs_t{i}", [128, XROW], BF16) for i in range(2)]
                xsT = sbt(pb, "xsT", [128, 16, CAP], BF16)
                gate_r = sbt(pb, "gate_r", [128, NB])
                hT = sbt(pb, "hT", [128, 16, CAP], BF16)
                wgu = [sbt(pb, f"wgu{i}", [128, 2, 16, 256], BF16) for i in range(2)]
                wdn = [sbt(pb, f"wdn{i}", [128, 16, 512], BF16) for i in range(2)]
                gcl = [sbt(pb, f"gcl{i}", [128, HALF]) for i in range(2)]
                sgm = [sbt(pb, f"sgm{i}", [128, HALF]) for i in range(2)]
                ucl = [sbt(pb, f"ucl{i}", [128, HALF]) for i in range(2)]
                yo = [sbt(pb, f"yo{i}", [128, 512]) for i in range(4)]
                stg = [sbt(pb, f"stg{i}", [128, 16, 256]) for i in range(2)]
                stg_rot = [0]

                def load_w(dst_ap, dst_key, src_ap, cast_eng):
                    si = stg_rot[0]; stg_rot[0] ^= 1
                    A("sp", lambda e: e.dma_start(out=stg[si][:], in_=src_ap.rearrange("(k p) n -> p k n", p=128)), writes=[f"stg{si}"], semkey=f"stg{si}")
                    if cast_eng == "act":
                        A("act", lambda e: e.copy(out=dst_ap, in_=stg[si][:]), reads=[f"stg{si}"], writes=[dst_key])
                    else:
                        A(cast_eng, lambda e: e.tensor_copy(out=dst_ap, in_=stg[si][:]), reads=[f"stg{si}"], writes=[dst_key])
                n_exp = NEXP
                if os.environ.get("MK_NEXP") is not None:
                    n_exp = int(os.environ["MK_NEXP"])
                for ex in range(n_exp):
                    for blk in range(NB):
                        xi = blk % 2
                        r0 = ex * CAP + blk * 128
                        A("sp", lambda e, xi=xi, r0=r0: e.dma_start(out=xs_t[xi][:], in_=xsorted[r0:r0 + 128, :]), reads=["xsorted"], writes=[f"xs_t{xi}"], semkey=f"xsl{xi}")
                        A("pool", lambda e, xi=xi, blk=blk: e.tensor_tensor(out=gate_r[:, blk:blk + 1], in0=xs_t[xi][:, D:D + 1], in1=xs_t[xi][:, D + 1:D + 2], op=ALU.add), reads=[f"xs_t{xi}"], writes=["gate_r"])
                        for g in range(4):
                            pt = nxt("m", 2)
                            ptb = PS[pt].bitcast(BF16)
                            for j in range(4):
                                kc = 4 * g + j
                                A("pe", lambda e, xi=xi, kc=kc, j=j, ptb=ptb: e.transpose(out=ptb[:, j * 128:(j + 1) * 128], in_=xs_t[xi][:, kc * 128:(kc + 1) * 128], identity=identb[:]),
                                  reads=[f"xs_t{xi}", "identb"], writes=[pt])
                            if g % 2 == 0:
                                A("act", lambda e, g=g, blk=blk, ptb=ptb: e.copy(out=xsT[:, 4 * g:4 * g + 4, blk * 128:(blk + 1) * 128], in_=ptb[:, 0:512].rearrange("p (j t) -> p j t", j=4)), reads=[pt], writes=["xsT"])
                            else:
                                A("dve", lambda e, g=g, blk=blk, ptb=ptb: e.tensor_copy(out=xsT[:, 4 * g:4 * g + 4, blk * 128:(blk + 1) * 128], in_=ptb[:, 0:512].rearrange("p (j t) -> p j t", j=4)), reads=[pt], writes=["xsT"])
                    for fg in range(8):
                        wi = fg % 2
                        load_w(wgu[wi][:, 0, :, :], f"wgu{wi}", w_gate[ex, :, fg * 256:(fg + 1) * 256], "act")
                        load_w(wgu[wi][:, 1, :, :], f"wgu{wi}", w_up[ex, :, fg * 256:(fg + 1) * 256], "act")
                        for fl in range(2):
                            ft = fg * 2 + fl
                            for hf in range(2):
                                pg_ = nxt("pj", 2)
                                pu_ = nxt("fb", 2)
                                rs = slice(hf * HALF, (hf + 1) * HALF)
                                for kc in range(16):
                                    A("pe", lambda e, kc=kc, pg_=pg_, wi=wi, fl=fl, rs=rs: e.matmul(PS[pg_][:, 0:HALF], lhsT=wgu[wi][:, 0, kc, fl * 128:(fl + 1) * 128], rhs=xsT[:, kc, rs], start=(kc == 0), stop=(kc == 15)),
                                      reads=[f"wgu{wi}", "xsT"], writes=[pg_])
                                for kc in range(16):
                                    A("pe", lambda e, kc=kc, pu_=pu_, wi=wi, fl=fl, rs=rs: e.matmul(PS[pu_][:, 0:HALF], lhsT=wgu[wi][:, 1, kc, fl * 128:(fl + 1) * 128], rhs=xsT[:, kc, rs], start=(kc == 0), stop=(kc == 15)),
                                      reads=[f"wgu{wi}", "xsT"], writes=[pu_])
                                bi = hf
                                A("dve", lambda e, pg_=pg_, bi=bi, ft=ft, ex=ex: e.tensor_scalar(out=gcl[bi][:], in0=PS[pg_][:, 0:HALF], scalar1=bgu[:, 0, ex, ft:ft + 1], scalar2=7.0, op0=ALU.add, op1=ALU.min),
                                  reads=[pg_, "bgu"], writes=[f"gcl{bi}"])
                                A("act", lambda e, bi=bi: e.activation(out=sgm[bi][:], in_=gcl[bi][:], func=AF.Sigmoid, scale=1.702), reads=[f"gcl{bi}"], writes=[f"sgm{bi}"])
                                A("dve", lambda e, pu_=pu_, bi=bi, ft=ft, ex=ex: e.tensor_scalar(out=ucl[bi][:], in0=PS[pu_][:, 0:HALF], scalar1=bgu[:, 1, ex, ft:ft + 1], scalar2=7.0, op0=ALU.add, op1=ALU.min),
                                  reads=[pu_, "bgu"], writes=[f"ucl{bi}"])
                                A("pool", lambda e, bi=bi: e.tensor_scalar(out=ucl[bi][:], in0=ucl[bi][:], scalar1=-7.0, scalar2=1.0, op0=ALU.max, op1=ALU.add), reads=[f"ucl{bi}"], writes=[f"ucl{bi}"])
                                A("pool", lambda e, bi=bi: e.tensor_tensor(out=gcl[bi][:], in0=gcl[bi][:], in1=sgm[bi][:], op=ALU.mult), reads=[f"gcl{bi}", f"sgm{bi}"], writes=[f"gcl{bi}"])
                                A("dve", lambda e, bi=bi, ft=ft, rs=rs: e.tensor_tensor(out=hT[:, ft, rs], in0=gcl[bi][:], in1=ucl[bi][:], op=ALU.mult), reads=[f"gcl{bi}", f"ucl{bi}"], writes=["hT"])
                    for ct in range(4):
                        wi = ct % 2
                        load_w(wdn[wi][:, :, 0:256], f"wdn{wi}", w_down[ex, :, ct * 512:ct * 512 + 256], "pool")
                        load_w(wdn[wi][:, :, 256:512], f"wdn{wi}", w_down[ex, :, ct * 512 + 256:(ct + 1) * 512], "act")
                        for blk in range(NB):
                            pd = nxt("c", 2)
                            for fc in range(16):
                                A("pe", lambda e, fc=fc, pd=pd, blk=blk, wi=wi: e.matmul(PS[pd][:], lhsT=hT[:, fc, blk * 128:(blk + 1) * 128], rhs=wdn[wi][:, fc, :], start=(fc == 0), stop=(fc == 15)),
                                  reads=["hT", f"wdn{wi}"], writes=[pd])
                            yi = int(nxt("yo", 4)[2:])
                            r0 = ex * CAP + blk * 128
                            if yi % 2 == 0:
                                A("act", lambda e, pd=pd, blk=blk, yi=yi: e.activation(out=yo[yi][:], in_=PS[pd][:], func=AF.Copy, scale=gate_r[:, blk:blk + 1]), reads=[pd, "gate_r"], writes=[f"yo{yi}"])
                            else:
                                A("dve", lambda e, pd=pd, blk=blk, yi=yi: e.tensor_scalar(out=yo[yi][:], in0=PS[pd][:], scalar1=gate_r[:, blk:blk + 1], scalar2=None, op0=ALU.mult), reads=[pd, "gate_r"], writes=[f"yo{yi}"])
                            A("sp", lambda e, yi=yi, r0=r0, ct=ct: e.dma_start(out=ysorted[r0:r0 + 128, ct * 512:(ct + 1) * 512], in_=yo[yi][:]), reads=[f"yo{yi}"], writes=["ysorted"], semkey=f"yst{yi}")
                P.flush()
            with ExitStack() as pc_:
                bdn = sbt(pc_, "bdn", [NEXP, D])
                A("sp", lambda e: e.dma_start(out=bdn[:], in_=bdn_d), writes=["bdn"], semkey="ld0")
                lnv2 = sbt(pc_, "lnv2", [128, 2, D])
                A("sp", lambda e: e.dma_start(out=lnv2[:, 0, :], in_=lnv_d[2:3, :].partition_broadcast(128)), writes=["lnv2"], semkey="ld0")
                A("sp", lambda e: e.dma_start(out=lnv2[:, 1, :], in_=lnv_d[3:4, :].partition_broadcast(128)), writes=["lnv2"], semkey="ld0")
                Gk = [sbt(pc_, f"Gk{i}", [128, D]) for i in range(8)]
                xr = [sbt(pc_, f"xr{i}", [128, D]) for i in range(2)]
                gTt = [sbt(pc_, f"gTt{i}", [NEXP, 128]) for i in range(2)]
                bst2 = sbt(pc_, "bst2", [128, 4, 6]); mv2 = sbt(pc_, "mv2", [128, 2]); rstd2 = sbt(pc_, "rstd2", [128, 1])
                for sidx in done_subs:
                    bi = sidx % 2
                    X, Xk = xr[bi], f"xr{bi}"
                    A("sp", lambda e, X=X, sidx=sidx: e.dma_start(out=X[:], in_=x1s[sidx * 128:(sidx + 1) * 128, :]), reads=["x1s"], writes=[Xk], semkey=f"xr{bi}")
                    for k in range(4):
                        gi = bi * 4 + k
                        A("gq", lambda e, gi=gi, k=k, sidx=sidx: e.indirect_dma_start(out=Gk[gi][:], out_offset=None, in_=ysorted,
                                                                                      in_offset=bass.IndirectOffsetOnAxis(ap=idx_all[:, sidx, k:k + 1], axis=0)),
                          reads=["ysorted", "idx_all"], writes=[f"Gk{gi}"], semkey=f"gk{gi}")
                    pg = nxt("pj", 2)
                    A("pe", lambda e, pg=pg, sidx=sidx: e.transpose(out=PS[pg][0:NEXP, 0:128], in_=gd_all[:, sidx, :], identity=ident[:]), reads=["gd_all", "ident"], writes=[pg])
                    A("act", lambda e, pg=pg, bi=bi: e.copy(out=gTt[bi][:], in_=PS[pg][0:NEXP, 0:128]), reads=[pg], writes=[f"gTt{bi}"])
                    A("dve", lambda e, X=X, bi=bi: e.scalar_tensor_tensor(out=X[:], in0=X[:], scalar=ALPHA, in1=Gk[bi * 4][:], op0=ALU.mult, op1=ALU.add), reads=[Xk, f"Gk{bi * 4}"], writes=[Xk])
                    A("pool", lambda e, bi=bi: e.tensor_tensor(out=Gk[bi * 4 + 1][:], in0=Gk[bi * 4 + 1][:], in1=Gk[bi * 4 + 2][:], op=ALU.add), reads=[f"Gk{bi * 4 + 1}", f"Gk{bi * 4 + 2}"], writes=[f"Gk{bi * 4 + 1}"])
                    A("pool", lambda e, bi=bi: e.tensor_tensor(out=Gk[bi * 4 + 1][:], in0=Gk[bi * 4 + 1][:], in1=Gk[bi * 4 + 3][:], op=ALU.add), reads=[f"Gk{bi * 4 + 1}", f"Gk{bi * 4 + 3}"], writes=[f"Gk{bi * 4 + 1}"])
                    A("dve", lambda e, X=X, bi=bi: e.tensor_tensor(out=X[:], in0=X[:], in1=Gk[bi * 4 + 1][:], op=ALU.add), reads=[Xk, f"Gk{bi * 4 + 1}"], writes=[Xk])
                    for ct in range(4):
                        pb_ = nxt("fb", 2)
                        A("pe", lambda e, pb_=pb_, ct=ct, bi=bi: e.matmul(PS[pb_][:], lhsT=gTt[bi][:], rhs=bdn[:, ct * 512:(ct + 1) * 512], start=True, stop=True), reads=[f"gTt{bi}", "bdn"], writes=[pb_])
                        A("dve", lambda e, pb_=pb_, ct=ct, X=X: e.tensor_tensor(out=X[:, ct * 512:(ct + 1) * 512], in0=X[:, ct * 512:(ct + 1) * 512], in1=PS[pb_][:], op=ALU.add), reads=[pb_, Xk], writes=[Xk])
                    for q in range(4):
                        A("dve", lambda e, q=q, X=X: e.bn_stats(out=bst2[:, q, :], in_=X[:, q * 512:(q + 1) * 512]), reads=[Xk], writes=["bst2"])
                    A("dve", lambda e: e.bn_aggr(out=mv2[:], in_=bst2[:].rearrange("p a b -> p (a b)")), reads=["bst2"], writes=["mv2"])
                    rsqrt(rstd2[:], mv2[:, 1:2], 0, ["mv2"], ["rstd2"])
                    A("dve", lambda e, X=X: e.tensor_scalar(out=X[:], in0=X[:], scalar1=mv2[:, 0:1], scalar2=rstd2[:, 0:1], op0=ALU.subtract, op1=ALU.mult), reads=[Xk, "mv2", "rstd2"], writes=[Xk])
                    A("pool", lambda e, X=X: e.tensor_tensor(out=X[:], in0=X[:], in1=lnv2[:, 0, :], op=ALU.mult), reads=[Xk, "lnv2"], writes=[Xk])
                    A("pool", lambda e, X=X: e.tensor_tensor(out=X[:], in0=X[:], in1=lnv2[:, 1, :], op=ALU.add), reads=[Xk, "lnv2"], writes=[Xk])
                    A("sp", lambda e, X=X, sidx=sidx: e.dma_start(out=y_own[sidx * 128:(sidx + 1) * 128, :], in_=X[:]), reads=[Xk], semkey="out")
                P.flush()
        P.finish()
    return nc


def _consts():
    p = np.arange(128)
    h, t = p // 64, p % 64
    same = (h[:, None] == h[None, :])
    c = {}
    c["ident"] = np.eye(128, dtype=np.float32)
    c["maskh"] = (h[:, None] == np.arange(2)[None, :]).astype(np.float32)
    nt = same & (t[:, None] > t[None, :])
    n_ = same & (t[:, None] < t[None, :])
    c["m1"] = np.concatenate([nt, n_], axis=1).astype(np.float32)
    incl = (t[:, None] <= np.arange(64)[None, :])
    c["m2"] = np.concatenate([incl, n_, incl], axis=1).astype(np.float32)
    c["onesbd"] = same.astype(np.float32)
    c["tri"] = (p[:, None] < p[None, :]).astype(np.float32)
    c["rvt"] = np.stack([(p < 16).astype(np.float32), (NROWS + p).astype(np.float32)], axis=1)
    c["ecap"] = np.broadcast_to((np.arange(NEXP) * CAP).astype(np.float32)[None, :], (128, NEXP)).copy()
    return c


def _colmajor(v, ntile):
    out = np.zeros((ntile * 128,), np.float32)
    out[:v.shape[0]] = v
    return np.ascontiguousarray(out.reshape(ntile, 128).T)


def _shared_inputs(inp, stage):
    g = lambda k: np.asarray(inp[k], dtype=np.float32)[0]
    sh = dict(_consts())
    sh["w_in"] = g("w_in")
    sh["binT"] = _colmajor(g("b_in"), 43)
    sh["muT"] = _colmajor(g("mu_shift"), 27)
    sh["cwT"] = np.ascontiguousarray(g("conv_w").reshape(31, 8, 128).transpose(2, 1, 0))
    sh["cvec"] = np.ascontiguousarray(np.stack([_colmajor(g("conv_b"), 8), _colmajor(g("conv_ln_g"), 8), _colmajor(g("conv_ln_b"), 8)], axis=1))
    pv = [g("rwkv_w0"), g("rwkv_a0"), g("rwkv_k_k"), g("rwkv_k_a"), g("rwkv_r_k").reshape(-1), g("rwkv_ln_g"), g("rwkv_ln_b")]
    sh["pvec"] = np.ascontiguousarray(np.stack([_colmajor(v, 8) for v in pv], axis=1))
    sh["w2"] = g("rwkv_w2"); sh["a2"] = g("rwkv_a2"); sh["g2"] = g("rwkv_g2")
    sh["w_out"] = g("w_out")
    sh["lnv"] = np.ascontiguousarray(np.stack([g("ln1_g"), g("ln1_b"), g("ln2_g"), g("ln2_b")], axis=0))
    sh["rw"] = g("router_w"); sh["rb"] = g("router_b").reshape(1, NEXP)
    if stage >= 4:
        sh["w_gate"] = g("w_gate"); sh["w_up"] = g("w_up"); sh["w_down"] = g("w_down")
        bg = g("b_gate").reshape(NEXP, 16, 128).transpose(2, 0, 1)
        bu = g("b_up").reshape(NEXP, 16, 128).transpose(2, 0, 1)
        sh["bgu"] = np.ascontiguousarray(np.stack([bg, bu], axis=1))
        sh["bdn"] = g("b_down")
    return sh


def _core_inputs(c, inp, sh):
    b, half = c // 2, c % 2
    xpr = np.asarray(inp["x_prompt"], dtype=np.float32)
    m = dict(sh)
    m["xo"] = np.ascontiguousarray(xpr[b, half * NOWN:(half + 1) * NOWN])
    m["xp"] = np.ascontiguousarray(xpr[b, 0:NOWN])
    m["xs"] = np.ascontiguousarray(np.asarray(inp["x_sample"], dtype=np.float32)[c])
    m["flag"] = np.full((128, 1), float(half), np.float32)
    m["sconv"] = np.ascontiguousarray(np.asarray(inp["state_conv"], dtype=np.float32)[0, c])
    m["sshT"] = _colmajor(np.asarray(inp["state_shift"], dtype=np.float32)[0, c, 0], 27)
    sw = np.asarray(inp["state_wkv"], dtype=np.float32)[0, c]
    m["swkv"] = np.ascontiguousarray(sw.reshape(8, 2, 64, 64).transpose(0, 1, 3, 2).reshape(8, 128, 64))
    return m


_NC_CACHE = {}


def run_cores(inp, stage=99):
    if stage not in _NC_CACHE:
        _NC_CACHE[stage] = build_nc(stage)
    nc = _NC_CACHE[stage]
    sh = _shared_inputs(inp, stage)
    in_maps = [_core_inputs(c, inp, sh) for c in range(8)]
    res = run_bass_kernel_spmd(nc, in_maps, core_ids=list(range(8)))
    return res.results


def assemble(rs):
    y_p = np.zeros((4, 8192, D), np.float32); y_s = np.zeros((8, 16, D), np.float32)
    conv_p = np.zeros((1, 4, 30, C_CONV), np.float32); shift_p = np.zeros((1, 4, 1, NSH), np.float32); wkv_p = np.zeros((1, 4, 16, 64, 64), np.float32)
    conv_s = np.zeros((1, 8, 30, C_CONV), np.float32); shift_s = np.zeros((1, 8, 1, NSH), np.float32); wkv_s = np.zeros((1, 8, 16, 64, 64), np.float32)
    unshift = lambda a: np.ascontiguousarray(a.T).reshape(-1)[:NSH]
    unwkv = lambda a: a.reshape(8, 2, 64, 64).transpose(0, 1, 3, 2).reshape(16, 64, 64)
    for c in range(8):
        r = rs[c]
        b, half = c // 2, c % 2
        y_p[b, half * NOWN:(half + 1) * NOWN] = r["y_own"][0:NOWN]
        y_s[c] = r["y_own"][NOWN:NOWN + 16]
        if half == 1:
            conv_p[0, b] = r["conv_o"]; shift_p[0, b, 0] = unshift(r["shift_o"]); wkv_p[0, b] = unwkv(r["wkv_o"])
        conv_s[0, c] = r["conv_so"]; shift_s[0, c, 0] = unshift(r["shift_so"]); wkv_s[0, c] = unwkv(r["wkv_so"])
    return (y_p, y_s, conv_p, shift_p, wkv_p, conv_s, shift_s, wkv_s)


def kernel(**inputs):
    return assemble(run_cores(inputs, 99))
```

```python
import os
import numpy as np
from contextlib import ExitStack
import concourse.bass as bass
import concourse.mybir as mybir
from concourse.bass_utils import run_bass_kernel_spmd

F32 = mybir.dt.float32
BF16 = mybir.dt.bfloat16
I32 = mybir.dt.int32
AF = mybir.ActivationFunctionType
ALU = mybir.AluOpType

D = 2048
C_CONV = 1024
NSH = 3360
P_IN = 5408
NEXP = 32
ALPHA = 2.0 ** 0.25
LN_EPS = 1e-5
GN_EPS = 64e-5
DEC = 0.6065306597126334
CH = 64
T = 256
TS = 128
NOWN = 4096
NTILES = NOWN // T
CAP = 768
NROWS = NEXP * CAP
NSUB = 33
XROW = 2050


class _Op:
    __slots__ = ("eng", "fn", "reads", "writes", "semkey", "idx", "waits", "inc", "tick", "is_dma")


class Prog:
    ISSUE = {"pe": "pe", "act": "act", "dve": "dve", "pool": "pool", "sp": "sp", "aq": "act", "gq": "pool"}
    ENG = {"pe": "tensor", "act": "scalar", "dve": "vector", "pool": "gpsimd", "sp": "sync"}

    def __init__(self, nc, stack):
        self.nc = nc
        self.stack = stack
        self.ops = []
        self.last_w = {}
        self.readers = {}
        self.cnt = {}
        self.waited = {}
        self.sems = {}
        self.pending_barrier = None
        self.barrier_done = {}
        self.n_emitted = 0

    def add(self, eng, fn, reads=(), writes=(), semkey=None):
        op = _Op()
        op.eng = eng
        op.fn = fn
        op.reads = tuple(reads)
        op.writes = tuple(writes)
        op.is_dma = eng in ("sp", "aq", "gq")
        op.semkey = semkey if semkey is not None else (("dma_" + eng) if op.is_dma else None)
        op.waits = {}
        op.inc = False
        op.tick = 0
        op.idx = len(self.ops)
        self.ops.append(op)
        return op

    def _chan(self, op):
        return op.semkey if op.is_dma else op.eng

    def _sem(self, c):
        if c not in self.sems:
            self.sems[c] = self.stack.enter_context(self.nc.semaphore("s_" + str(c)))
        return self.sems[c]

    def flush(self):
        nc = self.nc
        ops = self.ops
        if not ops:
            return
        n = len(ops)
        deps = [None] * n
        last_w, readers = {}, {}
        for op in ops:
            d = set()
            for k in op.reads:
                if k in last_w:
                    d.add(last_w[k])
            for k in op.writes:
                if k in last_w:
                    d.add(last_w[k])
                for r in readers.get(k, ()):
                    d.add(r)
            d.discard(op.idx)
            best = {}
            keep = set()
            for di in d:
                dop = ops[di]
                if dop.is_dma:
                    keep.add(di)
                else:
                    if dop.eng not in best or di > best[dop.eng]:
                        best[dop.eng] = di
            keep.update(best.values())
            deps[op.idx] = keep
            for k in op.reads:
                readers.setdefault(k, []).append(op.idx)
            for k in op.writes:
                last_w[k] = op.idx
                readers[k] = []

        def skip(dop, op):
            return (not dop.is_dma) and (not op.is_dma) and dop.eng == "pe" and op.eng == "pe"

        needed = [False] * n
        for op in ops:
            for d in deps[op.idx]:
                if not skip(ops[d], op):
                    needed[d] = True
        lastc = {}
        for op in ops:
            if not op.is_dma:
                lastc[op.eng] = op.idx
        for e, i in lastc.items():
            needed[i] = True
        for op in ops:
            if op.is_dma or needed[op.idx]:
                c = self._chan(op)
                self.cnt[c] = self.cnt.get(c, 0) + (16 if op.is_dma else 1)
                op.tick = self.cnt[c]
                op.inc = True
                self._sem(c)
        bar = self.pending_barrier
        grp_final = {}
        for op in ops:
            if op.is_dma and str(op.semkey).startswith("ld"):
                grp_final[op.semkey] = op.tick
        streams = {"pe": [], "act": [], "dve": [], "pool": [], "sp": []}
        for op in ops:
            ie = self.ISSUE[op.eng]
            w = {}
            for d in deps[op.idx]:
                dop = ops[d]
                if skip(dop, op):
                    continue
                c = self._chan(dop)
                if dop.is_dma and c in grp_final and op.is_dma and str(op.semkey).startswith("ld"):
                    continue
                w[c] = max(w.get(c, 0), grp_final.get(c, dop.tick) if dop.is_dma else dop.tick)
            if bar is not None and not self.barrier_done.get(ie, False):
                for c, t in bar.items():
                    w[c] = max(w.get(c, 0), t)
                self.barrier_done[ie] = True
            for c, t in list(w.items()):
                if self.waited.get((ie, c), 0) >= t:
                    del w[c]
                else:
                    self.waited[(ie, c)] = t
            op.waits = w
            streams[ie].append(op)
        sems = self.sems
        chan = self._chan
        with nc.Block() as block:
            def make(lst):
                def body(eng):
                    for op in lst:
                        for c, t in op.waits.items():
                            eng.wait_ge(sems[c], t)
                        ins = op.fn(eng)
                        if op.inc:
                            ins.then_inc(sems[chan(op)], 16 if op.is_dma else 1)
                return body
            for ename, lst in streams.items():
                if lst:
                    getattr(block, self.ENG[ename])(make(lst))
        self.n_emitted += n
        self.ops = []
        self.pending_barrier = dict(self.cnt)
        self.barrier_done = {}

    def finish(self):
        self.flush()
        nc = self.nc
        cnt = dict(self.cnt)
        sems = self.sems
        with nc.Block() as block:
            @block.sync
            def _(eng):
                for c, t in cnt.items():
                    eng.wait_ge(sems[c], t)


def fap(base, dims, off=0):
    return bass.AP(base.tensor, base.offset + off, [list(base.ap[0])] + [list(d) for d in dims])


def build_nc(stage=99):
    nc = bass.Bass("TRN2", target_bir_lowering=False)
    dI = lambda n, s, dt=F32: nc.dram_tensor(n, list(s), dt, kind="ExternalInput").ap()
    dO = lambda n, s, dt=F32: nc.dram_tensor(n, list(s), dt, kind="ExternalOutput").ap()
    dS = lambda n, s, dt=F32: nc.dram_tensor(n, list(s), dt, kind="Internal").ap()
    xo = dI("xo", [NOWN, D]); xp = dI("xp", [NOWN, D]); xs = dI("xs", [16, D]); flag_d = dI("flag", [128, 1])
    w_in = dI("w_in", [43, 128, D]); binT_d = dI("binT", [128, 43]); muT_d = dI("muT", [128, 27])
    cwT_d = dI("cwT", [128, 8, 31]); cvec_d = dI("cvec", [128, 3, 8])
    pvec_d = dI("pvec", [128, 7, 8])
    w2_d = dI("w2", [64, 1024]); a2_d = dI("a2", [64, 1024]); g2_d = dI("g2", [160, 1024])
    w_out = dI("w_out", [16, 128, D]); lnv_d = dI("lnv", [4, D])
    rw_d = dI("rw", [D, NEXP]); rb_d = dI("rb", [1, NEXP])
    if stage >= 4:
        w_gate = dI("w_gate", [NEXP, 8, 128, 4096]); w_up = dI("w_up", [NEXP, 8, 128, 4096]); w_down = dI("w_down", [NEXP, 8, 128, 4096])
        bgu_d = dI("bgu", [128, 2, NEXP, 16]); bdn_d = dI("bdn", [NEXP, D])
    sconv_d = dI("sconv", [30, C_CONV]); sshT_d = dI("sshT", [128, 27]); swkv_d = dI("swkv", [8, 128, 64])
    ident_d = dI("ident", [128, 128]); maskh_d = dI("maskh", [128, 2]); m1_d = dI("m1", [128, 256]); m2_d = dI("m2", [128, 256])
    onesbd_d = dI("onesbd", [128, 128]); tri_d = dI("tri", [128, 128]); ecap_d = dI("ecap", [128, NEXP]); rvt_d = dI("rvt", [128, 2])
    y_own = dO("y_own", [NSUB * 128, D])
    conv_o = dO("conv_o", [30, C_CONV]); conv_so = dO("conv_so", [30, C_CONV])
    shift_o = dO("shift_o", [128, 27]); shift_so = dO("shift_so", [128, 27])
    wkv_o = dO("wkv_o", [8, 128, 64]); wkv_so = dO("wkv_so", [8, 128, 64])
    x1s = dS("x1s", [NSUB * 128, D])
    xsorted = dS("xsorted", [NROWS + 128, XROW], BF16)
    ysorted = dS("ysorted", [NROWS, D])

    with ExitStack() as top:
        P = Prog(nc, top)
        A = P.add
        sbt = lambda st, n, s, dt=F32: st.enter_context(nc.sbuf_tensor("sb_" + n, list(s), dt))
        pbank = [top.enter_context(nc.psum_tensor(f"pb{i}", [128, 512], F32)) for i in range(8)]
        PS = {}
        for i_, n_ in enumerate(("pj0", "pj1", "m0", "m1", "fb0", "fb1", "c0", "c1")):
            PS[n_] = pbank[i_][:, :]
        rot = {}
        done_subs = []

        def nxt(prefix, n):
            i = rot.get(prefix, 0)
            rot[prefix] = (i + 1) % n
            return f"{prefix}{i}"

        cst = top
        ident = sbt(cst, "ident", [128, 128]); identb = sbt(cst, "identb", [128, 128], BF16)
        maskh = sbt(cst, "maskh", [128, 2]); m1 = sbt(cst, "m1", [128, 256]); m2 = sbt(cst, "m2", [128, 256])
        onesbd = sbt(cst, "onesbd", [128, 128]); flag = sbt(cst, "flag", [128, 1])
        idx_all = sbt(top, "idx_all", [128, NSUB, 4], I32)
        rvt = sbt(top, "rvt", [128, 2])
        A("sp", lambda e: e.dma_start(out=rvt[:], in_=rvt_d), writes=["rvt"], semkey="ld0")
        epsc = sbt(top, "epsc", [128, 3])
        A("pool", lambda e: e.memset(epsc[:, 0:1], LN_EPS), writes=["epsc"])
        A("pool", lambda e: e.memset(epsc[:, 1:2], GN_EPS), writes=["epsc"])
        A("pool", lambda e: e.memset(epsc[:, 2:3], 1e-24), writes=["epsc"])

        def rsqrt(dst, src, col, rk, wk):
            A("act", lambda e: e.activation(out=dst, in_=src, func=AF.Sqrt, bias=epsc[0:dst.shape[0], col:col + 1], scale=1.0), reads=rk + ["epsc"], writes=wk)
            A("dve", lambda e: e.reciprocal(out=dst, in_=dst), reads=wk, writes=wk)
        gd_all = sbt(top, "gd_all", [128, NSUB, NEXP])
        A("sp", lambda e: e.dma_start(out=ident[:], in_=ident_d), writes=["ident"], semkey="ld0")
        A("gq", lambda e: e.dma_start(out=identb[:], in_=ident_d), writes=["identb"], semkey="ld1")
        A("sp", lambda e: e.dma_start(out=maskh[:], in_=maskh_d), writes=["maskh"], semkey="ld0")
        A("sp", lambda e: e.dma_start(out=m1[:], in_=m1_d), writes=["m1"], semkey="ld0")
        A("sp", lambda e: e.dma_start(out=m2[:], in_=m2_d), writes=["m2"], semkey="ld0")
        A("sp", lambda e: e.dma_start(out=onesbd[:], in_=onesbd_d), writes=["onesbd"], semkey="ld0")
        A("sp", lambda e: e.dma_start(out=flag[:], in_=flag_d), writes=["flag"], semkey="ld0")

        w_in_bf = dS("w_in_bf", [43, 128, D], BF16)
        w_out_bf = dS("w_out_bf", [16, 128, D], BF16)
        for q in range(43):
            A("gq", lambda e, q=q: e.dma_start(out=w_in_bf[q], in_=w_in[q]), writes=["w_in_bf"], semkey="ld1")
        for q in range(16):
            A("gq", lambda e, q=q: e.dma_start(out=w_out_bf[q], in_=w_out[q]), writes=["w_out_bf"], semkey="ld1")
        with ExitStack() as pa:
            binT = sbt(pa, "binT", [128, 43]); muT = sbt(pa, "muT", [128, 27])
            cwT = sbt(pa, "cwT", [128, 8, 31]); cvec = sbt(pa, "cvec", [128, 3, 8]); pvec = sbt(pa, "pvec", [128, 7, 8])
            w2a2 = sbt(pa, "w2a2", [128, 1024]); g2a = sbt(pa, "g2a", [128, 1024]); g2b = sbt(pa, "g2b", [32, 1024])
            for (t_, d_, k_) in ((binT, binT_d, "binT"), (muT, muT_d, "muT"), (cwT, cwT_d, "cwT"), (cvec, cvec_d, "cvec"), (pvec, pvec_d, "pvec")):
                A("sp", lambda e, t_=t_, d_=d_: e.dma_start(out=t_[:], in_=d_), writes=[k_], semkey="ld0")
            A("sp", lambda e: e.dma_start(out=w2a2[0:64, :], in_=w2_d), writes=["w2a2"], semkey="ld0")
            A("sp", lambda e: e.dma_start(out=w2a2[64:128, :], in_=a2_d), writes=["w2a2"], semkey="ld0")
            A("sp", lambda e: e.dma_start(out=g2a[:], in_=g2_d[0:128, :]), writes=["g2a"], semkey="ld0")
            A("sp", lambda e: e.dma_start(out=g2b[:], in_=g2_d[128:160, :]), writes=["g2b"], semkey="ld0")
            lnv = sbt(pa, "lnv", [128, 2, D])
            A("sp", lambda e: e.dma_start(out=lnv[:, 0, :], in_=lnv_d[0:1, :].partition_broadcast(128)), writes=["lnv"], semkey="ld0")
            A("sp", lambda e: e.dma_start(out=lnv[:, 1, :], in_=lnv_d[1:2, :].partition_broadcast(128)), writes=["lnv"], semkey="ld0")
            rw = sbt(pa, "rw", [128, 16, NEXP]); rb = sbt(pa, "rb", [128, NEXP]); ecap = sbt(pa, "ecap", [128, NEXP])
            tri = sbt(pa, "tri", [128, 128], BF16); onesb = sbt(pa, "onesb", [128, 128], BF16)
            A("sp", lambda e: e.dma_start(out=rw[:], in_=rw_d.rearrange("(k p) n -> p k n", p=128)), writes=["rw"], semkey="ld0")
            A("sp", lambda e: e.dma_start(out=rb[:], in_=rb_d.partition_broadcast(128)), writes=["rb"], semkey="ld0")
            A("sp", lambda e: e.dma_start(out=ecap[:], in_=ecap_d), writes=["ecap"], semkey="ld0")
            A("gq", lambda e: e.dma_start(out=tri[:], in_=tri_d), writes=["tri"], semkey="ld1")
            A("pool", lambda e: e.memset(onesb[:], 1.0), writes=["onesb"])
            onesf = sbt(pa, "onesf", [128, 128]); A("pool", lambda e: e.memset(onesf[:], 1.0), writes=["onesf"])
            ones_t = sbt(pa, "ones_t", [128, CH]); A("pool", lambda e: e.memset(ones_t[:], 1.0), writes=["ones_t"])
            carry = sbt(pa, "carry", [128, 27]); A("pool", lambda e: e.memset(carry[:], 0.0), writes=["carry"])
            uhist = sbt(pa, "uhist", [128, 8, 30]); A("pool", lambda e: e.memset(uhist[:], 0.0), writes=["uhist"])
            S32 = [sbt(pa, f"S32_{p}", [128, 128]) for p in range(8)]
            Sbf = [sbt(pa, f"Sbf_{p}", [128, 128], BF16) for p in range(8)]
            for p in range(8):
                A("pool", lambda e, p=p: e.memset(S32[p][:], 0.0), writes=[f"S32_{p}"])
                A("pool", lambda e, p=p: e.memset(Sbf[p][:], 0.0), writes=[f"Sbf_{p}"])
            cntbase = sbt(pa, "cntbase", [128, NEXP]); A("pool", lambda e: e.memset(cntbase[:], 0.0), writes=["cntbase"])
            xt = sbt(pa, "xt", [128, D])
            xT = sbt(pa, "xT", [128, 16, T], BF16)
            wst = [sbt(pa, f"wst{i}", [128, 16, 128], BF16) for i in range(2)]
            zraw = [sbt(pa, f"zraw{i}", [128, T + 1]) for i in range(2)]
            zd = [sbt(pa, f"zd{i}", [128, T]) for i in range(2)]
            zg = sbt(pa, "zg", [128, 12, T])
            zl = sbt(pa, "zl", [128, 3, T]); lor = zl
            uT = sbt(pa, "uT", [128, 8, 30 + T])
            sgt = [sbt(pa, f"sgt{i}", [128, T]) for i in range(2)]
            cacc = [sbt(pa, f"cacc{i}", [128, T]) for i in range(2)]
            csq = [sbt(pa, f"csq{i}", [128, T]) for i in range(2)]
            cfull = sbt(pa, "cfull", [128, 8, T])
            lnm = sbt(pa, "lnm", [128, 3, T])
            catT = sbt(pa, "catT", [128, 16, T], BF16)
            NR = 4
            PBQ = ["m0", "m1", "fb0", "fb1"]
            def mk(n, s, dt=F32):
                return [sbt(pa, f"{n}{i}", s, dt) for i in range(NR)]
            e_sg = mk("e_sg", [128, TS]); e_a = mk("e_a", [128, TS]); e_kk0 = mk("e_kk0", [128, TS])
            e_t = mk("e_t", [128, TS]); e_kk = mk("e_kk", [128, TS]); e_km = mk("e_km", [128, TS])
            e_cs = mk("e_cs", [128, TS]); e_csm = mk("e_csm", [128, TS]); e_eg = mk("e_eg", [128, TS]); e_ieg = mk("e_ieg", [128, TS])
            e_egm = mk("e_egm", [128, TS]); e_n1 = mk("e_n1", [128, TS], BF16); e_n2 = mk("e_n2", [128, TS], BF16)
            e_n3 = mk("e_n3", [128, TS], BF16); e_t2 = mk("e_t2", [128, TS])
            NCS = TS // CH
            AR = [sbt(pa, f"AR{q}", [128, NCS, 192], BF16) for q in range(4)]
            BT = [sbt(pa, f"BT{q}", [128, NCS, 128], BF16) for q in range(4)]
            KT = [sbt(pa, f"KT{q}", [128, NCS, 128], BF16) for q in range(4)]
            VT = [sbt(pa, f"VT{q}", [128, NCS, 128], BF16) for q in range(4)]
            TOK = [sbt(pa, f"TOK{q}", [128, NCS, 384], BF16) for q in range(4)]
            GC = [sbt(pa, f"GC{q}", [128, NCS]) for q in range(4)]
            BON = [sbt(pa, f"BON{q}", [128, TS]) for q in range(4)]
            GG = [sbt(pa, f"GG{q}", [128, TS]) for q in range(4)]
            YS = [sbt(pa, f"YS{q}", [128, TS]) for q in range(4)]
            NU = 4
            Lt = [sbt(pa, f"Lt{i}", [128, 640], BF16) for i in range(NU)]
            Xa = [sbt(pa, f"Xa{i}", [128, 384], BF16) for i in range(NU)]
            Xb = [sbt(pa, f"Xb{i}", [128, 384], BF16) for i in range(NU)]
            for i_ in range(NU):
                A("pool", lambda e, i_=i_: e.tensor_copy(out=Lt[i_][:, 256:384], in_=identb[:]), reads=["identb"], writes=[f"Lt{i_}"])
            TT = [sbt(pa, f"TT{q}", [128, 128], BF16) for q in range(4)]
            Wb = [sbt(pa, f"Wb{q}", [128, 128], BF16) for q in range(4)]
            Ub = [sbt(pa, f"Ub{q}", [128, 128], BF16) for q in range(4)]
            stmp = [sbt(pa, f"stmp{i}", [128, 128]) for i in range(2)]
            xrow = [sbt(pa, f"xrow{i}", [128, XROW], BF16) for i in range(2)]
            x1Tb = [sbt(pa, "x1Tb0", [128, 4, 128])] * 2
            A("pool", lambda e: e.memset(xrow[0][:], 0.0), writes=["xrow0"])
            xs_v = xsorted.rearrange("(r p) c -> p r c", p=128)
            nblk_x = (NROWS + 128) // 128
            for q0 in range(0, nblk_x, 20):
                nb_ = min(20, nblk_x - q0)
                A("sp", lambda e, q0=q0, nb_=nb_: e.dma_start(out=xs_v[:, q0:q0 + nb_, :], in_=fap(xrow[0][:], [[0, nb_], [1, XROW]])), reads=["xrow0"], writes=["xsorted"], semkey="ld0")
            bst = sbt(pa, "bst", [128, 4, 6]); mv = sbt(pa, "mv", [128, 2]); rstd = sbt(pa, "rstd", [128, 1])
            lg = sbt(pa, "lg", [128, NEXP]); m8 = sbt(pa, "m8", [128, 8]); negm = sbt(pa, "negm", [128, 1])
            e4 = sbt(pa, "e4", [128, 4]); s4 = sbt(pa, "s4", [128, 1]); g4 = sbt(pa, "g4", [128, 4])
            oh = sbt(pa, "oh", [128, 4, NEXP]); msk = sbt(pa, "msk", [128, NEXP]); mskb = sbt(pa, "mskb", [128, NEXP], BF16)
            posf = sbt(pa, "posf", [128, NEXP]); pk = sbt(pa, "pk", [128, 4]); junk = sbt(pa, "junk", [128, NEXP])
            cT8 = sbt(pa, "cT8", [30, C_CONV])
            idx_sc = sbt(pa, "idx_sc", [128, 4], I32)

            def load_x(src_ap, rows):
                if rows < 128:
                    A("pool", lambda e: e.memset(xt[:], 0.0), writes=["xt"])
                A("sp", lambda e: e.dma_start(out=xt[0:rows, :], in_=src_ap), writes=["xt"], semkey="x0")

            def transpose_x(col0, ncol):
                for g in range(4):
                    pn = nxt("pj", 2)
                    for j in range(4):
                        kc = 4 * g + j
                        A("pe", lambda e, pn=pn, j=j, kc=kc: e.transpose(out=PS[pn][:, j * 128:(j + 1) * 128], in_=xt[:, kc * 128:(kc + 1) * 128], identity=ident[:]),
                          reads=["xt", "ident"], writes=[pn])
                    eng = "act" if g % 2 == 0 else "dve"
                    def ev(e, pn=pn, g=g, eng=eng):
                        src = PS[pn].rearrange("p (j t) -> p j t", j=4)[:, :, 0:ncol]
                        dst = xT[:, 4 * g:4 * g + 4, col0:col0 + ncol]
                        return e.copy(out=dst, in_=src) if eng == "act" else e.tensor_copy(out=dst, in_=src)
                    A(eng, ev, reads=[pn], writes=["xT"])

            wst_rot = [0]

            def proj_cols(cts, ncol, consume):
                for ct in cts:
                    wi = wst_rot[0]; wst_rot[0] ^= 1
                    width = min(128, P_IN - ct * 128)
                    A("sp", lambda e, ct=ct, wi=wi: e.dma_start(out=wst[wi][:], in_=w_in_bf[ct].rearrange("p (k n) -> p k n", k=16)),
                      reads=["w_in_bf"], writes=[f"wst{wi}"], semkey=f"wst{wi}")
                    pn = nxt("pj", 2)
                    for kc in range(16):
                        A("pe", lambda e, pn=pn, kc=kc, width=width, wi=wi: e.matmul(PS[pn][0:width, 0:ncol], lhsT=wst[wi][:, kc, 0:width],
                                                                                  rhs=xT[:, kc, 0:ncol], start=(kc == 0), stop=(kc == 15)),
                          reads=[f"wst{wi}", "xT"], writes=[pn])
                    consume(ct, pn, width)

            zr_rot = [0]

            def z_consume(ncol, valid, dst, dstk, slot_of):
                def consume(ct, pn, width):
                    zi = ct - 16
                    sl = slot_of(ct)
                    ri = zr_rot[0]; zr_rot[0] = (ri + 1) % 2
                    zr, zk = zraw[ri], f"zraw{ri}"
                    di = ri % 2
                    A("act", lambda e: e.activation(out=zr[0:width, 1:ncol + 1], in_=PS[pn][0:width, 0:ncol], func=AF.Identity, bias=binT[0:width, ct:ct + 1], scale=1.0),
                      reads=[pn, "binT"], writes=[zk])
                    A("pool", lambda e: e.tensor_copy(out=zr[0:width, 0:1], in_=carry[0:width, zi:zi + 1]), reads=["carry"], writes=[zk])
                    A("pool", lambda e: e.tensor_copy(out=carry[0:width, zi:zi + 1], in_=zr[0:width, valid:valid + 1]), reads=[zk], writes=["carry"])
                    A("dve", lambda e: e.tensor_tensor(out=zd[di][0:width, 0:ncol], in0=zr[0:width, 0:ncol], in1=zr[0:width, 1:ncol + 1], op=ALU.subtract),
                      reads=[zk], writes=[f"zd{di}"])
                    A("dve", lambda e: e.scalar_tensor_tensor(out=dst[0:width, sl, 0:ncol], in0=zd[di][0:width, 0:ncol], scalar=muT[0:width, zi:zi + 1],
                                                              in1=zr[0:width, 1:ncol + 1], op0=ALU.mult, op1=ALU.add),
                      reads=[f"zd{di}", zk, "muT"], writes=[dstk])
                return consume

            def u_consume(ncol):
                def consume(ct, pn, width):
                    if ct >= 8:
                        gi = ct - 8
                        A("act", lambda e: e.activation(out=sgt[gi % 2][:, 0:ncol], in_=PS[pn][:, 0:ncol], func=AF.Sigmoid, bias=binT[:, ct:ct + 1], scale=1.0),
                          reads=[pn, "binT"], writes=[f"sgt{gi % 2}"])
                    else:
                        A("dve", lambda e: e.scalar_tensor_tensor(out=uT[:, ct, 30:30 + ncol], in0=PS[pn][:, 0:ncol], scalar=binT[:, ct:ct + 1],
                                                                  in1=sgt[ct % 2][:, 0:ncol], op0=ALU.add, op1=ALU.mult),
                          reads=[pn, "binT", f"sgt{ct % 2}"], writes=["uT"])
                return consume

            def glu_proj(ncol):
                for ci in range(8):
                    proj_cols([8 + ci, ci], ncol, u_consume(ncol))

            def conv_block(ncol):
                for ci in range(8):
                    ai = ci % 2
                    A("dve", lambda e, ci=ci, ai=ai: e.tensor_scalar(out=cacc[ai][:, 0:ncol], in0=uT[:, ci, 0:ncol], scalar1=cwT[:, ci, 0:1], scalar2=cvec[:, 0, ci:ci + 1],
                                                                    op0=ALU.mult, op1=ALU.add), reads=["uT", "cwT", "cvec"], writes=[f"cacc{ai}"])
                    for j in range(1, 31):
                        last = (j == 30)
                        A("dve", lambda e, ci=ci, ai=ai, j=j, last=last: e.scalar_tensor_tensor(
                            out=(cfull[:, ci, 0:ncol] if last else cacc[ai][:, 0:ncol]), in0=uT[:, ci, j:j + ncol], scalar=cwT[:, ci, j:j + 1],
                            in1=cacc[ai][:, 0:ncol], op0=ALU.mult, op1=ALU.add),
                          reads=["uT", "cwT", f"cacc{ai}"], writes=(["cfull"] if last else [f"cacc{ai}"]))
                    A("act", lambda e, ci=ci, ai=ai: e.activation(out=csq[ai][:, 0:ncol], in_=cfull[:, ci, 0:ncol], func=AF.Square), reads=["cfull"], writes=[f"csq{ai}"])
                    A("pe", lambda e, ci=ci: e.matmul(PS["m0"][:, 0:ncol], lhsT=onesf[:], rhs=cfull[:, ci, 0:ncol], start=(ci == 0), stop=(ci == 7)),
                      reads=["cfull", "onesf"], writes=["m0"])
                    A("pe", lambda e, ci=ci, ai=ai: e.matmul(PS["m1"][:, 0:ncol], lhsT=onesf[:], rhs=csq[ai][:, 0:ncol], start=(ci == 0), stop=(ci == 7)),
                      reads=[f"csq{ai}", "onesf"], writes=["m1"])
                A("act", lambda e: e.activation(out=lnm[:, 0, 0:ncol], in_=PS["m0"][:, 0:ncol], func=AF.Copy, scale=1.0 / C_CONV), reads=["m0"], writes=["lnm"])
                A("pool", lambda e: e.tensor_tensor(out=lnm[:, 1, 0:ncol], in0=lnm[:, 0, 0:ncol], in1=lnm[:, 0, 0:ncol], op=ALU.mult), reads=["lnm"], writes=["lnm"])
                A("dve", lambda e: e.scalar_tensor_tensor(out=lnm[:, 2, 0:ncol], in0=PS["m1"][:, 0:ncol], scalar=1.0 / C_CONV, in1=lnm[:, 1, 0:ncol], op0=ALU.mult, op1=ALU.subtract),
                  reads=["m1", "lnm"], writes=["lnm"])
                rsqrt(lnm[:, 2, 0:ncol], lnm[:, 2, 0:ncol], 0, ["lnm"], ["lnm"])
                for ci in range(8):
                    ai = ci % 2
                    A("pool", lambda e, ci=ci, ai=ai: e.tensor_tensor(out=csq[ai][:, 0:ncol], in0=cfull[:, ci, 0:ncol], in1=lnm[:, 0, 0:ncol], op=ALU.subtract), reads=["cfull", "lnm"], writes=[f"csq{ai}"])
                    A("dve", lambda e, ci=ci, ai=ai: e.tensor_tensor(out=csq[ai][:, 0:ncol], in0=csq[ai][:, 0:ncol], in1=lnm[:, 2, 0:ncol], op=ALU.mult), reads=[f"csq{ai}", "lnm"], writes=[f"csq{ai}"])
                    A("act", lambda e, ci=ci, ai=ai: e.activation(out=catT[:, ci, 0:ncol], in_=csq[ai][:, 0:ncol], func=AF.Silu, bias=cvec[:, 2, ci:ci + 1], scale=cvec[:, 1, ci:ci + 1]),
                      reads=[f"csq{ai}", "cvec"], writes=["catT"])

            def save_uhist(valid):
                A("pool", lambda e: e.tensor_copy(out=uhist[:], in_=uT[:, :, valid:valid + 30]), reads=["uT"], writes=["uhist"])

            def load_uhist():
                A("pool", lambda e: e.tensor_copy(out=uT[:, :, 0:30], in_=uhist[:]), reads=["uhist"], writes=["uT"])

            def lora_acts(ncol, full):
                A("act", lambda e: e.activation(out=lor[0:64, 0, 0:ncol], in_=zl[0:64, 0, 0:ncol], func=AF.Tanh), reads=["zl"], writes=["zl"])
                if full:
                    A("act", lambda e: e.activation(out=lor[:, 1, 0:ncol], in_=zl[:, 1, 0:ncol], func=AF.Sigmoid), reads=["zl"], writes=["zl"])
                    A("act", lambda e: e.activation(out=lor[0:32, 2, 0:ncol], in_=zl[0:32, 2, 0:ncol], func=AF.Sigmoid), reads=["zl"], writes=["zl"])

            def bd(eng_name, dst3, src2, nch, keyr, keyw):
                def f(e):
                    in0 = fap(src2, [[CH, nch], [0, 2], [1, CH]])
                    in1 = fap(maskh[:], [[0, nch], [1, 2], [0, CH]])
                    return e.tensor_tensor(out=dst3, in0=in0, in1=in1, op=ALU.mult)
                A(eng_name, f, reads=keyr + ["maskh"], writes=keyw)

            def pair_prep(p, col0, ncol, nch, full, valid):
                q = p % 4
                i = q % NR
                pv = lambda j: pvec[:, j, p:p + 1]
                cw = slice(col0, col0 + ncol)
                zr_ = zg[:, q, cw]; zk_ = zg[:, 4 + q, cw]; zv_ = zg[:, 8 + q, cw]
                K = lambda n: [f"{n}{i}"]
                cs_ = slice(p * 128, (p + 1) * 128)
                N = slice(0, ncol)
                if valid < ncol:
                    for sl in (q, 4 + q, 8 + q):
                        yield A("pool", lambda e, sl=sl: e.memset(zg[:, sl, col0 + valid:col0 + ncol], 0.0), writes=["zg"])
                pw = PBQ[q]
                yield A("pe", lambda e: e.matmul(PS[pw][:, N], lhsT=w2a2[0:64, cs_], rhs=lor[0:64, 0, cw], start=True, stop=True), reads=["w2a2", "zl"], writes=[pw])
                yield A("act", lambda e: e.activation(out=e_sg[i][:, N], in_=PS[pw][:, N], func=AF.Sigmoid, bias=pv(0), scale=1.0), reads=[pw, "pvec"], writes=K("e_sg"))
                if valid < ncol:
                    yield A("pool", lambda e: e.memset(e_sg[i][:, valid:ncol], 0.0), writes=K("e_sg"))
                pa_ = PBQ[q]
                yield A("pe", lambda e: e.matmul(PS[pa_][:, N], lhsT=w2a2[64:128, cs_], rhs=lor[64:128, 0, cw], start=True, stop=True), reads=["w2a2", "zl"], writes=[pa_])
                yield A("act", lambda e: e.activation(out=e_a[i][:, N], in_=PS[pa_][:, N], func=AF.Sigmoid, bias=pv(1), scale=1.0), reads=[pa_, "pvec"], writes=K("e_a"))
                if full:
                    pg = PBQ[q]
                    yield A("pe", lambda e: e.matmul(PS[pg][:, N], lhsT=g2a[:, cs_], rhs=lor[:, 1, cw], start=True, stop=False), reads=["g2a", "zl"], writes=[pg])
                    yield A("pe", lambda e: e.matmul(PS[pg][:, N], lhsT=g2b[0:32, cs_], rhs=lor[0:32, 2, cw], start=False, stop=True), reads=["g2b", "zl"], writes=[pg])
                    yield A("act", lambda e: e.copy(out=GG[q][:, N], in_=PS[pg][:, N]), reads=[pg], writes=[f"GG{q}"])
                yield A("dve", lambda e: e.tensor_scalar(out=e_kk0[i][:, N], in0=zk_, scalar1=pv(2), scalar2=None, op0=ALU.mult), reads=["zg", "pvec"], writes=K("e_kk0"))
                yield A("act", lambda e: e.activation(out=e_t[i][:, N], in_=e_kk0[i][:, N], func=AF.Square), reads=K("e_kk0"), writes=K("e_t"))
                pss = PBQ[q]
                yield A("pe", lambda e: e.matmul(PS[pss][:, N], lhsT=onesbd[:], rhs=e_t[i][:, N], start=True, stop=True), reads=["onesbd"] + K("e_t"), writes=[pss])
                yield rsqrt(e_t[i][:, N], PS[pss][:, N], 2, [pss], K("e_t"))
                yield A("dve", lambda e: e.tensor_tensor(out=e_kk[i][:, N], in0=e_kk0[i][:, N], in1=e_t[i][:, N], op=ALU.mult), reads=K("e_kk0") + K("e_t"), writes=K("e_kk"))
                yield A("dve", lambda e: e.tensor_scalar(out=e_t2[i][:, N], in0=e_a[i][:, N], scalar1=-1.0, scalar2=pv(3), op0=ALU.add, op1=ALU.mult), reads=K("e_a") + ["pvec"], writes=K("e_t2"))
                yield A("dve", lambda e: e.scalar_tensor_tensor(out=e_km[i][:, N], in0=e_t2[i][:, N], scalar=1.0, in1=zk_, op0=ALU.add, op1=ALU.mult), reads=K("e_t2") + ["zg"], writes=K("e_km"))
                if full:
                    yield A("dve", lambda e: e.scalar_tensor_tensor(out=e_t2[i][:, N], in0=zr_, scalar=pv(4), in1=e_km[i][:, N], op0=ALU.mult, op1=ALU.mult), reads=["zg", "pvec"] + K("e_km"), writes=K("e_t2"))
                    pb_ = PBQ[q]
                    yield A("pe", lambda e: e.matmul(PS[pb_][:, N], lhsT=onesbd[:], rhs=e_t2[i][:, N], start=True, stop=True), reads=["onesbd"] + K("e_t2"), writes=[pb_])
                    yield A("dve", lambda e: e.tensor_tensor(out=BON[q][:, N], in0=PS[pb_][:, N], in1=zv_, op=ALU.mult), reads=[pb_, "zg"], writes=[f"BON{q}"])
                for c in range(nch):
                    yield A("dve", lambda e, c=c: e.tensor_tensor_scan(out=e_cs[i][:, c * CH:(c + 1) * CH], data0=ones_t[:], data1=e_sg[i][:, c * CH:(c + 1) * CH], initial=0.0, op0=ALU.mult, op1=ALU.add),
                      reads=K("e_sg") + ["ones_t"], writes=K("e_cs"))
                yield A("pool", lambda e: e.tensor_tensor(out=e_csm[i][:, N], in0=e_cs[i][:, N], in1=e_sg[i][:, N], op=ALU.subtract), reads=K("e_cs") + K("e_sg"), writes=K("e_csm"))
                yield A("act", lambda e: e.activation(out=e_eg[i][:, N], in_=e_cs[i][:, N], func=AF.Exp, scale=-DEC), reads=K("e_cs"), writes=K("e_eg"))
                yield A("act", lambda e: e.activation(out=e_ieg[i][:, N], in_=e_cs[i][:, N], func=AF.Exp, scale=DEC), reads=K("e_cs"), writes=K("e_ieg"))
                yield A("act", lambda e: e.activation(out=e_egm[i][:, N], in_=e_csm[i][:, N], func=AF.Exp, scale=-DEC), reads=K("e_csm"), writes=K("e_egm"))
                yield A("pool", lambda e: e.tensor_copy(out=GC[q][:, 0:nch], in_=fap(e_eg[i][:, N], [[CH, nch]], off=CH - 1)), reads=K("e_eg"), writes=[f"GC{q}"])
                yield A("dve", lambda e: e.scalar_tensor_tensor(out=e_n1[i][:, N], in0=e_kk[i][:, N], scalar=-1.0, in1=e_egm[i][:, N], op0=ALU.mult, op1=ALU.mult), reads=K("e_kk") + K("e_egm"), writes=K("e_n1"))
                yield bd("pool", AR[q][:, 0:nch, 0:128].rearrange("p c (h t) -> p c h t", h=2), e_n1[i][:, N], nch, K("e_n1"), [f"AR{q}"])
                yield A("dve", lambda e: e.tensor_tensor(out=e_t[i][:, N], in0=e_kk[i][:, N], in1=e_a[i][:, N], op=ALU.mult), reads=K("e_kk") + K("e_a"), writes=K("e_t"))
                yield A("dve", lambda e: e.tensor_tensor(out=e_n2[i][:, N], in0=e_t[i][:, N], in1=e_ieg[i][:, N], op=ALU.mult), reads=K("e_t") + K("e_ieg"), writes=K("e_n2"))
                yield bd("pool", BT[q][:, 0:nch, :].rearrange("p c (h t) -> p c h t", h=2), e_n2[i][:, N], nch, K("e_n2"), [f"BT{q}"])
                yield A("dve", lambda e: e.tensor_tensor(out=e_n3[i][:, N], in0=e_km[i][:, N], in1=e_ieg[i][:, N], op=ALU.mult), reads=K("e_km") + K("e_ieg"), writes=K("e_n3"))
                yield bd("pool", KT[q][:, 0:nch, :].rearrange("p c (h t) -> p c h t", h=2), e_n3[i][:, N], nch, K("e_n3"), [f"KT{q}"])
                if full:
                    yield A("dve", lambda e: e.tensor_tensor(out=AR[q][:, 0:nch, 128:192], in0=zr_.rearrange("p (c t) -> p c t", t=CH), in1=e_eg[i][:, N].rearrange("p (c t) -> p c t", t=CH), op=ALU.mult),
                      reads=["zg"] + K("e_eg"), writes=[f"AR{q}"])
                yield bd("pool", VT[q][:, 0:nch, :].rearrange("p c (h t) -> p c h t", h=2), zv_, nch, ["zg"], [f"VT{q}"])
                for c in range(nch):
                    pt = PBQ[q]
                    ptb = PS[pt].bitcast(BF16)
                    for j, (src, sn) in enumerate(((BT, "BT"), (KT, "KT"), (VT, "VT"))):
                        yield A("pe", lambda e, j=j, src=src, c=c, ptb=ptb: e.transpose(out=ptb[:, j * 128:(j + 1) * 128], in_=src[q][:, c, :], identity=identb[:]),
                          reads=[f"{sn}{q}", "identb"], writes=[pt])
                    yield A("act", lambda e, c=c, ptb=ptb: e.copy(out=TOK[q][:, c, :], in_=ptb[:, 0:384]), reads=[pt], writes=[f"TOK{q}"])

            u_rot = [0]

            def unit_local(p, c, full):
                q = p % 4
                ui = q
                L, Lk = Lt[ui], f"Lt{ui}"
                nR = 192 if full else 128
                gb = PBQ[q]
                yield A("pe", lambda e: e.matmul(PS[gb][:, 0:128], lhsT=AR[q][:, c, 0:128], rhs=BT[q][:, c, :], start=True, stop=True), reads=[f"AR{q}", f"BT{q}"], writes=[gb])
                yield A("pe", lambda e: e.matmul(PS[gb][:, 128:128 + nR], lhsT=BT[q][:, c, :], rhs=AR[q][:, c, 0:nR], start=True, stop=True), reads=[f"AR{q}", f"BT{q}"], writes=[gb])
                yield A("pe", lambda e: e.matmul(PS[gb][:, 320:320 + nR], lhsT=KT[q][:, c, :], rhs=AR[q][:, c, 0:nR], start=True, stop=True), reads=[f"AR{q}", f"KT{q}"], writes=[gb])
                yield A("dve", lambda e: e.tensor_tensor(out=L[:, 0:256], in0=PS[gb][:, 0:256], in1=m1[:], op=ALU.mult), reads=[gb, "m1"], writes=[Lk])
                if full:
                    yield A("dve", lambda e: e.tensor_tensor(out=L[:, 384:640], in0=PS[gb][:, 256:512], in1=m2[:], op=ALU.mult), reads=[gb, "m2"], writes=[Lk])
                else:
                    yield A("dve", lambda e: e.tensor_tensor(out=L[:, 448:576], in0=PS[gb][:, 320:448], in1=m2[:, 64:192], op=ALU.mult), reads=[gb, "m2"], writes=[Lk])
                cur, curk = L, Lk
                bufs = [(Xa[ui], f"Xa{ui}"), (Xb[ui], f"Xb{ui}")]
                for lev in range(6):
                    fbn = PBQ[q]
                    if lev == 5:
                        yield A("pe", lambda e, cur=cur, fbn=fbn: e.matmul(PS[fbn][:, 256:384], lhsT=cur[:, 0:128], rhs=cur[:, 256:384], start=True, stop=True), reads=[curk], writes=[fbn])
                        yield A("dve", lambda e, cur=cur, fbn=fbn: e.tensor_tensor(out=TT[q][:], in0=PS[fbn][:, 256:384], in1=cur[:, 256:384], op=ALU.add), reads=[fbn, curk], writes=[f"TT{q}"])
                    else:
                        nx, nxk = bufs[lev % 2]
                        yield A("pe", lambda e, cur=cur, fbn=fbn: e.matmul(PS[fbn][:, 128:384], lhsT=cur[:, 0:128], rhs=cur[:, 128:384], start=True, stop=True), reads=[curk], writes=[fbn])
                        yield A("pe", lambda e, cur=cur, fbn=fbn: e.matmul(PS[fbn][:, 0:128], lhsT=cur[:, 128:256], rhs=cur[:, 0:128], start=True, stop=True), reads=[curk], writes=[fbn])
                        yield A("act", lambda e, nx=nx, fbn=fbn: e.copy(out=nx[:, 0:256], in_=PS[fbn][:, 0:256]), reads=[fbn], writes=[nxk])
                        yield A("dve", lambda e, nx=nx, cur=cur, fbn=fbn: e.tensor_tensor(out=nx[:, 256:384], in0=PS[fbn][:, 256:384], in1=cur[:, 256:384], op=ALU.add), reads=[fbn, curk], writes=[nxk])
                        cur, curk = nx, nxk

            def chain_stage_w(p, c, ui):
                q = p % 4
                cn = nxt("c", 2)
                A("pe", lambda e: e.matmul(PS[cn][:, 0:128], lhsT=AR[q][:, c, 0:128], rhs=Sbf[p][:], start=True, stop=False), reads=[f"AR{q}", f"Sbf_{p}"], writes=[cn])
                A("pe", lambda e: e.matmul(PS[cn][:, 0:128], lhsT=Lt[ui][:, 448:576], rhs=TOK[q][:, c, 256:384], start=False, stop=True), reads=[f"Lt{ui}", f"TOK{q}"], writes=[cn])
                A("act", lambda e: e.copy(out=Wb[q][:], in_=PS[cn][:, 0:128]), reads=[cn], writes=[f"Wb{q}"])

            def chain_stage_u(p, c):
                q = p % 4
                cn = nxt("c", 2)
                A("pe", lambda e: e.matmul(PS[cn][:, 0:128], lhsT=TT[q][:], rhs=Wb[q][:], start=True, stop=True), reads=[f"TT{q}", f"Wb{q}"], writes=[cn])
                A("dve", lambda e: e.tensor_copy(out=Ub[q][:], in_=PS[cn][:, 0:128]), reads=[cn], writes=[f"Ub{q}"])

            def chain_stage_y(p, c, ui):
                q = p % 4
                cn = nxt("c", 2)
                A("pe", lambda e: e.matmul(PS[cn][:, 0:CH], lhsT=Sbf[p][:], rhs=AR[q][:, c, 128:192], start=True, stop=False), reads=[f"Sbf_{p}", f"AR{q}"], writes=[cn])
                A("pe", lambda e: e.matmul(PS[cn][:, 0:CH], lhsT=Ub[q][:], rhs=Lt[ui][:, 384:448], start=False, stop=False), reads=[f"Ub{q}", f"Lt{ui}"], writes=[cn])
                A("pe", lambda e: e.matmul(PS[cn][:, 0:CH], lhsT=TOK[q][:, c, 256:384], rhs=Lt[ui][:, 576:640], start=False, stop=True), reads=[f"TOK{q}", f"Lt{ui}"], writes=[cn])
                A("act", lambda e: e.copy(out=YS[q][:, c * CH:(c + 1) * CH], in_=PS[cn][:, 0:CH]), reads=[cn], writes=[f"YS{q}"])

            def chain_stage_s(p, c):
                q = p % 4
                cn = nxt("c", 2)
                si = p % 2
                A("pe", lambda e: e.matmul(PS[cn][:, 0:128], lhsT=TOK[q][:, c, 0:128], rhs=Ub[q][:], start=True, stop=False), reads=[f"TOK{q}", f"Ub{q}"], writes=[cn])
                A("pe", lambda e: e.matmul(PS[cn][:, 0:128], lhsT=TOK[q][:, c, 128:256], rhs=TOK[q][:, c, 256:384], start=False, stop=True), reads=[f"TOK{q}"], writes=[cn])
                A("dve", lambda e: e.tensor_tensor(out=stmp[si][:], in0=PS[cn][:, 0:128], in1=S32[p][:], op=ALU.add), reads=[cn, f"S32_{p}"], writes=[f"stmp{si}"])
                A("act", lambda e: e.activation(out=S32[p][:], in_=stmp[si][:], func=AF.Copy, scale=GC[q][:, c:c + 1]), reads=[f"stmp{si}", f"GC{q}"], writes=[f"S32_{p}"])
                A("pool", lambda e: e.tensor_copy(out=Sbf[p][:], in_=S32[p][:]), reads=[f"S32_{p}"], writes=[f"Sbf_{p}"])

            def rwkv_finish(p, col0, ncol):
                q = p % 4
                i = q % NR
                K = lambda n: [f"{n}{i}"]
                pv = lambda j: pvec[:, j, p:p + 1]
                N = slice(0, ncol)
                yield A("act", lambda e: e.activation(out=e_t[i][:, N], in_=YS[q][:, N], func=AF.Square), reads=[f"YS{q}"], writes=K("e_t"))
                pm = PBQ[q]
                yield A("pe", lambda e: e.matmul(PS[pm][:, N], lhsT=onesbd[:], rhs=YS[q][:, N], start=True, stop=True), reads=["onesbd", f"YS{q}"], writes=[pm])
                yield A("act", lambda e: e.activation(out=e_kk0[i][:, N], in_=PS[pm][:, N], func=AF.Copy, scale=1.0 / 64), reads=[pm], writes=K("e_kk0"))
                pq = PBQ[q]
                yield A("pe", lambda e: e.matmul(PS[pq][:, N], lhsT=onesbd[:], rhs=e_t[i][:, N], start=True, stop=True), reads=["onesbd"] + K("e_t"), writes=[pq])
                yield A("pool", lambda e: e.tensor_tensor(out=e_kk[i][:, N], in0=e_kk0[i][:, N], in1=e_kk0[i][:, N], op=ALU.mult), reads=K("e_kk0"), writes=K("e_kk"))
                yield A("dve", lambda e: e.scalar_tensor_tensor(out=e_t[i][:, N], in0=PS[pq][:, N], scalar=1.0 / 64, in1=e_kk[i][:, N], op0=ALU.mult, op1=ALU.subtract), reads=[pq] + K("e_kk"), writes=K("e_t"))
                yield rsqrt(e_t[i][:, N], e_t[i][:, N], 1, K("e_t"), K("e_t"))
                yield A("pool", lambda e: e.tensor_tensor(out=e_km[i][:, N], in0=YS[q][:, N], in1=e_kk0[i][:, N], op=ALU.subtract), reads=[f"YS{q}"] + K("e_kk0"), writes=K("e_km"))
                yield A("dve", lambda e: e.tensor_tensor(out=e_km[i][:, N], in0=e_km[i][:, N], in1=e_t[i][:, N], op=ALU.mult), reads=K("e_km") + K("e_t"), writes=K("e_km"))
                yield A("dve", lambda e: e.tensor_scalar(out=e_km[i][:, N], in0=e_km[i][:, N], scalar1=pv(5), scalar2=pv(6), op0=ALU.mult, op1=ALU.add), reads=K("e_km") + ["pvec"], writes=K("e_km"))
                yield A("pool", lambda e: e.tensor_tensor(out=e_km[i][:, N], in0=e_km[i][:, N], in1=BON[q][:, N], op=ALU.add), reads=K("e_km") + [f"BON{q}"], writes=K("e_km"))
                yield A("pool", lambda e: e.tensor_tensor(out=catT[:, 8 + p, col0:col0 + ncol], in0=e_km[i][:, N], in1=GG[q][:, N], op=ALU.mult), reads=K("e_km") + [f"GG{q}"], writes=["catT"])

            def run_il(gens):
                gens = list(gens)
                while gens:
                    for g_ in list(gens):
                        try:
                            next(g_)
                        except StopIteration:
                            gens.remove(g_)

            def rwkv_group(g, ncolt, full, valid):
                nsubs = (ncolt + TS - 1) // TS
                for sub in range(nsubs):
                    col0 = sub * TS
                    ncol = min(TS, ncolt - col0)
                    nch = ncol // CH
                    v = max(0, min(ncol, valid - col0))
                    run_il([pair_prep(4 * g + q, col0, ncol, nch, full, v) for q in range(4)])
                    for c in range(nch):
                        run_il([unit_local(4 * g + q, c, full) for q in range(4)])
                        for q in range(4):
                            chain_stage_w(4 * g + q, c, q)
                        for q in range(4):
                            chain_stage_u(4 * g + q, c)
                        if full:
                            for q in range(4):
                                chain_stage_y(4 * g + q, c, q)
                        for q in range(4):
                            chain_stage_s(4 * g + q, c)
                    if full:
                        run_il([rwkv_finish(4 * g + q, col0, ncol) for q in range(4)])

            def apply_flag_state():
                for p in range(8):
                    A("dve", lambda e, p=p: e.tensor_scalar(out=S32[p][:], in0=S32[p][:], scalar1=flag[:, 0:1], scalar2=None, op0=ALU.mult), reads=[f"S32_{p}", "flag"], writes=[f"S32_{p}"])
                    A("pool", lambda e, p=p: e.tensor_copy(out=Sbf[p][:], in_=S32[p][:]), reads=[f"S32_{p}"], writes=[f"Sbf_{p}"])
                A("dve", lambda e: e.tensor_scalar(out=carry[:], in0=carry[:], scalar1=flag[:, 0:1], scalar2=None, op0=ALU.mult), reads=["carry", "flag"], writes=["carry"])
                A("dve", lambda e: e.tensor_scalar(out=uhist[:].rearrange("p a b -> p (a b)"), in0=uhist[:].rearrange("p a b -> p (a b)"), scalar1=flag[:, 0:1], scalar2=None, op0=ALU.mult),
                  reads=["uhist", "flag"], writes=["uhist"])

            def out_conv(dst):
                for ci in range(8):
                    pn = nxt("pj", 2)
                    A("pe", lambda e, ci=ci, pn=pn: e.transpose(out=PS[pn][0:30, 0:128], in_=uhist[:, ci, :], identity=ident[:]), reads=["uhist", "ident"], writes=[pn])
                    A("act", lambda e, ci=ci, pn=pn: e.copy(out=cT8[:, ci * 128:(ci + 1) * 128], in_=PS[pn][0:30, 0:128]), reads=[pn], writes=["cT8"])
                A("sp", lambda e: e.dma_start(out=dst, in_=cT8[:]), reads=["cT8"], semkey="out")

            def out_wkv(dst):
                for p in range(8):
                    A("sp", lambda e, p=p: e.dma_start(out=dst[p, 0:64, :], in_=S32[p][0:64, 0:64]), reads=[f"S32_{p}"], semkey="out")
                    A("sp", lambda e, p=p: e.dma_start(out=dst[p, 64:128, :], in_=S32[p][64:128, 64:128]), reads=[f"S32_{p}"], semkey="out")

            def layer_norm(buf, bufk, gb, gbk):
                for q in range(4):
                    A("dve", lambda e, q=q: e.bn_stats(out=bst[:, q, :], in_=buf[:, q * 512:(q + 1) * 512]), reads=[bufk], writes=["bst"])
                A("dve", lambda e: e.bn_aggr(out=mv[:], in_=bst[:].rearrange("p a b -> p (a b)")), reads=["bst"], writes=["mv"])
                rsqrt(rstd[:], mv[:, 1:2], 0, ["mv"], ["rstd"])
                A("dve", lambda e: e.tensor_scalar(out=buf[:], in0=buf[:], scalar1=mv[:, 0:1], scalar2=rstd[:, 0:1], op0=ALU.subtract, op1=ALU.mult), reads=[bufk, "mv", "rstd"], writes=[bufk])
                A("pool", lambda e: e.tensor_tensor(out=buf[:], in0=buf[:], in1=gb[:, 0, :], op=ALU.mult), reads=[bufk, gbk], writes=[bufk])
                A("pool", lambda e: e.tensor_tensor(out=buf[:], in0=buf[:], in1=gb[:, 1, :], op=ALU.add), reads=[bufk, gbk], writes=[bufk])

            def front_sub(src_ap, rows, col0, ncolm, sidx):
                done_subs.append(sidx)
                load_x(src_ap, rows)
                if ncolm < 128:
                    pass
                for piece in range(16):
                    wi = wst_rot[0]; wst_rot[0] ^= 1
                    A("sp", lambda e, wi=wi, piece=piece: e.dma_start(out=wst[wi][:], in_=w_out_bf[piece].rearrange("p (k n) -> p k n", k=16)),
                      reads=["w_out_bf"], writes=[f"wst{wi}"], semkey=f"wst{wi}")
                    pn = nxt("pj", 2)
                    for kc in range(16):
                        A("pe", lambda e, kc=kc, pn=pn, wi=wi: e.matmul(PS[pn][0:ncolm, 0:128], lhsT=catT[:, kc, col0:col0 + ncolm], rhs=wst[wi][:, kc, :], start=(kc == 0), stop=(kc == 15)),
                          reads=["catT", f"wst{wi}"], writes=[pn])
                    A("dve", lambda e, pn=pn, piece=piece: e.scalar_tensor_tensor(out=xt[0:ncolm, piece * 128:(piece + 1) * 128], in0=xt[0:ncolm, piece * 128:(piece + 1) * 128], scalar=ALPHA,
                                                                                  in1=PS[pn][0:ncolm, 0:128], op0=ALU.mult, op1=ALU.add), reads=[pn, "xt"], writes=["xt"])
                layer_norm(xt, "xt", lnv, "lnv")
                A("sp", lambda e: e.dma_start(out=x1s[sidx * 128:(sidx + 1) * 128, :], in_=xt[:]), reads=["xt"], writes=["x1s"], semkey="x1s")
                if stage < 3:
                    return
                pl = nxt("m", 2)
                for g in range(4):
                    pn = nxt("pj", 2)
                    bi = g % 2
                    for j in range(4):
                        kc = 4 * g + j
                        A("pe", lambda e, pn=pn, j=j, kc=kc: e.transpose(out=PS[pn][:, j * 128:(j + 1) * 128], in_=xt[:, kc * 128:(kc + 1) * 128], identity=ident[:]), reads=["xt", "ident"], writes=[pn])
                    if g % 2 == 0:
                        A("act", lambda e, pn=pn, bi=bi: e.copy(out=x1Tb[bi][:], in_=PS[pn].rearrange("p (j t) -> p j t", j=4)), reads=[pn], writes=["x1Tb0"])
                    else:
                        A("dve", lambda e, pn=pn, bi=bi: e.tensor_copy(out=x1Tb[bi][:], in_=PS[pn].rearrange("p (j t) -> p j t", j=4)), reads=[pn], writes=["x1Tb0"])
                    for j in range(4):
                        kc = 4 * g + j
                        A("pe", lambda e, kc=kc, j=j, bi=bi: e.matmul(PS[pl][:, 0:NEXP], lhsT=x1Tb[bi][:, j, :], rhs=rw[:, kc, :], start=(kc == 0), stop=(kc == 15)), reads=["x1Tb0", "rw"], writes=[pl])
                A("dve", lambda e: e.tensor_tensor(out=lg[:], in0=PS[pl][:, 0:NEXP], in1=rb[:], op=ALU.add), reads=[pl, "rb"], writes=["lg"])
                A("dve", lambda e: e.max(out=m8[:], in_=lg[:]), reads=["lg"], writes=["m8"])
                A("dve", lambda e: e.tensor_scalar(out=negm[:], in0=m8[:, 0:1], scalar1=-1.0, scalar2=None, op0=ALU.mult), reads=["m8"], writes=["negm"])
                A("act", lambda e: e.activation(out=e4[:], in_=m8[:, 0:4], func=AF.Exp, bias=negm[:, 0:1], scale=1.0), reads=["m8", "negm"], writes=["e4"])
                A("dve", lambda e: e.tensor_reduce(out=s4[:], in_=e4[:], axis=mybir.AxisListType.X, op=ALU.add), reads=["e4"], writes=["s4"])
                A("dve", lambda e: e.reciprocal(out=s4[:], in_=s4[:]), reads=["s4"], writes=["s4"])
                A("dve", lambda e: e.tensor_scalar(out=g4[:], in0=e4[:], scalar1=s4[:, 0:1], scalar2=None, op0=ALU.mult), reads=["e4", "s4"], writes=["g4"])
                for k in range(4):
                    A("dve", lambda e, k=k: e.tensor_scalar(out=oh[:, k, :], in0=lg[:], scalar1=m8[:, k:k + 1], scalar2=None, op0=ALU.is_equal), reads=["lg", "m8"], writes=["oh"])
                A("dve", lambda e: e.tensor_tensor(out=msk[:], in0=oh[:, 0, :], in1=oh[:, 1, :], op=ALU.add), reads=["oh"], writes=["msk"])
                A("dve", lambda e: e.tensor_tensor(out=msk[:], in0=msk[:], in1=oh[:, 2, :], op=ALU.add), reads=["oh", "msk"], writes=["msk"])
                A("dve", lambda e: e.tensor_tensor(out=msk[:], in0=msk[:], in1=oh[:, 3, :], op=ALU.add), reads=["oh", "msk"], writes=["msk"])
                if ncolm < 128:
                    A("dve", lambda e: e.tensor_scalar(out=msk[:], in0=msk[:], scalar1=rvt[:, 0:1], scalar2=None, op0=ALU.mult), reads=["msk", "rvt"], writes=["msk"])
                A("pool", lambda e: e.tensor_copy(out=mskb[:], in_=msk[:]), reads=["msk"], writes=["mskb"])
                A("dve", lambda e: e.tensor_scalar(out=gd_all[:, sidx, :], in0=oh[:, 0, :], scalar1=g4[:, 0:1], scalar2=None, op0=ALU.mult), reads=["oh", "g4"], writes=["gd_all"])
                for k in range(1, 4):
                    A("dve", lambda e, k=k: e.scalar_tensor_tensor(out=gd_all[:, sidx, :], in0=oh[:, k, :], scalar=g4[:, k:k + 1], in1=gd_all[:, sidx, :], op0=ALU.mult, op1=ALU.add), reads=["oh", "g4", "gd_all"], writes=["gd_all"])
                pc = nxt("m", 2)
                A("pe", lambda e: e.matmul(PS[pc][:, 0:NEXP], lhsT=tri[:], rhs=mskb[:], start=True, stop=True), reads=["tri", "mskb"], writes=[pc])
                A("pe", lambda e: e.matmul(PS[pc][:, NEXP:2 * NEXP], lhsT=onesb[:], rhs=mskb[:], start=True, stop=True), reads=["onesb", "mskb"], writes=[pc])
                A("dve", lambda e: e.tensor_tensor(out=posf[:], in0=PS[pc][:, 0:NEXP], in1=cntbase[:], op=ALU.add), reads=[pc, "cntbase"], writes=["posf"])
                A("dve", lambda e: e.tensor_tensor(out=posf[:], in0=posf[:], in1=ecap[:], op=ALU.add), reads=["posf", "ecap"], writes=["posf"])
                A("dve", lambda e: e.tensor_tensor(out=cntbase[:], in0=PS[pc][:, NEXP:2 * NEXP], in1=cntbase[:], op=ALU.add), reads=[pc, "cntbase"], writes=["cntbase"])
                for k in range(4):
                    A("dve", lambda e, k=k: e.tensor_tensor(out=junk[:], in0=oh[:, k, :], in1=posf[:], op=ALU.mult), reads=["oh", "posf"], writes=["junk"])
                    A("dve", lambda e, k=k: e.tensor_reduce(out=pk[:, k:k + 1], in_=junk[:], axis=mybir.AxisListType.X, op=ALU.add), reads=["junk"], writes=["pk"])
                A("dve", lambda e: e.tensor_scalar(out=pk[:], in0=pk[:], scalar1=float(NROWS - 1), scalar2=0.0, op0=ALU.min, op1=ALU.max), reads=["pk"], writes=["pk"])
                A("dve", lambda e: e.tensor_copy(out=idx_all[:, sidx, :], in_=pk[:]), reads=["pk"], writes=["idx_all"])
                if ncolm < 128:
                    A("dve", lambda e: e.tensor_scalar(out=pk[:], in0=pk[:], scalar1=rvt[:, 1:2], scalar2=rvt[:, 0:1], op0=ALU.subtract, op1=ALU.mult), reads=["pk", "rvt"], writes=["pk"])
                    A("dve", lambda e: e.tensor_scalar(out=pk[:], in0=pk[:], scalar1=rvt[:, 1:2], scalar2=None, op0=ALU.add), reads=["pk", "rvt"], writes=["pk"])
                A("dve", lambda e: e.tensor_copy(out=idx_sc[:], in_=pk[:]), reads=["pk"], writes=["idx_sc"])
                for k in range(4):
                    xi = k % 2
                    if xi == 0:
                        A("act", lambda e, xi=xi: e.copy(out=xrow[xi][:, 0:D], in_=xt[:]), reads=["xt"], writes=[f"xrow{xi}"])
                    else:
                        A("pool", lambda e, xi=xi: e.tensor_copy(out=xrow[xi][:, 0:D], in_=xt[:]), reads=["xt"], writes=[f"xrow{xi}"])
                    A("dve", lambda e, k=k, xi=xi: e.tensor_copy(out=xrow[xi][:, D:D + 1], in_=g4[:, k:k + 1]), reads=["g4"], writes=[f"xrow{xi}"])
                    A("dve", lambda e, xi=xi: e.tensor_copy(out=negm[:], in_=xrow[xi][:, D:D + 1]), reads=[f"xrow{xi}"], writes=["negm"])
                    A("dve", lambda e, k=k, xi=xi: e.tensor_tensor(out=xrow[xi][:, D + 1:D + 2], in0=g4[:, k:k + 1], in1=negm[:], op=ALU.subtract), reads=["g4", "negm"], writes=[f"xrow{xi}"])
                    A("gq", lambda e, k=k, xi=xi: e.indirect_dma_start(out=xsorted, out_offset=bass.IndirectOffsetOnAxis(ap=idx_sc[:, k:k + 1], axis=0), in_=xrow[xi][:], in_offset=None),
                      reads=[f"xrow{xi}", "idx_sc"], writes=["xsorted"], semkey=f"sc{xi}")

            def z_slot_g(g):
                return lambda ct: ((ct - 16) // 8) * 4 + (ct - 16) % 8 - 4 * g

            def rwkv_tile(src, row0, ncol, nrows, full, valid):
                nsub = (ncol + 127) // 128
                for s in range(nsub):
                    r = min(128, nrows - s * 128)
                    load_x(src[row0 + s * 128: row0 + s * 128 + r, :], r)
                    transpose_x(s * 128, min(128, ncol - s * 128))
                zc_l = z_consume(ncol, valid, zl, "zl", lambda ct: ct - 40)
                if full:
                    proj_cols([40, 41], ncol, zc_l)
                    proj_cols([42], ncol, zc_l)
                else:
                    proj_cols([40], ncol, zc_l)
                lora_acts(ncol, full)
                for g in range(2):
                    zc = z_consume(ncol, valid, zg, "zg", z_slot_g(g))
                    kinds = (0, 1, 2) if full else (1, 2)
                    for kind in kinds:
                        base = 16 + 8 * kind + 4 * g
                        proj_cols([base, base + 1], ncol, zc)
                        proj_cols([base + 2, base + 3], ncol, zc)
                    rwkv_group(g, ncol, full, valid)

            def do_conv(ncol, valid):
                load_uhist()
                glu_proj(ncol)
                conv_block(ncol)
                save_uhist(valid)

            n_pre = NTILES
            if os.environ.get("MK_NPRE") is not None:
                n_pre = int(os.environ["MK_NPRE"])
            n_own = NTILES
            if os.environ.get("MK_NOWN") is not None:
                n_own = int(os.environ["MK_NOWN"])
            for ti in range(n_pre):
                rwkv_tile(xp, ti * T, T, T, False, T)
                if ti == n_pre - 1:
                    glu_proj(T)
                    save_uhist(T)
            apply_flag_state()
            for ti in range(n_own):
                rwkv_tile(xo, ti * T, T, T, True, T)
                do_conv(T, T)
                if stage >= 2:
                    for s in range(2):
                        front_sub(xo[ti * T + s * 128: ti * T + (s + 1) * 128, :], 128, s * 128, 128, ti * 2 + s)
            A("sp", lambda e: e.dma_start(out=shift_o, in_=carry[:]), reads=["carry"], semkey="out")
            out_conv(conv_o)
            out_wkv(wkv_o)
            P.flush()
            A("sp", lambda e: e.dma_start(out=carry[:], in_=sshT_d), writes=["carry"], semkey="ld0")
            A("sp", lambda e: e.dma_start(out=cT8[:], in_=sconv_d), writes=["cT8"], semkey="ld0")
            for ci in range(8):
                pn = nxt("pj", 2)
                A("pe", lambda e, ci=ci, pn=pn: e.transpose(out=PS[pn][:, 0:30], in_=cT8[0:30, ci * 128:(ci + 1) * 128], identity=ident[0:30, 0:30]), reads=["cT8", "ident"], writes=[pn])
                A("act", lambda e, ci=ci, pn=pn: e.copy(out=uhist[:, ci, :], in_=PS[pn][:, 0:30]), reads=[pn], writes=["uhist"])
            for p in range(8):
                A("sp", lambda e, p=p: e.dma_start(out=stmp[p % 2][:, 0:64], in_=swkv_d[p]), writes=[f"stmp{p % 2}"], semkey=f"stl{p % 2}")
                def fbd(e, p=p):
                    in0 = fap(stmp[p % 2][:, 0:64], [[0, 2], [1, 64]])
                    in1 = fap(maskh[:], [[1, 2], [0, 64]])
                    return e.tensor_tensor(out=S32[p][:].rearrange("p (h v) -> p h v", h=2), in0=in0, in1=in1, op=ALU.mult)
                A("dve", fbd, reads=[f"stmp{p % 2}", "maskh"], writes=[f"S32_{p}"])
                A("pool", lambda e, p=p: e.tensor_copy(out=Sbf[p][:], in_=S32[p][:]), reads=[f"S32_{p}"], writes=[f"Sbf_{p}"])
            rwkv_tile(xs, 0, CH, 16, True, 16)
            do_conv(CH, 16)
            if stage >= 2:
                front_sub(xs[0:16, :], 16, 0, CH, 32)
            A("sp", lambda e: e.dma_start(out=shift_so, in_=carry[:]), reads=["carry"], semkey="out")
            out_conv(conv_so)
            out_wkv(wkv_so)
            P.flush()

        if stage >= 4:
            NB = CAP // 128
            HALF = CAP // 2
            with ExitStack() as pb:
                bgu = sbt(pb, "bgu", [128, 2, NEXP, 16])
                A("sp", lambda e: e.dma_start(out=bgu[:], in_=bgu_d), writes=["bgu"], semkey="ld0")
                xs_t = [sbt(pb, f"xs_t{i}", [128, XROW], BF16) for i in range(2)]
                xsT = sbt(pb, "xsT", [128, 16, CAP], BF16)
                gate_r = sbt(pb, "gate_r", [128, NB])
                hT = sbt(pb, "hT", [128, 16, CAP], BF16)
                wgu = [sbt(pb, f"wgu{i}", [128, 2, 16, 256], BF16) for i in range(2)]
                wdn = [sbt(pb, f"wdn{i}", [128, 16, 512], BF16) for i in range(2)]
                gcl = [sbt(pb, f"gcl{i}", [128, HALF]) for i in range(2)]
                sgm = [sbt(pb, f"sgm{i}", [128, HALF]) for i in range(2)]
                ucl = [sbt(pb, f"ucl{i}", [128, HALF]) for i in range(2)]
                yo = [sbt(pb, f"yo{i}", [128, 512]) for i in range(4)]
                stg = [sbt(pb, f"stg{i}", [128, 16, 256]) for i in range(2)]
                stg_rot = [0]

                def load_w(dst_ap, dst_key, src_ap, cast_eng):
                    si = stg_rot[0]; stg_rot[0] ^= 1
                    A("sp", lambda e: e.dma_start(out=stg[si][:], in_=src_ap.rearrange("p (k n) -> p k n", k=16)), writes=[f"stg{si}"], semkey=f"stg{si}")
                    if cast_eng == "act":
                        A("act", lambda e: e.copy(out=dst_ap, in_=stg[si][:]), reads=[f"stg{si}"], writes=[dst_key])
                    else:
                        A(cast_eng, lambda e: e.tensor_copy(out=dst_ap, in_=stg[si][:]), reads=[f"stg{si}"], writes=[dst_key])
                n_exp = NEXP
                if os.environ.get("MK_NEXP") is not None:
                    n_exp = int(os.environ["MK_NEXP"])
                for ex in range(n_exp):
                    for blk in range(NB):
                        xi = blk % 2
                        r0 = ex * CAP + blk * 128
                        A("sp", lambda e, xi=xi, r0=r0: e.dma_start(out=xs_t[xi][:], in_=xsorted[r0:r0 + 128, :]), reads=["xsorted"], writes=[f"xs_t{xi}"], semkey=f"xsl{xi}")
                        A("pool", lambda e, xi=xi, blk=blk: e.tensor_tensor(out=gate_r[:, blk:blk + 1], in0=xs_t[xi][:, D:D + 1], in1=xs_t[xi][:, D + 1:D + 2], op=ALU.add), reads=[f"xs_t{xi}"], writes=["gate_r"])
                        for g in range(4):
                            pt = nxt("m", 2)
                            ptb = PS[pt].bitcast(BF16)
                            for j in range(4):
                                kc = 4 * g + j
                                A("pe", lambda e, xi=xi, kc=kc, j=j, ptb=ptb: e.transpose(out=ptb[:, j * 128:(j + 1) * 128], in_=xs_t[xi][:, kc * 128:(kc + 1) * 128], identity=identb[:]),
                                  reads=[f"xs_t{xi}", "identb"], writes=[pt])
                            if g % 2 == 0:
                                A("act", lambda e, g=g, blk=blk, ptb=ptb: e.copy(out=xsT[:, 4 * g:4 * g + 4, blk * 128:(blk + 1) * 128], in_=ptb[:, 0:512].rearrange("p (j t) -> p j t", j=4)), reads=[pt], writes=["xsT"])
                            else:
                                A("dve", lambda e, g=g, blk=blk, ptb=ptb: e.tensor_copy(out=xsT[:, 4 * g:4 * g + 4, blk * 128:(blk + 1) * 128], in_=ptb[:, 0:512].rearrange("p (j t) -> p j t", j=4)), reads=[pt], writes=["xsT"])
                    for fg in range(8):
                        wi = fg % 2
                        load_w(wgu[wi][:, 0, :, :], f"wgu{wi}", w_gate[ex, fg], "act")
                        load_w(wgu[wi][:, 1, :, :], f"wgu{wi}", w_up[ex, fg], "act")
                        for fl in range(2):
                            ft = fg * 2 + fl
                            for hf in range(2):
                                pg_ = nxt("pj", 2)
                                pu_ = nxt("fb", 2)
                                rs = slice(hf * HALF, (hf + 1) * HALF)
                                for kc in range(16):
                                    A("pe", lambda e, kc=kc, pg_=pg_, wi=wi, fl=fl, rs=rs: e.matmul(PS[pg_][:, 0:HALF], lhsT=wgu[wi][:, 0, kc, fl * 128:(fl + 1) * 128], rhs=xsT[:, kc, rs], start=(kc == 0), stop=(kc == 15)),
                                      reads=[f"wgu{wi}", "xsT"], writes=[pg_])
                                for kc in range(16):
                                    A("pe", lambda e, kc=kc, pu_=pu_, wi=wi, fl=fl, rs=rs: e.matmul(PS[pu_][:, 0:HALF], lhsT=wgu[wi][:, 1, kc, fl * 128:(fl + 1) * 128], rhs=xsT[:, kc, rs], start=(kc == 0), stop=(kc == 15)),
                                      reads=[f"wgu{wi}", "xsT"], writes=[pu_])
                                bi = hf
                                A("dve", lambda e, pg_=pg_, bi=bi, ft=ft, ex=ex: e.tensor_scalar(out=gcl[bi][:], in0=PS[pg_][:, 0:HALF], scalar1=bgu[:, 0, ex, ft:ft + 1], scalar2=7.0, op0=ALU.add, op1=ALU.min),
                                  reads=[pg_, "bgu"], writes=[f"gcl{bi}"])
                                A("act", lambda e, bi=bi: e.activation(out=sgm[bi][:], in_=gcl[bi][:], func=AF.Sigmoid, scale=1.702), reads=[f"gcl{bi}"], writes=[f"sgm{bi}"])
                                A("dve", lambda e, pu_=pu_, bi=bi, ft=ft, ex=ex: e.tensor_scalar(out=ucl[bi][:], in0=PS[pu_][:, 0:HALF], scalar1=bgu[:, 1, ex, ft:ft + 1], scalar2=7.0, op0=ALU.add, op1=ALU.min),
                                  reads=[pu_, "bgu"], writes=[f"ucl{bi}"])
                                A("pool", lambda e, bi=bi: e.tensor_scalar(out=ucl[bi][:], in0=ucl[bi][:], scalar1=-7.0, scalar2=1.0, op0=ALU.max, op1=ALU.add), reads=[f"ucl{bi}"], writes=[f"ucl{bi}"])
                                A("pool", lambda e, bi=bi: e.tensor_tensor(out=gcl[bi][:], in0=gcl[bi][:], in1=sgm[bi][:], op=ALU.mult), reads=[f"gcl{bi}", f"sgm{bi}"], writes=[f"gcl{bi}"])
                                A("dve", lambda e, bi=bi, ft=ft, rs=rs: e.tensor_tensor(out=hT[:, ft, rs], in0=gcl[bi][:], in1=ucl[bi][:], op=ALU.mult), reads=[f"gcl{bi}", f"ucl{bi}"], writes=["hT"])
                    for ct in range(4):
                        wi = ct % 2
                        load_w(wdn[wi][:, :, 0:256], f"wdn{wi}", w_down[ex, 2 * ct], "pool")
                        load_w(wdn[wi][:, :, 256:512], f"wdn{wi}", w_down[ex, 2 * ct + 1], "act")
                        for blk in range(NB):
                            pd = nxt("c", 2)
                            for fc in range(16):
                                A("pe", lambda e, fc=fc, pd=pd, blk=blk, wi=wi: e.matmul(PS[pd][:], lhsT=hT[:, fc, blk * 128:(blk + 1) * 128], rhs=wdn[wi][:, fc, :], start=(fc == 0), stop=(fc == 15)),
                                  reads=["hT", f"wdn{wi}"], writes=[pd])
                            yi = int(nxt("yo", 4)[2:])
                            r0 = ex * CAP + blk * 128
                            if yi % 2 == 0:
                                A("act", lambda e, pd=pd, blk=blk, yi=yi: e.activation(out=yo[yi][:], in_=PS[pd][:], func=AF.Copy, scale=gate_r[:, blk:blk + 1]), reads=[pd, "gate_r"], writes=[f"yo{yi}"])
                            else:
                                A("dve", lambda e, pd=pd, blk=blk, yi=yi: e.tensor_scalar(out=yo[yi][:], in0=PS[pd][:], scalar1=gate_r[:, blk:blk + 1], scalar2=None, op0=ALU.mult), reads=[pd, "gate_r"], writes=[f"yo{yi}"])
                            A("sp", lambda e, yi=yi, r0=r0, ct=ct: e.dma_start(out=ysorted[r0:r0 + 128, ct * 512:(ct + 1) * 512], in_=yo[yi][:]), reads=[f"yo{yi}"], writes=["ysorted"], semkey=f"yst{yi}")
                P.flush()
            with ExitStack() as pc_:
                bdn = sbt(pc_, "bdn", [NEXP, D])
                A("sp", lambda e: e.dma_start(out=bdn[:], in_=bdn_d), writes=["bdn"], semkey="ld0")
                lnv2 = sbt(pc_, "lnv2", [128, 2, D])
                A("sp", lambda e: e.dma_start(out=lnv2[:, 0, :], in_=lnv_d[2:3, :].partition_broadcast(128)), writes=["lnv2"], semkey="ld0")
                A("sp", lambda e: e.dma_start(out=lnv2[:, 1, :], in_=lnv_d[3:4, :].partition_broadcast(128)), writes=["lnv2"], semkey="ld0")
                Gk = [sbt(pc_, f"Gk{i}", [128, D]) for i in range(8)]
                xr = [sbt(pc_, f"xr{i}", [128, D]) for i in range(2)]
                gTt = [sbt(pc_, f"gTt{i}", [NEXP, 128]) for i in range(2)]
                bst2 = sbt(pc_, "bst2", [128, 4, 6]); mv2 = sbt(pc_, "mv2", [128, 2]); rstd2 = sbt(pc_, "rstd2", [128, 1])
                for sidx in done_subs:
                    bi = sidx % 2
                    X, Xk = xr[bi], f"xr{bi}"
                    A("sp", lambda e, X=X, sidx=sidx: e.dma_start(out=X[:], in_=x1s[sidx * 128:(sidx + 1) * 128, :]), reads=["x1s"], writes=[Xk], semkey=f"xr{bi}")
                    for k in range(4):
                        gi = bi * 4 + k
                        A("gq", lambda e, gi=gi, k=k, sidx=sidx: e.indirect_dma_start(out=Gk[gi][:], out_offset=None, in_=ysorted,
                                                                                      in_offset=bass.IndirectOffsetOnAxis(ap=idx_all[:, sidx, k:k + 1], axis=0)),
                          reads=["ysorted", "idx_all"], writes=[f"Gk{gi}"], semkey=f"gk{gi}")
                    pg = nxt("pj", 2)
                    A("pe", lambda e, pg=pg, sidx=sidx: e.transpose(out=PS[pg][0:NEXP, 0:128], in_=gd_all[:, sidx, :], identity=ident[:]), reads=["gd_all", "ident"], writes=[pg])
                    A("act", lambda e, pg=pg, bi=bi: e.copy(out=gTt[bi][:], in_=PS[pg][0:NEXP, 0:128]), reads=[pg], writes=[f"gTt{bi}"])
                    A("dve", lambda e, X=X, bi=bi: e.scalar_tensor_tensor(out=X[:], in0=X[:], scalar=ALPHA, in1=Gk[bi * 4][:], op0=ALU.mult, op1=ALU.add), reads=[Xk, f"Gk{bi * 4}"], writes=[Xk])
                    A("pool", lambda e, bi=bi: e.tensor_tensor(out=Gk[bi * 4 + 1][:], in0=Gk[bi * 4 + 1][:], in1=Gk[bi * 4 + 2][:], op=ALU.add), reads=[f"Gk{bi * 4 + 1}", f"Gk{bi * 4 + 2}"], writes=[f"Gk{bi * 4 + 1}"])
                    A("pool", lambda e, bi=bi: e.tensor_tensor(out=Gk[bi * 4 + 1][:], in0=Gk[bi * 4 + 1][:], in1=Gk[bi * 4 + 3][:], op=ALU.add), reads=[f"Gk{bi * 4 + 1}", f"Gk{bi * 4 + 3}"], writes=[f"Gk{bi * 4 + 1}"])
                    A("dve", lambda e, X=X, bi=bi: e.tensor_tensor(out=X[:], in0=X[:], in1=Gk[bi * 4 + 1][:], op=ALU.add), reads=[Xk, f"Gk{bi * 4 + 1}"], writes=[Xk])
                    for ct in range(4):
                        pb_ = nxt("fb", 2)
                        A("pe", lambda e, pb_=pb_, ct=ct, bi=bi: e.matmul(PS[pb_][:], lhsT=gTt[bi][:], rhs=bdn[:, ct * 512:(ct + 1) * 512], start=True, stop=True), reads=[f"gTt{bi}", "bdn"], writes=[pb_])
                        A("dve", lambda e, pb_=pb_, ct=ct, X=X: e.tensor_tensor(out=X[:, ct * 512:(ct + 1) * 512], in0=X[:, ct * 512:(ct + 1) * 512], in1=PS[pb_][:], op=ALU.add), reads=[pb_, Xk], writes=[Xk])
                    for q in range(4):
                        A("dve", lambda e, q=q, X=X: e.bn_stats(out=bst2[:, q, :], in_=X[:, q * 512:(q + 1) * 512]), reads=[Xk], writes=["bst2"])
                    A("dve", lambda e: e.bn_aggr(out=mv2[:], in_=bst2[:].rearrange("p a b -> p (a b)")), reads=["bst2"], writes=["mv2"])
                    rsqrt(rstd2[:], mv2[:, 1:2], 0, ["mv2"], ["rstd2"])
                    A("dve", lambda e, X=X: e.tensor_scalar(out=X[:], in0=X[:], scalar1=mv2[:, 0:1], scalar2=rstd2[:, 0:1], op0=ALU.subtract, op1=ALU.mult), reads=[Xk, "mv2", "rstd2"], writes=[Xk])
                    A("pool", lambda e, X=X: e.tensor_tensor(out=X[:], in0=X[:], in1=lnv2[:, 0, :], op=ALU.mult), reads=[Xk, "lnv2"], writes=[Xk])
                    A("pool", lambda e, X=X: e.tensor_tensor(out=X[:], in0=X[:], in1=lnv2[:, 1, :], op=ALU.add), reads=[Xk, "lnv2"], writes=[Xk])
                    A("sp", lambda e, X=X, sidx=sidx: e.dma_start(out=y_own[sidx * 128:(sidx + 1) * 128, :], in_=X[:]), reads=[Xk], semkey="out")
                P.flush()
        P.finish()
    return nc


def _consts():
    p = np.arange(128)
    h, t = p // 64, p % 64
    same = (h[:, None] == h[None, :])
    c = {}
    c["ident"] = np.eye(128, dtype=np.float32)
    c["maskh"] = (h[:, None] == np.arange(2)[None, :]).astype(np.float32)
    nt = same & (t[:, None] > t[None, :])
    n_ = same & (t[:, None] < t[None, :])
    c["m1"] = np.concatenate([nt, n_], axis=1).astype(np.float32)
    incl = (t[:, None] <= np.arange(64)[None, :])
    c["m2"] = np.concatenate([incl, n_, incl], axis=1).astype(np.float32)
    c["onesbd"] = same.astype(np.float32)
    c["tri"] = (p[:, None] < p[None, :]).astype(np.float32)
    c["rvt"] = np.stack([(p < 16).astype(np.float32), (NROWS + p).astype(np.float32)], axis=1)
    c["ecap"] = np.broadcast_to((np.arange(NEXP) * CAP).astype(np.float32)[None, :], (128, NEXP)).copy()
    return c


def _colmajor(v, ntile):
    out = np.zeros((ntile * 128,), np.float32)
    out[:v.shape[0]] = v
    return np.ascontiguousarray(out.reshape(ntile, 128).T)


def _shared_inputs(inp, stage):
    g = lambda k: np.asarray(inp[k], dtype=np.float32)[0]
    sh = dict(_consts())
    wi_ = np.zeros((D, 43 * 128), np.float32)
    wi_[:, :P_IN] = g("w_in")
    sh["w_in"] = np.ascontiguousarray(wi_.reshape(16, 128, 43, 128).transpose(2, 1, 0, 3)).reshape(43, 128, D)
    sh["binT"] = _colmajor(g("b_in"), 43)
    sh["muT"] = _colmajor(g("mu_shift"), 27)
    sh["cwT"] = np.ascontiguousarray(g("conv_w").reshape(31, 8, 128).transpose(2, 1, 0))
    sh["cvec"] = np.ascontiguousarray(np.stack([_colmajor(g("conv_b"), 8), _colmajor(g("conv_ln_g"), 8), _colmajor(g("conv_ln_b"), 8)], axis=1))
    pv = [g("rwkv_w0"), g("rwkv_a0"), g("rwkv_k_k"), g("rwkv_k_a"), g("rwkv_r_k").reshape(-1), g("rwkv_ln_g"), g("rwkv_ln_b")]
    sh["pvec"] = np.ascontiguousarray(np.stack([_colmajor(v, 8) for v in pv], axis=1))
    sh["w2"] = g("rwkv_w2"); sh["a2"] = g("rwkv_a2"); sh["g2"] = g("rwkv_g2")
    sh["w_out"] = np.ascontiguousarray(g("w_out").reshape(16, 128, 16, 128).transpose(2, 1, 0, 3)).reshape(16, 128, D)
    sh["lnv"] = np.ascontiguousarray(np.stack([g("ln1_g"), g("ln1_b"), g("ln2_g"), g("ln2_b")], axis=0))
    sh["rw"] = g("router_w"); sh["rb"] = g("router_b").reshape(1, NEXP)
    if stage >= 4:
        lay = lambda w: np.ascontiguousarray(w.reshape(NEXP, 16, 128, 8, 256).transpose(0, 3, 2, 1, 4)).reshape(NEXP, 8, 128, 4096)
        sh["w_gate"] = lay(g("w_gate")); sh["w_up"] = lay(g("w_up")); sh["w_down"] = lay(g("w_down"))
        bg = g("b_gate").reshape(NEXP, 16, 128).transpose(2, 0, 1)
        bu = g("b_up").reshape(NEXP, 16, 128).transpose(2, 0, 1)
        sh["bgu"] = np.ascontiguousarray(np.stack([bg, bu], axis=1))
        sh["bdn"] = g("b_down")
    return sh


def _core_inputs(c, inp, sh):
    b, half = c // 2, c % 2
    xpr = np.asarray(inp["x_prompt"], dtype=np.float32)
    m = dict(sh)
    m["xo"] = np.ascontiguousarray(xpr[b, half * NOWN:(half + 1) * NOWN])
    m["xp"] = np.ascontiguousarray(xpr[b, 0:NOWN])
    m["xs"] = np.ascontiguousarray(np.asarray(inp["x_sample"], dtype=np.float32)[c])
    m["flag"] = np.full((128, 1), float(half), np.float32)
    m["sconv"] = np.ascontiguousarray(np.asarray(inp["state_conv"], dtype=np.float32)[0, c])
    m["sshT"] = _colmajor(np.asarray(inp["state_shift"], dtype=np.float32)[0, c, 0], 27)
    sw = np.asarray(inp["state_wkv"], dtype=np.float32)[0, c]
    m["swkv"] = np.ascontiguousarray(sw.reshape(8, 2, 64, 64).transpose(0, 1, 3, 2).reshape(8, 128, 64))
    return m


_NC_CACHE = {}


def run_cores(inp, stage=99):
    if stage not in _NC_CACHE:
        _NC_CACHE[stage] = build_nc(stage)
    nc = _NC_CACHE[stage]
    sh = _shared_inputs(inp, stage)
    in_maps = [_core_inputs(c, inp, sh) for c in range(8)]
    res = run_bass_kernel_spmd(nc, in_maps, core_ids=list(range(8)))
    return res.results


def assemble(rs):
    y_p = np.zeros((4, 8192, D), np.float32); y_s = np.zeros((8, 16, D), np.float32)
    conv_p = np.zeros((1, 4, 30, C_CONV), np.float32); shift_p = np.zeros((1, 4, 1, NSH), np.float32); wkv_p = np.zeros((1, 4, 16, 64, 64), np.float32)
    conv_s = np.zeros((1, 8, 30, C_CONV), np.float32); shift_s = np.zeros((1, 8, 1, NSH), np.float32); wkv_s = np.zeros((1, 8, 16, 64, 64), np.float32)
    unshift = lambda a: np.ascontiguousarray(a.T).reshape(-1)[:NSH]
    unwkv = lambda a: a.reshape(8, 2, 64, 64).transpose(0, 1, 3, 2).reshape(16, 64, 64)
    for c in range(8):
        r = rs[c]
        b, half = c // 2, c % 2
        y_p[b, half * NOWN:(half + 1) * NOWN] = r["y_own"][0:NOWN]
        y_s[c] = r["y_own"][NOWN:NOWN + 16]
        if half == 1:
            conv_p[0, b] = r["conv_o"]; shift_p[0, b, 0] = unshift(r["shift_o"]); wkv_p[0, b] = unwkv(r["wkv_o"])
        conv_s[0, c] = r["conv_so"]; shift_s[0, c, 0] = unshift(r["shift_so"]); wkv_s[0, c] = unwkv(r["wkv_so"])
    return (y_p, y_s, conv_p, shift_p, wkv_p, conv_s, shift_s, wkv_s)


def kernel(**inputs):
    return assemble(run_cores(inputs, 99))
```

```python
import os
import numpy as np
from contextlib import ExitStack
import concourse.bass as bass
import concourse.mybir as mybir
from concourse.bass_utils import run_bass_kernel_spmd

F32 = mybir.dt.float32
BF16 = mybir.dt.bfloat16
I32 = mybir.dt.int32
AF = mybir.ActivationFunctionType
ALU = mybir.AluOpType

D = 2048
C_CONV = 1024
NSH = 3360
P_IN = 5408
NEXP = 32
ALPHA = 2.0 ** 0.25
LN_EPS = 1e-5
GN_EPS = 64e-5
DEC = 0.6065306597126334
CH = 64
T = 256
TS = 128
NOWN = 4096
NTILES = NOWN // T
CAP = 768
NROWS = NEXP * CAP
NSUB = 33
XROW = 2050


class _Op:
    __slots__ = ("eng", "fn", "reads", "writes", "semkey", "idx", "waits", "inc", "tick", "is_dma")


class Prog:
    ISSUE = {"pe": "pe", "act": "act", "dve": "dve", "pool": "pool", "sp": "sp", "aq": "act", "gq": "pool"}
    ENG = {"pe": "tensor", "act": "scalar", "dve": "vector", "pool": "gpsimd", "sp": "sync"}

    def __init__(self, nc, stack):
        self.nc = nc
        self.stack = stack
        self.ops = []
        self.last_w = {}
        self.readers = {}
        self.cnt = {}
        self.waited = {}
        self.sems = {}
        self.pending_barrier = None
        self.barrier_done = {}
        self.n_emitted = 0

    def add(self, eng, fn, reads=(), writes=(), semkey=None):
        op = _Op()
        op.eng = eng
        op.fn = fn
        op.reads = tuple(reads)
        op.writes = tuple(writes)
        op.is_dma = eng in ("sp", "aq", "gq")
        op.semkey = semkey if semkey is not None else (("dma_" + eng) if op.is_dma else None)
        op.waits = {}
        op.inc = False
        op.tick = 0
        op.idx = len(self.ops)
        self.ops.append(op)
        return op

    def _chan(self, op):
        return op.semkey if op.is_dma else op.eng

    def _sem(self, c):
        if c not in self.sems:
            self.sems[c] = self.stack.enter_context(self.nc.semaphore("s_" + str(c)))
        return self.sems[c]

    def flush(self):
        nc = self.nc
        ops = self.ops
        if not ops:
            return
        n = len(ops)
        deps = [None] * n
        last_w, readers = {}, {}
        for op in ops:
            d = set()
            for k in op.reads:
                if k in last_w:
                    d.add(last_w[k])
            for k in op.writes:
                if k in last_w:
                    d.add(last_w[k])
                for r in readers.get(k, ()):
                    d.add(r)
            d.discard(op.idx)
            best = {}
            keep = set()
            for di in d:
                dop = ops[di]
                if dop.is_dma:
                    keep.add(di)
                else:
                    if dop.eng not in best or di > best[dop.eng]:
                        best[dop.eng] = di
            keep.update(best.values())
            deps[op.idx] = keep
            for k in op.reads:
                readers.setdefault(k, []).append(op.idx)
            for k in op.writes:
                last_w[k] = op.idx
                readers[k] = []

        def skip(dop, op):
            return (not dop.is_dma) and (not op.is_dma) and dop.eng == "pe" and op.eng == "pe"

        needed = [False] * n
        for op in ops:
            for d in deps[op.idx]:
                if not skip(ops[d], op):
                    needed[d] = True
        lastc = {}
        for op in ops:
            if not op.is_dma:
                lastc[op.eng] = op.idx
        for e, i in lastc.items():
            needed[i] = True
        for op in ops:
            if op.is_dma or needed[op.idx]:
                c = self._chan(op)
                self.cnt[c] = self.cnt.get(c, 0) + (16 if op.is_dma else 1)
                op.tick = self.cnt[c]
                op.inc = True
                self._sem(c)
        bar = self.pending_barrier
        grp_final = {}
        for op in ops:
            if op.is_dma and str(op.semkey).startswith("ld"):
                grp_final[op.semkey] = op.tick
        streams = {"pe": [], "act": [], "dve": [], "pool": [], "sp": []}
        for op in ops:
            ie = self.ISSUE[op.eng]
            w = {}
            for d in deps[op.idx]:
                dop = ops[d]
                if skip(dop, op):
                    continue
                c = self._chan(dop)
                if dop.is_dma and c in grp_final and op.is_dma and str(op.semkey).startswith("ld"):
                    continue
                w[c] = max(w.get(c, 0), grp_final.get(c, dop.tick) if dop.is_dma else dop.tick)
            if bar is not None and not self.barrier_done.get(ie, False):
                for c, t in bar.items():
                    w[c] = max(w.get(c, 0), t)
                self.barrier_done[ie] = True
            for c, t in list(w.items()):
                if self.waited.get((ie, c), 0) >= t:
                    del w[c]
                else:
                    self.waited[(ie, c)] = t
            op.waits = w
            streams[ie].append(op)
        sems = self.sems
        chan = self._chan
        with nc.Block() as block:
            def make(lst):
                def body(eng):
                    for op in lst:
                        for c, t in op.waits.items():
                            eng.wait_ge(sems[c], t)
                        ins = op.fn(eng)
                        if op.inc:
                            ins.then_inc(sems[chan(op)], 16 if op.is_dma else 1)
                return body
            for ename, lst in streams.items():
                if lst:
                    getattr(block, self.ENG[ename])(make(lst))
        self.n_emitted += n
        self.ops = []
        self.pending_barrier = dict(self.cnt)
        self.barrier_done = {}

    def finish(self):
        self.flush()
        nc = self.nc
        cnt = dict(self.cnt)
        sems = self.sems
        with nc.Block() as block:
            @block.sync
            def _(eng):
                for c, t in cnt.items():
                    eng.wait_ge(sems[c], t)


def fap(base, dims, off=0):
    return bass.AP(base.tensor, base.offset + off, [list(base.ap[0])] + [list(d) for d in dims])


def build_nc(stage=99):
    nc = bass.Bass("TRN2", target_bir_lowering=False)
    dI = lambda n, s, dt=F32: nc.dram_tensor(n, list(s), dt, kind="ExternalInput").ap()
    dO = lambda n, s, dt=F32: nc.dram_tensor(n, list(s), dt, kind="ExternalOutput").ap()
    dS = lambda n, s, dt=F32: nc.dram_tensor(n, list(s), dt, kind="Internal").ap()
    xo = dI("xo", [NOWN, D]); xp = dI("xp", [NOWN, D]); xs = dI("xs", [16, D]); flag_d = dI("flag", [128, 1])
    w_in = dI("w_in", [43, 128, D]); binT_d = dI("binT", [128, 43]); muT_d = dI("muT", [128, 27])
    cwT_d = dI("cwT", [128, 8, 31]); cvec_d = dI("cvec", [128, 3, 8])
    pvec_d = dI("pvec", [128, 7, 8])
    w2_d = dI("w2", [64, 1024]); a2_d = dI("a2", [64, 1024]); g2_d = dI("g2", [160, 1024])
    w_out = dI("w_out", [16, 128, D]); lnv_d = dI("lnv", [4, D])
    rw_d = dI("rw", [D, NEXP]); rb_d = dI("rb", [1, NEXP])
    if stage >= 4:
        w_gate = dI("w_gate", [NEXP, 8, 128, 4096]); w_up = dI("w_up", [NEXP, 8, 128, 4096]); w_down = dI("w_down", [NEXP, 8, 128, 4096])
        bgu_d = dI("bgu", [128, 2, NEXP, 16]); bdn_d = dI("bdn", [NEXP, D])
    sconv_d = dI("sconv", [30, C_CONV]); sshT_d = dI("sshT", [128, 27]); swkv_d = dI("swkv", [8, 128, 64])
    ident_d = dI("ident", [128, 128]); maskh_d = dI("maskh", [128, 2]); m1_d = dI("m1", [128, 256]); m2_d = dI("m2", [128, 256])
    onesbd_d = dI("onesbd", [128, 128]); tri_d = dI("tri", [128, 128]); ecap_d = dI("ecap", [128, NEXP]); rvt_d = dI("rvt", [128, 2])
    y_own = dO("y_own", [NSUB * 128, D])
    conv_o = dO("conv_o", [30, C_CONV]); conv_so = dO("conv_so", [30, C_CONV])
    shift_o = dO("shift_o", [128, 27]); shift_so = dO("shift_so", [128, 27])
    wkv_o = dO("wkv_o", [8, 128, 64]); wkv_so = dO("wkv_so", [8, 128, 64])
    x1s = dS("x1s", [NSUB * 128, D])
    xsorted = dS("xsorted", [NROWS + 128, XROW], BF16)
    ysorted = dS("ysorted", [NROWS, D])

    with ExitStack() as top:
        P = Prog(nc, top)
        A = P.add
        sbt = lambda st, n, s, dt=F32: st.enter_context(nc.sbuf_tensor("sb_" + n, list(s), dt))
        pbank = [top.enter_context(nc.psum_tensor(f"pb{i}", [128, 512], F32)) for i in range(8)]
        PS = {}
        for i_, n_ in enumerate(("pj0", "pj1", "m0", "m1", "fb0", "fb1", "c0", "c1")):
            PS[n_] = pbank[i_][:, :]
        rot = {}
        done_subs = []

        def nxt(prefix, n):
            i = rot.get(prefix, 0)
            rot[prefix] = (i + 1) % n
            return f"{prefix}{i}"

        cst = top
        ident = sbt(cst, "ident", [128, 128]); identb = sbt(cst, "identb", [128, 128], BF16)
        maskh = sbt(cst, "maskh", [128, 2]); m1 = sbt(cst, "m1", [128, 256]); m2 = sbt(cst, "m2", [128, 256])
        onesbd = sbt(cst, "onesbd", [128, 128]); flag = sbt(cst, "flag", [128, 1])
        idx_all = sbt(top, "idx_all", [128, NSUB, 4], I32)
        rvt = sbt(top, "rvt", [128, 2])
        A("sp", lambda e: e.dma_start(out=rvt[:], in_=rvt_d), writes=["rvt"], semkey="ld0")
        epsc = sbt(top, "epsc", [128, 3])
        A("pool", lambda e: e.memset(epsc[:, 0:1], LN_EPS), writes=["epsc"])
        A("pool", lambda e: e.memset(epsc[:, 1:2], GN_EPS), writes=["epsc"])
        A("pool", lambda e: e.memset(epsc[:, 2:3], 1e-24), writes=["epsc"])

        def rsqrt(dst, src, col, rk, wk):
            A("act", lambda e: e.activation(out=dst, in_=src, func=AF.Sqrt, bias=epsc[0:dst.shape[0], col:col + 1], scale=1.0), reads=rk + ["epsc"], writes=wk)
            A("dve", lambda e: e.reciprocal(out=dst, in_=dst), reads=wk, writes=wk)
        gd_all = sbt(top, "gd_all", [128, NSUB, NEXP])
        A("sp", lambda e: e.dma_start(out=ident[:], in_=ident_d), writes=["ident"], semkey="ld0")
        A("gq", lambda e: e.dma_start(out=identb[:], in_=ident_d), writes=["identb"], semkey="ld1")
        A("sp", lambda e: e.dma_start(out=maskh[:], in_=maskh_d), writes=["maskh"], semkey="ld0")
        A("sp", lambda e: e.dma_start(out=m1[:], in_=m1_d), writes=["m1"], semkey="ld0")
        A("sp", lambda e: e.dma_start(out=m2[:], in_=m2_d), writes=["m2"], semkey="ld0")
        A("sp", lambda e: e.dma_start(out=onesbd[:], in_=onesbd_d), writes=["onesbd"], semkey="ld0")
        A("sp", lambda e: e.dma_start(out=flag[:], in_=flag_d), writes=["flag"], semkey="ld0")

        w_in_bf = dS("w_in_bf", [43, 128, D], BF16)
        w_out_bf = dS("w_out_bf", [16, 128, D], BF16)
        for q in range(43):
            A("gq", lambda e, q=q: e.dma_start(out=w_in_bf[q], in_=w_in[q]), writes=["w_in_bf"], semkey="ld1")
        for q in range(16):
            A("gq", lambda e, q=q: e.dma_start(out=w_out_bf[q], in_=w_out[q]), writes=["w_out_bf"], semkey="ld1")
        with ExitStack() as pa:
            binT = sbt(pa, "binT", [128, 43]); muT = sbt(pa, "muT", [128, 27])
            cwT = sbt(pa, "cwT", [128, 8, 31]); cvec = sbt(pa, "cvec", [128, 3, 8]); pvec = sbt(pa, "pvec", [128, 7, 8])
            w2a2 = sbt(pa, "w2a2", [128, 1024]); g2a = sbt(pa, "g2a", [128, 1024]); g2b = sbt(pa, "g2b", [32, 1024])
            for (t_, d_, k_) in ((binT, binT_d, "binT"), (muT, muT_d, "muT"), (cwT, cwT_d, "cwT"), (cvec, cvec_d, "cvec"), (pvec, pvec_d, "pvec")):
                A("sp", lambda e, t_=t_, d_=d_: e.dma_start(out=t_[:], in_=d_), writes=[k_], semkey="ld0")
            A("sp", lambda e: e.dma_start(out=w2a2[0:64, :], in_=w2_d), writes=["w2a2"], semkey="ld0")
            A("sp", lambda e: e.dma_start(out=w2a2[64:128, :], in_=a2_d), writes=["w2a2"], semkey="ld0")
            A("sp", lambda e: e.dma_start(out=g2a[:], in_=g2_d[0:128, :]), writes=["g2a"], semkey="ld0")
            A("sp", lambda e: e.dma_start(out=g2b[:], in_=g2_d[128:160, :]), writes=["g2b"], semkey="ld0")
            lnv = sbt(pa, "lnv", [128, 2, D])
            A("sp", lambda e: e.dma_start(out=lnv[:, 0, :], in_=lnv_d[0:1, :].partition_broadcast(128)), writes=["lnv"], semkey="ld0")
            A("sp", lambda e: e.dma_start(out=lnv[:, 1, :], in_=lnv_d[1:2, :].partition_broadcast(128)), writes=["lnv"], semkey="ld0")
            rw = sbt(pa, "rw", [128, 16, NEXP]); rb = sbt(pa, "rb", [128, NEXP]); ecap = sbt(pa, "ecap", [128, NEXP])
            tri = sbt(pa, "tri", [128, 128], BF16); onesb = sbt(pa, "onesb", [128, 128], BF16)
            A("sp", lambda e: e.dma_start(out=rw[:], in_=rw_d.rearrange("(k p) n -> p k n", p=128)), writes=["rw"], semkey="ld0")
            A("sp", lambda e: e.dma_start(out=rb[:], in_=rb_d.partition_broadcast(128)), writes=["rb"], semkey="ld0")
            A("sp", lambda e: e.dma_start(out=ecap[:], in_=ecap_d), writes=["ecap"], semkey="ld0")
            A("gq", lambda e: e.dma_start(out=tri[:], in_=tri_d), writes=["tri"], semkey="ld1")
            A("pool", lambda e: e.memset(onesb[:], 1.0), writes=["onesb"])
            onesf = sbt(pa, "onesf", [128, 128]); A("pool", lambda e: e.memset(onesf[:], 1.0), writes=["onesf"])
            ones_t = sbt(pa, "ones_t", [128, CH]); A("pool", lambda e: e.memset(ones_t[:], 1.0), writes=["ones_t"])
            carry = sbt(pa, "carry", [128, 27]); A("pool", lambda e: e.memset(carry[:], 0.0), writes=["carry"])
            uhist = sbt(pa, "uhist", [128, 8, 30]); A("pool", lambda e: e.memset(uhist[:], 0.0), writes=["uhist"])
            S32 = [sbt(pa, f"S32_{p}", [128, 128]) for p in range(8)]
            Sbf = [sbt(pa, f"Sbf_{p}", [128, 128], BF16) for p in range(8)]
            for p in range(8):
                A("pool", lambda e, p=p: e.memset(S32[p][:], 0.0), writes=[f"S32_{p}"])
                A("pool", lambda e, p=p: e.memset(Sbf[p][:], 0.0), writes=[f"Sbf_{p}"])
            cntbase = sbt(pa, "cntbase", [128, NEXP]); A("pool", lambda e: e.memset(cntbase[:], 0.0), writes=["cntbase"])
            xt = sbt(pa, "xt", [128, D])
            xT = sbt(pa, "xT", [128, 16, T], BF16)
            wst = [sbt(pa, f"wst{i}", [128, 16, 128], BF16) for i in range(2)]
            zraw = [sbt(pa, f"zraw{i}", [128, T + 1]) for i in range(2)]
            zd = [sbt(pa, f"zd{i}", [128, T]) for i in range(2)]
            zg = sbt(pa, "zg", [128, 12, T])
            zl = sbt(pa, "zl", [128, 3, T]); lor = zl
            uT = sbt(pa, "uT", [128, 8, 30 + T])
            sgt = [sbt(pa, f"sgt{i}", [128, T]) for i in range(2)]
            cacc = [sbt(pa, f"cacc{i}", [128, T]) for i in range(2)]
            csq = [sbt(pa, f"csq{i}", [128, T]) for i in range(2)]
            cfull = sbt(pa, "cfull", [128, 8, T])
            lnm = sbt(pa, "lnm", [128, 3, T])
            catT = sbt(pa, "catT", [128, 16, T], BF16)
            NR = 4
            PBQ = ["m0", "m1", "fb0", "fb1"]
            def mk(n, s, dt=F32):
                return [sbt(pa, f"{n}{i}", s, dt) for i in range(NR)]
            e_sg = mk("e_sg", [128, TS]); e_a = mk("e_a", [128, TS]); e_kk0 = mk("e_kk0", [128, TS])
            e_t = mk("e_t", [128, TS]); e_kk = mk("e_kk", [128, TS]); e_km = mk("e_km", [128, TS])
            e_cs = mk("e_cs", [128, TS]); e_csm = mk("e_csm", [128, TS]); e_eg = mk("e_eg", [128, TS]); e_ieg = mk("e_ieg", [128, TS])
            e_egm = mk("e_egm", [128, TS]); e_n1 = mk("e_n1", [128, TS], BF16); e_n2 = mk("e_n2", [128, TS], BF16)
            e_n3 = mk("e_n3", [128, TS], BF16); e_t2 = mk("e_t2", [128, TS])
            NCS = TS // CH
            AR = [sbt(pa, f"AR{q}", [128, NCS, 192], BF16) for q in range(4)]
            BT = [sbt(pa, f"BT{q}", [128, NCS, 128], BF16) for q in range(4)]
            KT = [sbt(pa, f"KT{q}", [128, NCS, 128], BF16) for q in range(4)]
            VT = [sbt(pa, f"VT{q}", [128, NCS, 128], BF16) for q in range(4)]
            TOK = [sbt(pa, f"TOK{q}", [128, NCS, 384], BF16) for q in range(4)]
            GC = [sbt(pa, f"GC{q}", [128, NCS]) for q in range(4)]
            BON = [sbt(pa, f"BON{q}", [128, TS]) for q in range(4)]
            GG = [sbt(pa, f"GG{q}", [128, TS]) for q in range(4)]
            YS = [sbt(pa, f"YS{q}", [128, TS]) for q in range(4)]
            NU = 4
            Lt = [sbt(pa, f"Lt{i}", [128, 640], BF16) for i in range(NU)]
            Xa = [sbt(pa, f"Xa{i}", [128, 384], BF16) for i in range(NU)]
            Xb = [sbt(pa, f"Xb{i}", [128, 384], BF16) for i in range(NU)]
            for i_ in range(NU):
                A("pool", lambda e, i_=i_: e.tensor_copy(out=Lt[i_][:, 256:384], in_=identb[:]), reads=["identb"], writes=[f"Lt{i_}"])
            TT = [sbt(pa, f"TT{q}", [128, 128], BF16) for q in range(4)]
            Wb = [sbt(pa, f"Wb{q}", [128, 128], BF16) for q in range(4)]
            Ub = [sbt(pa, f"Ub{q}", [128, 128], BF16) for q in range(4)]
            stmp = [sbt(pa, f"stmp{i}", [128, 128]) for i in range(2)]
            xrow = [sbt(pa, f"xrow{i}", [128, XROW], BF16) for i in range(2)]
            x1Tb = [sbt(pa, "x1Tb0", [128, 4, 128])] * 2
            A("pool", lambda e: e.memset(xrow[0][:], 0.0), writes=["xrow0"])
            xs_v = xsorted.rearrange("(r p) c -> p r c", p=128)
            nblk_x = (NROWS + 128) // 128
            for q0 in range(0, nblk_x, 20):
                nb_ = min(20, nblk_x - q0)
                A("sp", lambda e, q0=q0, nb_=nb_: e.dma_start(out=xs_v[:, q0:q0 + nb_, :], in_=fap(xrow[0][:], [[0, nb_], [1, XROW]])), reads=["xrow0"], writes=["xsorted"], semkey="ld0")
            bst = sbt(pa, "bst", [128, 4, 6]); mv = sbt(pa, "mv", [128, 2]); rstd = sbt(pa, "rstd", [128, 1])
            lg = sbt(pa, "lg", [128, NEXP]); m8 = sbt(pa, "m8", [128, 8]); negm = sbt(pa, "negm", [128, 1])
            e4 = sbt(pa, "e4", [128, 4]); s4 = sbt(pa, "s4", [128, 1]); g4 = sbt(pa, "g4", [128, 4])
            oh = sbt(pa, "oh", [128, 4, NEXP]); msk = sbt(pa, "msk", [128, NEXP]); mskb = sbt(pa, "mskb", [128, NEXP], BF16)
            posf = sbt(pa, "posf", [128, NEXP]); pk = sbt(pa, "pk", [128, 4]); junk = sbt(pa, "junk", [128, NEXP])
            cT8 = sbt(pa, "cT8", [30, C_CONV])
            idx_sc = sbt(pa, "idx_sc", [128, 4], I32)

            def load_x(src_ap, rows):
                if rows < 128:
                    A("pool", lambda e: e.memset(xt[:], 0.0), writes=["xt"])
                A("sp", lambda e: e.dma_start(out=xt[0:rows, :], in_=src_ap), writes=["xt"], semkey="x0")

            def transpose_x(col0, ncol):
                for g in range(4):
                    pn = nxt("pj", 2)
                    for j in range(4):
                        kc = 4 * g + j
                        A("pe", lambda e, pn=pn, j=j, kc=kc: e.transpose(out=PS[pn][:, j * 128:(j + 1) * 128], in_=xt[:, kc * 128:(kc + 1) * 128], identity=ident[:]),
                          reads=["xt", "ident"], writes=[pn])
                    eng = "act" if g % 2 == 0 else "dve"
                    def ev(e, pn=pn, g=g, eng=eng):
                        src = PS[pn].rearrange("p (j t) -> p j t", j=4)[:, :, 0:ncol]
                        dst = xT[:, 4 * g:4 * g + 4, col0:col0 + ncol]
                        return e.copy(out=dst, in_=src) if eng == "act" else e.tensor_copy(out=dst, in_=src)
                    A(eng, ev, reads=[pn], writes=["xT"])

            wst_rot = [0]

            def proj_cols(cts, ncol, consume):
                for ct in cts:
                    wi = wst_rot[0]; wst_rot[0] ^= 1
                    width = min(128, P_IN - ct * 128)
                    A("sp", lambda e, ct=ct, wi=wi: e.dma_start(out=wst[wi][:], in_=w_in_bf[ct].rearrange("p (k n) -> p k n", k=16)),
                      reads=["w_in_bf"], writes=[f"wst{wi}"], semkey=f"wst{wi}")
                    pn = nxt("pj", 2)
                    for kc in range(16):
                        A("pe", lambda e, pn=pn, kc=kc, width=width, wi=wi: e.matmul(PS[pn][0:width, 0:ncol], lhsT=wst[wi][:, kc, 0:width],
                                                                                  rhs=xT[:, kc, 0:ncol], start=(kc == 0), stop=(kc == 15)),
                          reads=[f"wst{wi}", "xT"], writes=[pn])
                    consume(ct, pn, width)

            zr_rot = [0]

            def z_consume(ncol, valid, dst, dstk, slot_of):
                def consume(ct, pn, width):
                    zi = ct - 16
                    sl = slot_of(ct)
                    ri = zr_rot[0]; zr_rot[0] = (ri + 1) % 2
                    zr, zk = zraw[ri], f"zraw{ri}"
                    di = ri % 2
                    A("act", lambda e: e.activation(out=zr[0:width, 1:ncol + 1], in_=PS[pn][0:width, 0:ncol], func=AF.Identity, bias=binT[0:width, ct:ct + 1], scale=1.0),
                      reads=[pn, "binT"], writes=[zk])
                    A("pool", lambda e: e.tensor_copy(out=zr[0:width, 0:1], in_=carry[0:width, zi:zi + 1]), reads=["carry"], writes=[zk])
                    A("pool", lambda e: e.tensor_copy(out=carry[0:width, zi:zi + 1], in_=zr[0:width, valid:valid + 1]), reads=[zk], writes=["carry"])
                    A("dve", lambda e: e.tensor_tensor(out=zd[di][0:width, 0:ncol], in0=zr[0:width, 0:ncol], in1=zr[0:width, 1:ncol + 1], op=ALU.subtract),
                      reads=[zk], writes=[f"zd{di}"])
                    A("dve", lambda e: e.scalar_tensor_tensor(out=dst[0:width, sl, 0:ncol], in0=zd[di][0:width, 0:ncol], scalar=muT[0:width, zi:zi + 1],
                                                              in1=zr[0:width, 1:ncol + 1], op0=ALU.mult, op1=ALU.add),
                      reads=[f"zd{di}", zk, "muT"], writes=[dstk])
                return consume

            def u_consume(ncol):
                def consume(ct, pn, width):
                    if ct >= 8:
                        gi = ct - 8
                        A("act", lambda e: e.activation(out=sgt[gi % 2][:, 0:ncol], in_=PS[pn][:, 0:ncol], func=AF.Sigmoid, bias=binT[:, ct:ct + 1], scale=1.0),
                          reads=[pn, "binT"], writes=[f"sgt{gi % 2}"])
                    else:
                        A("dve", lambda e: e.scalar_tensor_tensor(out=uT[:, ct, 30:30 + ncol], in0=PS[pn][:, 0:ncol], scalar=binT[:, ct:ct + 1],
                                                                  in1=sgt[ct % 2][:, 0:ncol], op0=ALU.add, op1=ALU.mult),
                          reads=[pn, "binT", f"sgt{ct % 2}"], writes=["uT"])
                return consume

            def glu_proj(ncol):
                for ci in range(8):
                    proj_cols([8 + ci, ci], ncol, u_consume(ncol))

            def conv_block(ncol):
                for ci in range(8):
                    ai = ci % 2
                    A("dve", lambda e, ci=ci, ai=ai: e.tensor_scalar(out=cacc[ai][:, 0:ncol], in0=uT[:, ci, 0:ncol], scalar1=cwT[:, ci, 0:1], scalar2=cvec[:, 0, ci:ci + 1],
                                                                    op0=ALU.mult, op1=ALU.add), reads=["uT", "cwT", "cvec"], writes=[f"cacc{ai}"])
                    for j in range(1, 31):
                        last = (j == 30)
                        A("dve", lambda e, ci=ci, ai=ai, j=j, last=last: e.scalar_tensor_tensor(
                            out=(cfull[:, ci, 0:ncol] if last else cacc[ai][:, 0:ncol]), in0=uT[:, ci, j:j + ncol], scalar=cwT[:, ci, j:j + 1],
                            in1=cacc[ai][:, 0:ncol], op0=ALU.mult, op1=ALU.add),
                          reads=["uT", "cwT", f"cacc{ai}"], writes=(["cfull"] if last else [f"cacc{ai}"]))
                    A("act", lambda e, ci=ci, ai=ai: e.activation(out=csq[ai][:, 0:ncol], in_=cfull[:, ci, 0:ncol], func=AF.Square), reads=["cfull"], writes=[f"csq{ai}"])
                    A("pe", lambda e, ci=ci: e.matmul(PS["m0"][:, 0:ncol], lhsT=onesf[:], rhs=cfull[:, ci, 0:ncol], start=(ci == 0), stop=(ci == 7)),
                      reads=["cfull", "onesf"], writes=["m0"])
                    A("pe", lambda e, ci=ci, ai=ai: e.matmul(PS["m1"][:, 0:ncol], lhsT=onesf[:], rhs=csq[ai][:, 0:ncol], start=(ci == 0), stop=(ci == 7)),
                      reads=[f"csq{ai}", "onesf"], writes=["m1"])
                A("act", lambda e: e.activation(out=lnm[:, 0, 0:ncol], in_=PS["m0"][:, 0:ncol], func=AF.Copy, scale=1.0 / C_CONV), reads=["m0"], writes=["lnm"])
                A("pool", lambda e: e.tensor_tensor(out=lnm[:, 1, 0:ncol], in0=lnm[:, 0, 0:ncol], in1=lnm[:, 0, 0:ncol], op=ALU.mult), reads=["lnm"], writes=["lnm"])
                A("dve", lambda e: e.scalar_tensor_tensor(out=lnm[:, 2, 0:ncol], in0=PS["m1"][:, 0:ncol], scalar=1.0 / C_CONV, in1=lnm[:, 1, 0:ncol], op0=ALU.mult, op1=ALU.subtract),
                  reads=["m1", "lnm"], writes=["lnm"])
                rsqrt(lnm[:, 2, 0:ncol], lnm[:, 2, 0:ncol], 0, ["lnm"], ["lnm"])
                for ci in range(8):
                    ai = ci % 2
                    A("pool", lambda e, ci=ci, ai=ai: e.tensor_tensor(out=csq[ai][:, 0:ncol], in0=cfull[:, ci, 0:ncol], in1=lnm[:, 0, 0:ncol], op=ALU.subtract), reads=["cfull", "lnm"], writes=[f"csq{ai}"])
                    A("dve", lambda e, ci=ci, ai=ai: e.tensor_tensor(out=csq[ai][:, 0:ncol], in0=csq[ai][:, 0:ncol], in1=lnm[:, 2, 0:ncol], op=ALU.mult), reads=[f"csq{ai}", "lnm"], writes=[f"csq{ai}"])
                    A("act", lambda e, ci=ci, ai=ai: e.activation(out=catT[:, ci, 0:ncol], in_=csq[ai][:, 0:ncol], func=AF.Silu, bias=cvec[:, 2, ci:ci + 1], scale=cvec[:, 1, ci:ci + 1]),
                      reads=[f"csq{ai}", "cvec"], writes=["catT"])

            def save_uhist(valid):
                A("pool", lambda e: e.tensor_copy(out=uhist[:], in_=uT[:, :, valid:valid + 30]), reads=["uT"], writes=["uhist"])

            def load_uhist():
                A("pool", lambda e: e.tensor_copy(out=uT[:, :, 0:30], in_=uhist[:]), reads=["uhist"], writes=["uT"])

            def lora_acts(ncol, full):
                A("act", lambda e: e.activation(out=lor[0:64, 0, 0:ncol], in_=zl[0:64, 0, 0:ncol], func=AF.Tanh), reads=["zl"], writes=["zl"])
                if full:
                    A("act", lambda e: e.activation(out=lor[:, 1, 0:ncol], in_=zl[:, 1, 0:ncol], func=AF.Sigmoid), reads=["zl"], writes=["zl"])
                    A("act", lambda e: e.activation(out=lor[0:32, 2, 0:ncol], in_=zl[0:32, 2, 0:ncol], func=AF.Sigmoid), reads=["zl"], writes=["zl"])

            def bd(eng_name, dst3, src2, nch, keyr, keyw):
                def f(e):
                    in0 = fap(src2, [[CH, nch], [0, 2], [1, CH]])
                    in1 = fap(maskh[:], [[0, nch], [1, 2], [0, CH]])
                    return e.tensor_tensor(out=dst3, in0=in0, in1=in1, op=ALU.mult)
                A(eng_name, f, reads=keyr + ["maskh"], writes=keyw)

            def pair_prep(p, col0, ncol, nch, full, valid):
                q = p % 4
                i = q % NR
                pv = lambda j: pvec[:, j, p:p + 1]
                cw = slice(col0, col0 + ncol)
                zr_ = zg[:, q, cw]; zk_ = zg[:, 4 + q, cw]; zv_ = zg[:, 8 + q, cw]
                K = lambda n: [f"{n}{i}"]
                cs_ = slice(p * 128, (p + 1) * 128)
                N = slice(0, ncol)
                if valid < ncol:
                    for sl in (q, 4 + q, 8 + q):
                        yield A("pool", lambda e, sl=sl: e.memset(zg[:, sl, col0 + valid:col0 + ncol], 0.0), writes=["zg"])
                pw = PBQ[q]
                yield A("pe", lambda e: e.matmul(PS[pw][:, N], lhsT=w2a2[0:64, cs_], rhs=lor[0:64, 0, cw], start=True, stop=True), reads=["w2a2", "zl"], writes=[pw])
                yield A("act", lambda e: e.activation(out=e_sg[i][:, N], in_=PS[pw][:, N], func=AF.Sigmoid, bias=pv(0), scale=1.0), reads=[pw, "pvec"], writes=K("e_sg"))
                if valid < ncol:
                    yield A("pool", lambda e: e.memset(e_sg[i][:, valid:ncol], 0.0), writes=K("e_sg"))
                pa_ = PBQ[q]
                yield A("pe", lambda e: e.matmul(PS[pa_][:, N], lhsT=w2a2[64:128, cs_], rhs=lor[64:128, 0, cw], start=True, stop=True), reads=["w2a2", "zl"], writes=[pa_])
                yield A("act", lambda e: e.activation(out=e_a[i][:, N], in_=PS[pa_][:, N], func=AF.Sigmoid, bias=pv(1), scale=1.0), reads=[pa_, "pvec"], writes=K("e_a"))
                if full:
                    pg = PBQ[q]
                    yield A("pe", lambda e: e.matmul(PS[pg][:, N], lhsT=g2a[:, cs_], rhs=lor[:, 1, cw], start=True, stop=False), reads=["g2a", "zl"], writes=[pg])
                    yield A("pe", lambda e: e.matmul(PS[pg][:, N], lhsT=g2b[0:32, cs_], rhs=lor[0:32, 2, cw], start=False, stop=True), reads=["g2b", "zl"], writes=[pg])
                    yield A("act", lambda e: e.copy(out=GG[q][:, N], in_=PS[pg][:, N]), reads=[pg], writes=[f"GG{q}"])
                yield A("dve", lambda e: e.tensor_scalar(out=e_kk0[i][:, N], in0=zk_, scalar1=pv(2), scalar2=None, op0=ALU.mult), reads=["zg", "pvec"], writes=K("e_kk0"))
                yield A("act", lambda e: e.activation(out=e_t[i][:, N], in_=e_kk0[i][:, N], func=AF.Square), reads=K("e_kk0"), writes=K("e_t"))
                pss = PBQ[q]
                yield A("pe", lambda e: e.matmul(PS[pss][:, N], lhsT=onesbd[:], rhs=e_t[i][:, N], start=True, stop=True), reads=["onesbd"] + K("e_t"), writes=[pss])
                yield rsqrt(e_t[i][:, N], PS[pss][:, N], 2, [pss], K("e_t"))
                yield A("dve", lambda e: e.tensor_tensor(out=e_kk[i][:, N], in0=e_kk0[i][:, N], in1=e_t[i][:, N], op=ALU.mult), reads=K("e_kk0") + K("e_t"), writes=K("e_kk"))
                yield A("dve", lambda e: e.tensor_scalar(out=e_t2[i][:, N], in0=e_a[i][:, N], scalar1=-1.0, scalar2=pv(3), op0=ALU.add, op1=ALU.mult), reads=K("e_a") + ["pvec"], writes=K("e_t2"))
                yield A("dve", lambda e: e.scalar_tensor_tensor(out=e_km[i][:, N], in0=e_t2[i][:, N], scalar=1.0, in1=zk_, op0=ALU.add, op1=ALU.mult), reads=K("e_t2") + ["zg"], writes=K("e_km"))
                if full:
                    yield A("dve", lambda e: e.scalar_tensor_tensor(out=e_t2[i][:, N], in0=zr_, scalar=pv(4), in1=e_km[i][:, N], op0=ALU.mult, op1=ALU.mult), reads=["zg", "pvec"] + K("e_km"), writes=K("e_t2"))
                    pb_ = PBQ[q]
                    yield A("pe", lambda e: e.matmul(PS[pb_][:, N], lhsT=onesbd[:], rhs=e_t2[i][:, N], start=True, stop=True), reads=["onesbd"] + K("e_t2"), writes=[pb_])
                    yield A("dve", lambda e: e.tensor_tensor(out=BON[q][:, N], in0=PS[pb_][:, N], in1=zv_, op=ALU.mult), reads=[pb_, "zg"], writes=[f"BON{q}"])
                for c in range(nch):
                    yield A("dve", lambda e, c=c: e.tensor_tensor_scan(out=e_cs[i][:, c * CH:(c + 1) * CH], data0=ones_t[:], data1=e_sg[i][:, c * CH:(c + 1) * CH], initial=0.0, op0=ALU.mult, op1=ALU.add),
                      reads=K("e_sg") + ["ones_t"], writes=K("e_cs"))
                yield A("pool", lambda e: e.tensor_tensor(out=e_csm[i][:, N], in0=e_cs[i][:, N], in1=e_sg[i][:, N], op=ALU.subtract), reads=K("e_cs") + K("e_sg"), writes=K("e_csm"))
                yield A("act", lambda e: e.activation(out=e_eg[i][:, N], in_=e_cs[i][:, N], func=AF.Exp, scale=-DEC), reads=K("e_cs"), writes=K("e_eg"))
                yield A("act", lambda e: e.activation(out=e_ieg[i][:, N], in_=e_cs[i][:, N], func=AF.Exp, scale=DEC), reads=K("e_cs"), writes=K("e_ieg"))
                yield A("act", lambda e: e.activation(out=e_egm[i][:, N], in_=e_csm[i][:, N], func=AF.Exp, scale=-DEC), reads=K("e_csm"), writes=K("e_egm"))
                yield A("pool", lambda e: e.tensor_copy(out=GC[q][:, 0:nch], in_=fap(e_eg[i][:, N], [[CH, nch]], off=CH - 1)), reads=K("e_eg"), writes=[f"GC{q}"])
                yield A("dve", lambda e: e.scalar_tensor_tensor(out=e_n1[i][:, N], in0=e_kk[i][:, N], scalar=-1.0, in1=e_egm[i][:, N], op0=ALU.mult, op1=ALU.mult), reads=K("e_kk") + K("e_egm"), writes=K("e_n1"))
                yield bd("pool", AR[q][:, 0:nch, 0:128].rearrange("p c (h t) -> p c h t", h=2), e_n1[i][:, N], nch, K("e_n1"), [f"AR{q}"])
                yield A("dve", lambda e: e.tensor_tensor(out=e_t[i][:, N], in0=e_kk[i][:, N], in1=e_a[i][:, N], op=ALU.mult), reads=K("e_kk") + K("e_a"), writes=K("e_t"))
                yield A("dve", lambda e: e.tensor_tensor(out=e_n2[i][:, N], in0=e_t[i][:, N], in1=e_ieg[i][:, N], op=ALU.mult), reads=K("e_t") + K("e_ieg"), writes=K("e_n2"))
                yield bd("pool", BT[q][:, 0:nch, :].rearrange("p c (h t) -> p c h t", h=2), e_n2[i][:, N], nch, K("e_n2"), [f"BT{q}"])
                yield A("dve", lambda e: e.tensor_tensor(out=e_n3[i][:, N], in0=e_km[i][:, N], in1=e_ieg[i][:, N], op=ALU.mult), reads=K("e_km") + K("e_ieg"), writes=K("e_n3"))
                yield bd("pool", KT[q][:, 0:nch, :].rearrange("p c (h t) -> p c h t", h=2), e_n3[i][:, N], nch, K("e_n3"), [f"KT{q}"])
                if full:
                    yield A("dve", lambda e: e.tensor_tensor(out=AR[q][:, 0:nch, 128:192], in0=zr_.rearrange("p (c t) -> p c t", t=CH), in1=e_eg[i][:, N].rearrange("p (c t) -> p c t", t=CH), op=ALU.mult),
                      reads=["zg"] + K("e_eg"), writes=[f"AR{q}"])
                yield bd("pool", VT[q][:, 0:nch, :].rearrange("p c (h t) -> p c h t", h=2), zv_, nch, ["zg"], [f"VT{q}"])
                for c in range(nch):
                    pt = PBQ[q]
                    ptb = PS[pt].bitcast(BF16)
                    for j, (src, sn) in enumerate(((BT, "BT"), (KT, "KT"), (VT, "VT"))):
                        yield A("pe", lambda e, j=j, src=src, c=c, ptb=ptb: e.transpose(out=ptb[:, j * 128:(j + 1) * 128], in_=src[q][:, c, :], identity=identb[:]),
                          reads=[f"{sn}{q}", "identb"], writes=[pt])
                    yield A("act", lambda e, c=c, ptb=ptb: e.copy(out=TOK[q][:, c, :], in_=ptb[:, 0:384]), reads=[pt], writes=[f"TOK{q}"])

            u_rot = [0]

            def unit_local(p, c, full):
                q = p % 4
                ui = q
                L, Lk = Lt[ui], f"Lt{ui}"
                nR = 192 if full else 128
                gb = PBQ[q]
                yield A("pe", lambda e: e.matmul(PS[gb][:, 0:128], lhsT=AR[q][:, c, 0:128], rhs=BT[q][:, c, :], start=True, stop=True), reads=[f"AR{q}", f"BT{q}"], writes=[gb])
                yield A("pe", lambda e: e.matmul(PS[gb][:, 128:128 + nR], lhsT=BT[q][:, c, :], rhs=AR[q][:, c, 0:nR], start=True, stop=True), reads=[f"AR{q}", f"BT{q}"], writes=[gb])
                yield A("pe", lambda e: e.matmul(PS[gb][:, 320:320 + nR], lhsT=KT[q][:, c, :], rhs=AR[q][:, c, 0:nR], start=True, stop=True), reads=[f"AR{q}", f"KT{q}"], writes=[gb])
                yield A("dve", lambda e: e.tensor_tensor(out=L[:, 0:256], in0=PS[gb][:, 0:256], in1=m1[:], op=ALU.mult), reads=[gb, "m1"], writes=[Lk])
                if full:
                    yield A("dve", lambda e: e.tensor_tensor(out=L[:, 384:640], in0=PS[gb][:, 256:512], in1=m2[:], op=ALU.mult), reads=[gb, "m2"], writes=[Lk])
                else:
                    yield A("dve", lambda e: e.tensor_tensor(out=L[:, 448:576], in0=PS[gb][:, 320:448], in1=m2[:, 64:192], op=ALU.mult), reads=[gb, "m2"], writes=[Lk])
                cur, curk = L, Lk
                bufs = [(Xa[ui], f"Xa{ui}"), (Xb[ui], f"Xb{ui}")]
                for lev in range(6):
                    fbn = PBQ[q]
                    if lev == 5:
                        yield A("pe", lambda e, cur=cur, fbn=fbn: e.matmul(PS[fbn][:, 256:384], lhsT=cur[:, 0:128], rhs=cur[:, 256:384], start=True, stop=True), reads=[curk], writes=[fbn])
                        yield A("dve", lambda e, cur=cur, fbn=fbn: e.tensor_tensor(out=TT[q][:], in0=PS[fbn][:, 256:384], in1=cur[:, 256:384], op=ALU.add), reads=[fbn, curk], writes=[f"TT{q}"])
                    else:
                        nx, nxk = bufs[lev % 2]
                        yield A("pe", lambda e, cur=cur, fbn=fbn: e.matmul(PS[fbn][:, 128:384], lhsT=cur[:, 0:128], rhs=cur[:, 128:384], start=True, stop=True), reads=[curk], writes=[fbn])
                        yield A("pe", lambda e, cur=cur, fbn=fbn: e.matmul(PS[fbn][:, 0:128], lhsT=cur[:, 128:256], rhs=cur[:, 0:128], start=True, stop=True), reads=[curk], writes=[fbn])
                        yield A("act", lambda e, nx=nx, fbn=fbn: e.copy(out=nx[:, 0:256], in_=PS[fbn][:, 0:256]), reads=[fbn], writes=[nxk])
                        yield A("dve", lambda e, nx=nx, cur=cur, fbn=fbn: e.tensor_tensor(out=nx[:, 256:384], in0=PS[fbn][:, 256:384], in1=cur[:, 256:384], op=ALU.add), reads=[fbn, curk], writes=[nxk])
                        cur, curk = nx, nxk

            def chain_stage_w(p, c, ui):
                q = p % 4
                cn = nxt("c", 2)
                A("pe", lambda e: e.matmul(PS[cn][:, 0:128], lhsT=AR[q][:, c, 0:128], rhs=Sbf[p][:], start=True, stop=False), reads=[f"AR{q}", f"Sbf_{p}"], writes=[cn])
                A("pe", lambda e: e.matmul(PS[cn][:, 0:128], lhsT=Lt[ui][:, 448:576], rhs=TOK[q][:, c, 256:384], start=False, stop=True), reads=[f"Lt{ui}", f"TOK{q}"], writes=[cn])
                A("act", lambda e: e.copy(out=Wb[q][:], in_=PS[cn][:, 0:128]), reads=[cn], writes=[f"Wb{q}"])

            def chain_stage_u(p, c):
                q = p % 4
                cn = nxt("c", 2)
                A("pe", lambda e: e.matmul(PS[cn][:, 0:128], lhsT=TT[q][:], rhs=Wb[q][:], start=True, stop=True), reads=[f"TT{q}", f"Wb{q}"], writes=[cn])
                A("dve", lambda e: e.tensor_copy(out=Ub[q][:], in_=PS[cn][:, 0:128]), reads=[cn], writes=[f"Ub{q}"])

            def chain_stage_y(p, c, ui):
                q = p % 4
                cn = nxt("c", 2)
                A("pe", lambda e: e.matmul(PS[cn][:, 0:CH], lhsT=Sbf[p][:], rhs=AR[q][:, c, 128:192], start=True, stop=False), reads=[f"Sbf_{p}", f"AR{q}"], writes=[cn])
                A("pe", lambda e: e.matmul(PS[cn][:, 0:CH], lhsT=Ub[q][:], rhs=Lt[ui][:, 384:448], start=False, stop=False), reads=[f"Ub{q}", f"Lt{ui}"], writes=[cn])
                A("pe", lambda e: e.matmul(PS[cn][:, 0:CH], lhsT=TOK[q][:, c, 256:384], rhs=Lt[ui][:, 576:640], start=False, stop=True), reads=[f"TOK{q}", f"Lt{ui}"], writes=[cn])
                A("act", lambda e: e.copy(out=YS[q][:, c * CH:(c + 1) * CH], in_=PS[cn][:, 0:CH]), reads=[cn], writes=[f"YS{q}"])

            def chain_stage_s(p, c):
                q = p % 4
                cn = nxt("c", 2)
                si = p % 2
                A("pe", lambda e: e.matmul(PS[cn][:, 0:128], lhsT=TOK[q][:, c, 0:128], rhs=Ub[q][:], start=True, stop=False), reads=[f"TOK{q}", f"Ub{q}"], writes=[cn])
                A("pe", lambda e: e.matmul(PS[cn][:, 0:128], lhsT=TOK[q][:, c, 128:256], rhs=TOK[q][:, c, 256:384], start=False, stop=True), reads=[f"TOK{q}"], writes=[cn])
                A("dve", lambda e: e.tensor_tensor(out=stmp[si][:], in0=PS[cn][:, 0:128], in1=S32[p][:], op=ALU.add), reads=[cn, f"S32_{p}"], writes=[f"stmp{si}"])
                A("act", lambda e: e.activation(out=S32[p][:], in_=stmp[si][:], func=AF.Copy, scale=GC[q][:, c:c + 1]), reads=[f"stmp{si}", f"GC{q}"], writes=[f"S32_{p}"])
                A("pool", lambda e: e.tensor_copy(out=Sbf[p][:], in_=S32[p][:]), reads=[f"S32_{p}"], writes=[f"Sbf_{p}"])

            def rwkv_finish(p, col0, ncol):
                q = p % 4
                i = q % NR
                K = lambda n: [f"{n}{i}"]
                pv = lambda j: pvec[:, j, p:p + 1]
                N = slice(0, ncol)
                yield A("act", lambda e: e.activation(out=e_t[i][:, N], in_=YS[q][:, N], func=AF.Square), reads=[f"YS{q}"], writes=K("e_t"))
                pm = PBQ[q]
                yield A("pe", lambda e: e.matmul(PS[pm][:, N], lhsT=onesbd[:], rhs=YS[q][:, N], start=True, stop=True), reads=["onesbd", f"YS{q}"], writes=[pm])
                yield A("act", lambda e: e.activation(out=e_kk0[i][:, N], in_=PS[pm][:, N], func=AF.Copy, scale=1.0 / 64), reads=[pm], writes=K("e_kk0"))
                pq = PBQ[q]
                yield A("pe", lambda e: e.matmul(PS[pq][:, N], lhsT=onesbd[:], rhs=e_t[i][:, N], start=True, stop=True), reads=["onesbd"] + K("e_t"), writes=[pq])
                yield A("pool", lambda e: e.tensor_tensor(out=e_kk[i][:, N], in0=e_kk0[i][:, N], in1=e_kk0[i][:, N], op=ALU.mult), reads=K("e_kk0"), writes=K("e_kk"))
                yield A("dve", lambda e: e.scalar_tensor_tensor(out=e_t[i][:, N], in0=PS[pq][:, N], scalar=1.0 / 64, in1=e_kk[i][:, N], op0=ALU.mult, op1=ALU.subtract), reads=[pq] + K("e_kk"), writes=K("e_t"))
                yield rsqrt(e_t[i][:, N], e_t[i][:, N], 1, K("e_t"), K("e_t"))
                yield A("pool", lambda e: e.tensor_tensor(out=e_km[i][:, N], in0=YS[q][:, N], in1=e_kk0[i][:, N], op=ALU.subtract), reads=[f"YS{q}"] + K("e_kk0"), writes=K("e_km"))
                yield A("dve", lambda e: e.tensor_tensor(out=e_km[i][:, N], in0=e_km[i][:, N], in1=e_t[i][:, N], op=ALU.mult), reads=K("e_km") + K("e_t"), writes=K("e_km"))
                yield A("dve", lambda e: e.tensor_scalar(out=e_km[i][:, N], in0=e_km[i][:, N], scalar1=pv(5), scalar2=pv(6), op0=ALU.mult, op1=ALU.add), reads=K("e_km") + ["pvec"], writes=K("e_km"))
                yield A("pool", lambda e: e.tensor_tensor(out=e_km[i][:, N], in0=e_km[i][:, N], in1=BON[q][:, N], op=ALU.add), reads=K("e_km") + [f"BON{q}"], writes=K("e_km"))
                yield A("pool", lambda e: e.tensor_tensor(out=catT[:, 8 + p, col0:col0 + ncol], in0=e_km[i][:, N], in1=GG[q][:, N], op=ALU.mult), reads=K("e_km") + [f"GG{q}"], writes=["catT"])

            def run_il(gens):
                gens = list(gens)
                while gens:
                    for g_ in list(gens):
                        try:
                            next(g_)
                        except StopIteration:
                            gens.remove(g_)

            def rwkv_group(g, ncolt, full, valid):
                nsubs = (ncolt + TS - 1) // TS
                for sub in range(nsubs):
                    col0 = sub * TS
                    ncol = min(TS, ncolt - col0)
                    nch = ncol // CH
                    v = max(0, min(ncol, valid - col0))
                    run_il([pair_prep(4 * g + q, col0, ncol, nch, full, v) for q in range(4)])
                    for c in range(nch):
                        run_il([unit_local(4 * g + q, c, full) for q in range(4)])
                        for q in range(4):
                            chain_stage_w(4 * g + q, c, q)
                        for q in range(4):
                            chain_stage_u(4 * g + q, c)
                        if full:
                            for q in range(4):
                                chain_stage_y(4 * g + q, c, q)
                        for q in range(4):
                            chain_stage_s(4 * g + q, c)
                    if full:
                        run_il([rwkv_finish(4 * g + q, col0, ncol) for q in range(4)])

            def apply_flag_state():
                for p in range(8):
                    A("dve", lambda e, p=p: e.tensor_scalar(out=S32[p][:], in0=S32[p][:], scalar1=flag[:, 0:1], scalar2=None, op0=ALU.mult), reads=[f"S32_{p}", "flag"], writes=[f"S32_{p}"])
                    A("pool", lambda e, p=p: e.tensor_copy(out=Sbf[p][:], in_=S32[p][:]), reads=[f"S32_{p}"], writes=[f"Sbf_{p}"])
                A("dve", lambda e: e.tensor_scalar(out=carry[:], in0=carry[:], scalar1=flag[:, 0:1], scalar2=None, op0=ALU.mult), reads=["carry", "flag"], writes=["carry"])
                A("dve", lambda e: e.tensor_scalar(out=uhist[:].rearrange("p a b -> p (a b)"), in0=uhist[:].rearrange("p a b -> p (a b)"), scalar1=flag[:, 0:1], scalar2=None, op0=ALU.mult),
                  reads=["uhist", "flag"], writes=["uhist"])

            def out_conv(dst):
                for ci in range(8):
                    pn = nxt("pj", 2)
                    A("pe", lambda e, ci=ci, pn=pn: e.transpose(out=PS[pn][0:30, 0:128], in_=uhist[:, ci, :], identity=ident[:]), reads=["uhist", "ident"], writes=[pn])
                    A("act", lambda e, ci=ci, pn=pn: e.copy(out=cT8[:, ci * 128:(ci + 1) * 128], in_=PS[pn][0:30, 0:128]), reads=[pn], writes=["cT8"])
                A("sp", lambda e: e.dma_start(out=dst, in_=cT8[:]), reads=["cT8"], semkey="out")

            def out_wkv(dst):
                for p in range(8):
                    A("sp", lambda e, p=p: e.dma_start(out=dst[p, 0:64, :], in_=S32[p][0:64, 0:64]), reads=[f"S32_{p}"], semkey="out")
                    A("sp", lambda e, p=p: e.dma_start(out=dst[p, 64:128, :], in_=S32[p][64:128, 64:128]), reads=[f"S32_{p}"], semkey="out")

            def layer_norm(buf, bufk, gb, gbk):
                for q in range(4):
                    A("dve", lambda e, q=q: e.bn_stats(out=bst[:, q, :], in_=buf[:, q * 512:(q + 1) * 512]), reads=[bufk], writes=["bst"])
                A("dve", lambda e: e.bn_aggr(out=mv[:], in_=bst[:].rearrange("p a b -> p (a b)")), reads=["bst"], writes=["mv"])
                rsqrt(rstd[:], mv[:, 1:2], 0, ["mv"], ["rstd"])
                A("dve", lambda e: e.tensor_scalar(out=buf[:], in0=buf[:], scalar1=mv[:, 0:1], scalar2=rstd[:, 0:1], op0=ALU.subtract, op1=ALU.mult), reads=[bufk, "mv", "rstd"], writes=[bufk])
                A("pool", lambda e: e.tensor_tensor(out=buf[:], in0=buf[:], in1=gb[:, 0, :], op=ALU.mult), reads=[bufk, gbk], writes=[bufk])
                A("pool", lambda e: e.tensor_tensor(out=buf[:], in0=buf[:], in1=gb[:, 1, :], op=ALU.add), reads=[bufk, gbk], writes=[bufk])

            def front_sub(src_ap, rows, col0, ncolm, sidx):
                done_subs.append(sidx)
                load_x(src_ap, rows)
                if ncolm < 128:
                    pass
                for piece in range(16):
                    wi = wst_rot[0]; wst_rot[0] ^= 1
                    A("sp", lambda e, wi=wi, piece=piece: e.dma_start(out=wst[wi][:], in_=w_out_bf[piece].rearrange("p (k n) -> p k n", k=16)),
                      reads=["w_out_bf"], writes=[f"wst{wi}"], semkey=f"wst{wi}")
                    pn = nxt("pj", 2)
                    for kc in range(16):
                        A("pe", lambda e, kc=kc, pn=pn, wi=wi: e.matmul(PS[pn][0:ncolm, 0:128], lhsT=catT[:, kc, col0:col0 + ncolm], rhs=wst[wi][:, kc, :], start=(kc == 0), stop=(kc == 15)),
                          reads=["catT", f"wst{wi}"], writes=[pn])
                    A("dve", lambda e, pn=pn, piece=piece: e.scalar_tensor_tensor(out=xt[0:ncolm, piece * 128:(piece + 1) * 128], in0=xt[0:ncolm, piece * 128:(piece + 1) * 128], scalar=ALPHA,
                                                                                  in1=PS[pn][0:ncolm, 0:128], op0=ALU.mult, op1=ALU.add), reads=[pn, "xt"], writes=["xt"])
                layer_norm(xt, "xt", lnv, "lnv")
                A("sp", lambda e: e.dma_start(out=x1s[sidx * 128:(sidx + 1) * 128, :], in_=xt[:]), reads=["xt"], writes=["x1s"], semkey="x1s")
                if stage < 3:
                    return
                pl = nxt("m", 2)
                for g in range(4):
                    pn = nxt("pj", 2)
                    bi = g % 2
                    for j in range(4):
                        kc = 4 * g + j
                        A("pe", lambda e, pn=pn, j=j, kc=kc: e.transpose(out=PS[pn][:, j * 128:(j + 1) * 128], in_=xt[:, kc * 128:(kc + 1) * 128], identity=ident[:]), reads=["xt", "ident"], writes=[pn])
                    if g % 2 == 0:
                        A("act", lambda e, pn=pn, bi=bi: e.copy(out=x1Tb[bi][:], in_=PS[pn].rearrange("p (j t) -> p j t", j=4)), reads=[pn], writes=["x1Tb0"])
                    else:
                        A("dve", lambda e, pn=pn, bi=bi: e.tensor_copy(out=x1Tb[bi][:], in_=PS[pn].rearrange("p (j t) -> p j t", j=4)), reads=[pn], writes=["x1Tb0"])
                    for j in range(4):
                        kc = 4 * g + j
                        A("pe", lambda e, kc=kc, j=j, bi=bi: e.matmul(PS[pl][:, 0:NEXP], lhsT=x1Tb[bi][:, j, :], rhs=rw[:, kc, :], start=(kc == 0), stop=(kc == 15)), reads=["x1Tb0", "rw"], writes=[pl])
                A("dve", lambda e: e.tensor_tensor(out=lg[:], in0=PS[pl][:, 0:NEXP], in1=rb[:], op=ALU.add), reads=[pl, "rb"], writes=["lg"])
                A("dve", lambda e: e.max(out=m8[:], in_=lg[:]), reads=["lg"], writes=["m8"])
                A("dve", lambda e: e.tensor_scalar(out=negm[:], in0=m8[:, 0:1], scalar1=-1.0, scalar2=None, op0=ALU.mult), reads=["m8"], writes=["negm"])
                A("act", lambda e: e.activation(out=e4[:], in_=m8[:, 0:4], func=AF.Exp, bias=negm[:, 0:1], scale=1.0), reads=["m8", "negm"], writes=["e4"])
                A("dve", lambda e: e.tensor_reduce(out=s4[:], in_=e4[:], axis=mybir.AxisListType.X, op=ALU.add), reads=["e4"], writes=["s4"])
                A("dve", lambda e: e.reciprocal(out=s4[:], in_=s4[:]), reads=["s4"], writes=["s4"])
                A("dve", lambda e: e.tensor_scalar(out=g4[:], in0=e4[:], scalar1=s4[:, 0:1], scalar2=None, op0=ALU.mult), reads=["e4", "s4"], writes=["g4"])
                for k in range(4):
                    A("dve", lambda e, k=k: e.tensor_scalar(out=oh[:, k, :], in0=lg[:], scalar1=m8[:, k:k + 1], scalar2=None, op0=ALU.is_equal), reads=["lg", "m8"], writes=["oh"])
                A("dve", lambda e: e.tensor_tensor(out=msk[:], in0=oh[:, 0, :], in1=oh[:, 1, :], op=ALU.add), reads=["oh"], writes=["msk"])
                A("dve", lambda e: e.tensor_tensor(out=msk[:], in0=msk[:], in1=oh[:, 2, :], op=ALU.add), reads=["oh", "msk"], writes=["msk"])
                A("dve", lambda e: e.tensor_tensor(out=msk[:], in0=msk[:], in1=oh[:, 3, :], op=ALU.add), reads=["oh", "msk"], writes=["msk"])
                if ncolm < 128:
                    A("dve", lambda e: e.tensor_scalar(out=msk[:], in0=msk[:], scalar1=rvt[:, 0:1], scalar2=None, op0=ALU.mult), reads=["msk", "rvt"], writes=["msk"])
                A("pool", lambda e: e.tensor_copy(out=mskb[:], in_=msk[:]), reads=["msk"], writes=["mskb"])
                A("dve", lambda e: e.tensor_scalar(out=gd_all[:, sidx, :], in0=oh[:, 0, :], scalar1=g4[:, 0:1], scalar2=None, op0=ALU.mult), reads=["oh", "g4"], writes=["gd_all"])
                for k in range(1, 4):
                    A("dve", lambda e, k=k: e.scalar_tensor_tensor(out=gd_all[:, sidx, :], in0=oh[:, k, :], scalar=g4[:, k:k + 1], in1=gd_all[:, sidx, :], op0=ALU.mult, op1=ALU.add), reads=["oh", "g4", "gd_all"], writes=["gd_all"])
                pc = nxt("m", 2)
                A("pe", lambda e: e.matmul(PS[pc][:, 0:NEXP], lhsT=tri[:], rhs=mskb[:], start=True, stop=True), reads=["tri", "mskb"], writes=[pc])
                A("pe", lambda e: e.matmul(PS[pc][:, NEXP:2 * NEXP], lhsT=onesb[:], rhs=mskb[:], start=True, stop=True), reads=["onesb", "mskb"], writes=[pc])
                A("dve", lambda e: e.tensor_tensor(out=posf[:], in0=PS[pc][:, 0:NEXP], in1=cntbase[:], op=ALU.add), reads=[pc, "cntbase"], writes=["posf"])
                A("dve", lambda e: e.tensor_tensor(out=posf[:], in0=posf[:], in1=ecap[:], op=ALU.add), reads=["posf", "ecap"], writes=["posf"])
                A("dve", lambda e: e.tensor_tensor(out=cntbase[:], in0=PS[pc][:, NEXP:2 * NEXP], in1=cntbase[:], op=ALU.add), reads=[pc, "cntbase"], writes=["cntbase"])
                for k in range(4):
                    A("dve", lambda e, k=k: e.tensor_tensor(out=junk[:], in0=oh[:, k, :], in1=posf[:], op=ALU.mult), reads=["oh", "posf"], writes=["junk"])
                    A("dve", lambda e, k=k: e.tensor_reduce(out=pk[:, k:k + 1], in_=junk[:], axis=mybir.AxisListType.X, op=ALU.add), reads=["junk"], writes=["pk"])
                A("dve", lambda e: e.tensor_scalar(out=pk[:], in0=pk[:], scalar1=float(NROWS - 1), scalar2=0.0, op0=ALU.min, op1=ALU.max), reads=["pk"], writes=["pk"])
                A("dve", lambda e: e.tensor_copy(out=idx_all[:, sidx, :], in_=pk[:]), reads=["pk"], writes=["idx_all"])
                if ncolm < 128:
                    A("dve", lambda e: e.tensor_scalar(out=pk[:], in0=pk[:], scalar1=rvt[:, 1:2], scalar2=rvt[:, 0:1], op0=ALU.subtract, op1=ALU.mult), reads=["pk", "rvt"], writes=["pk"])
                    A("dve", lambda e: e.tensor_scalar(out=pk[:], in0=pk[:], scalar1=rvt[:, 1:2], scalar2=None, op0=ALU.add), reads=["pk", "rvt"], writes=["pk"])
                A("dve", lambda e: e.tensor_copy(out=idx_sc[:], in_=pk[:]), reads=["pk"], writes=["idx_sc"])
                for k in range(4):
                    xi = k % 2
                    if xi == 0:
                        A("act", lambda e, xi=xi: e.copy(out=xrow[xi][:, 0:D], in_=xt[:]), reads=["xt"], writes=[f"xrow{xi}"])
                    else:
                        A("pool", lambda e, xi=xi: e.tensor_copy(out=xrow[xi][:, 0:D], in_=xt[:]), reads=["xt"], writes=[f"xrow{xi}"])
                    A("dve", lambda e, k=k, xi=xi: e.tensor_copy(out=xrow[xi][:, D:D + 1], in_=g4[:, k:k + 1]), reads=["g4"], writes=[f"xrow{xi}"])
                    A("dve", lambda e, xi=xi: e.tensor_copy(out=negm[:], in_=xrow[xi][:, D:D + 1]), reads=[f"xrow{xi}"], writes=["negm"])
                    A("dve", lambda e, k=k, xi=xi: e.tensor_tensor(out=xrow[xi][:, D + 1:D + 2], in0=g4[:, k:k + 1], in1=negm[:], op=ALU.subtract), reads=["g4", "negm"], writes=[f"xrow{xi}"])
                    A("gq", lambda e, k=k, xi=xi: e.indirect_dma_start(out=xsorted, out_offset=bass.IndirectOffsetOnAxis(ap=idx_sc[:, k:k + 1], axis=0), in_=xrow[xi][:], in_offset=None),
                      reads=[f"xrow{xi}", "idx_sc"], writes=["xsorted"], semkey=f"sc{xi}")

            def z_slot_g(g):
                return lambda ct: ((ct - 16) // 8) * 4 + (ct - 16) % 8 - 4 * g

            def rwkv_tile(src, row0, ncol, nrows, full, valid):
                nsub = (ncol + 127) // 128
                for s in range(nsub):
                    r = min(128, nrows - s * 128)
                    load_x(src[row0 + s * 128: row0 + s * 128 + r, :], r)
                    transpose_x(s * 128, min(128, ncol - s * 128))
                zc_l = z_consume(ncol, valid, zl, "zl", lambda ct: ct - 40)
                if full:
                    proj_cols([40, 41], ncol, zc_l)
                    proj_cols([42], ncol, zc_l)
                else:
                    proj_cols([40], ncol, zc_l)
                lora_acts(ncol, full)
                for g in range(2):
                    zc = z_consume(ncol, valid, zg, "zg", z_slot_g(g))
                    kinds = (0, 1, 2) if full else (1, 2)
                    for kind in kinds:
                        base = 16 + 8 * kind + 4 * g
                        proj_cols([base, base + 1], ncol, zc)
                        proj_cols([base + 2, base + 3], ncol, zc)
                    rwkv_group(g, ncol, full, valid)

            def do_conv(ncol, valid):
                load_uhist()
                glu_proj(ncol)
                conv_block(ncol)
                save_uhist(valid)

            n_pre = NTILES
            if os.environ.get("MK_NPRE") is not None:
                n_pre = int(os.environ["MK_NPRE"])
            n_own = NTILES
            if os.environ.get("MK_NOWN") is not None:
                n_own = int(os.environ["MK_NOWN"])
            for ti in range(n_pre):
                rwkv_tile(xp, ti * T, T, T, False, T)
                if ti == n_pre - 1:
                    glu_proj(T)
                    save_uhist(T)
            apply_flag_state()
            for ti in range(n_own):
                rwkv_tile(xo, ti * T, T, T, True, T)
                do_conv(T, T)
                if stage >= 2:
                    for s in range(2):
                        front_sub(xo[ti * T + s * 128: ti * T + (s + 1) * 128, :], 128, s * 128, 128, ti * 2 + s)
            A("sp", lambda e: e.dma_start(out=shift_o, in_=carry[:]), reads=["carry"], semkey="out")
            out_conv(conv_o)
            out_wkv(wkv_o)
            P.flush()
            A("sp", lambda e: e.dma_start(out=carry[:], in_=sshT_d), writes=["carry"], semkey="ld0")
            A("sp", lambda e: e.dma_start(out=cT8[:], in_=sconv_d), writes=["cT8"], semkey="ld0")
            for ci in range(8):
                pn = nxt("pj", 2)
                A("pe", lambda e, ci=ci, pn=pn: e.transpose(out=PS[pn][:, 0:30], in_=cT8[0:30, ci * 128:(ci + 1) * 128], identity=ident[0:30, 0:30]), reads=["cT8", "ident"], writes=[pn])
                A("act", lambda e, ci=ci, pn=pn: e.copy(out=uhist[:, ci, :], in_=PS[pn][:, 0:30]), reads=[pn], writes=["uhist"])
            for p in range(8):
                A("sp", lambda e, p=p: e.dma_start(out=stmp[p % 2][:, 0:64], in_=swkv_d[p]), writes=[f"stmp{p % 2}"], semkey=f"stl{p % 2}")
                def fbd(e, p=p):
                    in0 = fap(stmp[p % 2][:, 0:64], [[0, 2], [1, 64]])
                    in1 = fap(maskh[:], [[1, 2], [0, 64]])
                    return e.tensor_tensor(out=S32[p][:].rearrange("p (h v) -> p h v", h=2), in0=in0, in1=in1, op=ALU.mult)
                A("dve", fbd, reads=[f"stmp{p % 2}", "maskh"], writes=[f"S32_{p}"])
                A("pool", lambda e, p=p: e.tensor_copy(out=Sbf[p][:], in_=S32[p][:]), reads=[f"S32_{p}"], writes=[f"Sbf_{p}"])
            rwkv_tile(xs, 0, CH, 16, True, 16)
            do_conv(CH, 16)
            if stage >= 2:
                front_sub(xs[0:16, :], 16, 0, CH, 32)
            A("sp", lambda e: e.dma_start(out=shift_so, in_=carry[:]), reads=["carry"], semkey="out")
            out_conv(conv_so)
            out_wkv(wkv_so)
            P.flush()

        if stage >= 4:
            NB = CAP // 128
            HALF = CAP // 2
            with ExitStack() as pb:
                bgu = sbt(pb, "bgu", [128, 2, NEXP, 16])
                A("sp", lambda e: e.dma_start(out=bgu[:], in_=bgu_d), writes=["bgu"], semkey="ld0")
                xs_t = [sbt(pb, f"xs_t{i}", [128, XROW], BF16) for i in range(2)]
                xsT = sbt(pb, "xsT", [128, 16, CAP], BF16)
                gate_r = sbt(pb, "gate_r", [128, NB])
                hT = sbt(pb, "hT", [128, 16, CAP], BF16)
                wgu = [sbt(pb, f"wgu{i}", [128, 2, 16, 256], BF16) for i in range(2)]
                wdn = [sbt(pb, f"wdn{i}", [128, 16, 512], BF16) for i in range(2)]
                gcl = [sbt(pb, f"gcl{i}", [128, HALF]) for i in range(2)]
                sgm = [sbt(pb, f"sgm{i}", [128, HALF]) for i in range(2)]
                ucl = [sbt(pb, f"ucl{i}", [128, HALF]) for i in range(2)]
                yo = [sbt(pb, f"yo{i}", [128, 512]) for i in range(4)]
                stg = [sbt(pb, f"stg{i}", [128, 16, 256]) for i in range(2)]
                stg_rot = [0]

                def load_w(dst_ap, dst_key, src_ap, cast_eng):
                    si = stg_rot[0]; stg_rot[0] ^= 1
                    A("sp", lambda e: e.dma_start(out=stg[si][:], in_=src_ap.rearrange("p (k n) -> p k n", k=16)), writes=[f"stg{si}"], semkey=f"stg{si}")
                    if cast_eng == "act":
                        A("act", lambda e: e.copy(out=dst_ap, in_=stg[si][:]), reads=[f"stg{si}"], writes=[dst_key])
                    else:
                        A(cast_eng, lambda e: e.tensor_copy(out=dst_ap, in_=stg[si][:]), reads=[f"stg{si}"], writes=[dst_key])
                n_exp = NEXP
                if os.environ.get("MK_NEXP") is not None:
                    n_exp = int(os.environ["MK_NEXP"])
                for ex in range(n_exp):
                    for blk in range(NB):
                        xi = blk % 2
                        r0 = ex * CAP + blk * 128
                        A("sp", lambda e, xi=xi, r0=r0: e.dma_start(out=xs_t[xi][:], in_=xsorted[r0:r0 + 128, :]), reads=["xsorted"], writes=[f"xs_t{xi}"], semkey=f"xsl{xi}")
                        A("pool", lambda e, xi=xi, blk=blk: e.tensor_tensor(out=gate_r[:, blk:blk + 1], in0=xs_t[xi][:, D:D + 1], in1=xs_t[xi][:, D + 1:D + 2], op=ALU.add), reads=[f"xs_t{xi}"], writes=["gate_r"])
                        for g in range(4):
                            pt = nxt("m", 2)
                            ptb = PS[pt].bitcast(BF16)
                            for j in range(4):
                                kc = 4 * g + j
                                A("pe", lambda e, xi=xi, kc=kc, j=j, ptb=ptb: e.transpose(out=ptb[:, j * 128:(j + 1) * 128], in_=xs_t[xi][:, kc * 128:(kc + 1) * 128], identity=identb[:]),
                                  reads=[f"xs_t{xi}", "identb"], writes=[pt])
                            if g % 2 == 0:
                                A("act", lambda e, g=g, blk=blk, ptb=ptb: e.copy(out=xsT[:, 4 * g:4 * g + 4, blk * 128:(blk + 1) * 128], in_=ptb[:, 0:512].rearrange("p (j t) -> p j t", j=4)), reads=[pt], writes=["xsT"])
                            else:
                                A("dve", lambda e, g=g, blk=blk, ptb=ptb: e.tensor_copy(out=xsT[:, 4 * g:4 * g + 4, blk * 128:(blk + 1) * 128], in_=ptb[:, 0:512].rearrange("p (j t) -> p j t", j=4)), reads=[pt], writes=["xsT"])
                    for fg in range(8):
                        wi = fg % 2
                        load_w(wgu[wi][:, 0, :, :], f"wgu{wi}", w_gate[ex, fg], "act")
                        load_w(wgu[wi][:, 1, :, :], f"wgu{wi}", w_up[ex, fg], "act")
                        for fl in range(2):
                            ft = fg * 2 + fl
                            for hf in range(2):
                                pg_ = nxt("pj", 2)
                                pu_ = nxt("fb", 2)
                                rs = slice(hf * HALF, (hf + 1) * HALF)
                                for kc in range(16):
                                    A("pe", lambda e, kc=kc, pg_=pg_, wi=wi, fl=fl, rs=rs: e.matmul(PS[pg_][:, 0:HALF], lhsT=wgu[wi][:, 0, kc, fl * 128:(fl + 1) * 128], rhs=xsT[:, kc, rs], start=(kc == 0), stop=(kc == 15)),
                                      reads=[f"wgu{wi}", "xsT"], writes=[pg_])
                                for kc in range(16):
                                    A("pe", lambda e, kc=kc, pu_=pu_, wi=wi, fl=fl, rs=rs: e.matmul(PS[pu_][:, 0:HALF], lhsT=wgu[wi][:, 1, kc, fl * 128:(fl + 1) * 128], rhs=xsT[:, kc, rs], start=(kc == 0), stop=(kc == 15)),
                                      reads=[f"wgu{wi}", "xsT"], writes=[pu_])
                                bi = hf
                                A("dve", lambda e, pg_=pg_, bi=bi, ft=ft, ex=ex: e.tensor_scalar(out=gcl[bi][:], in0=PS[pg_][:, 0:HALF], scalar1=bgu[:, 0, ex, ft:ft + 1], scalar2=7.0, op0=ALU.add, op1=ALU.min),
                                  reads=[pg_, "bgu"], writes=[f"gcl{bi}"])
                                A("act", lambda e, bi=bi: e.activation(out=sgm[bi][:], in_=gcl[bi][:], func=AF.Sigmoid, scale=1.702), reads=[f"gcl{bi}"], writes=[f"sgm{bi}"])
                                A("dve", lambda e, pu_=pu_, bi=bi, ft=ft, ex=ex: e.tensor_scalar(out=ucl[bi][:], in0=PS[pu_][:, 0:HALF], scalar1=bgu[:, 1, ex, ft:ft + 1], scalar2=7.0, op0=ALU.add, op1=ALU.min),
                                  reads=[pu_, "bgu"], writes=[f"ucl{bi}"])
                                A("pool", lambda e, bi=bi: e.tensor_scalar(out=ucl[bi][:], in0=ucl[bi][:], scalar1=-7.0, scalar2=1.0, op0=ALU.max, op1=ALU.add), reads=[f"ucl{bi}"], writes=[f"ucl{bi}"])
                                A("pool", lambda e, bi=bi: e.tensor_tensor(out=gcl[bi][:], in0=gcl[bi][:], in1=sgm[bi][:], op=ALU.mult), reads=[f"gcl{bi}", f"sgm{bi}"], writes=[f"gcl{bi}"])
                                A("pool", lambda e, bi=bi, ft=ft, rs=rs: e.tensor_tensor(out=hT[:, ft, rs], in0=gcl[bi][:], in1=ucl[bi][:], op=ALU.mult), reads=[f"gcl{bi}", f"ucl{bi}"], writes=["hT"])
                    for ct in range(4):
                        wi = ct % 2
                        load_w(wdn[wi][:, :, 0:256], f"wdn{wi}", w_down[ex, 2 * ct], "pool")
                        load_w(wdn[wi][:, :, 256:512], f"wdn{wi}", w_down[ex, 2 * ct + 1], "act")
                        for blk in range(NB):
                            pd = nxt("c", 2)
                            for fc in range(16):
                                A("pe", lambda e, fc=fc, pd=pd, blk=blk, wi=wi: e.matmul(PS[pd][:], lhsT=hT[:, fc, blk * 128:(blk + 1) * 128], rhs=wdn[wi][:, fc, :], start=(fc == 0), stop=(fc == 15)),
                                  reads=["hT", f"wdn{wi}"], writes=[pd])
                            yi = int(nxt("yo", 4)[2:])
                            r0 = ex * CAP + blk * 128
                            if yi % 2 == 0:
                                A("act", lambda e, pd=pd, blk=blk, yi=yi: e.activation(out=yo[yi][:], in_=PS[pd][:], func=AF.Copy, scale=gate_r[:, blk:blk + 1]), reads=[pd, "gate_r"], writes=[f"yo{yi}"])
                            else:
                                A("dve", lambda e, pd=pd, blk=blk, yi=yi: e.tensor_scalar(out=yo[yi][:], in0=PS[pd][:], scalar1=gate_r[:, blk:blk + 1], scalar2=None, op0=ALU.mult), reads=[pd, "gate_r"], writes=[f"yo{yi}"])
                            A("sp", lambda e, yi=yi, r0=r0, ct=ct: e.dma_start(out=ysorted[r0:r0 + 128, ct * 512:(ct + 1) * 512], in_=yo[yi][:]), reads=[f"yo{yi}"], writes=["ysorted"], semkey=f"yst{yi}")
                P.flush()
            with ExitStack() as pc_:
                bdn = sbt(pc_, "bdn", [NEXP, D])
                A("sp", lambda e: e.dma_start(out=bdn[:], in_=bdn_d), writes=["bdn"], semkey="ld0")
                lnv2 = sbt(pc_, "lnv2", [128, 2, D])
                A("sp", lambda e: e.dma_start(out=lnv2[:, 0, :], in_=lnv_d[2:3, :].partition_broadcast(128)), writes=["lnv2"], semkey="ld0")
                A("sp", lambda e: e.dma_start(out=lnv2[:, 1, :], in_=lnv_d[3:4, :].partition_broadcast(128)), writes=["lnv2"], semkey="ld0")
                Gk = [sbt(pc_, f"Gk{i}", [128, D]) for i in range(8)]
                xr = [sbt(pc_, f"xr{i}", [128, D]) for i in range(2)]
                gTt = [sbt(pc_, f"gTt{i}", [NEXP, 128]) for i in range(2)]
                bst2 = sbt(pc_, "bst2", [128, 4, 6]); mv2 = sbt(pc_, "mv2", [128, 2]); rstd2 = sbt(pc_, "rstd2", [128, 1])
                for sidx in done_subs:
                    bi = sidx % 2
                    X, Xk = xr[bi], f"xr{bi}"
                    A("sp", lambda e, X=X, sidx=sidx: e.dma_start(out=X[:], in_=x1s[sidx * 128:(sidx + 1) * 128, :]), reads=["x1s"], writes=[Xk], semkey=f"xr{bi}")
                    for k in range(4):
                        gi = bi * 4 + k
                        A("gq", lambda e, gi=gi, k=k, sidx=sidx: e.indirect_dma_start(out=Gk[gi][:], out_offset=None, in_=ysorted,
                                                                                      in_offset=bass.IndirectOffsetOnAxis(ap=idx_all[:, sidx, k:k + 1], axis=0)),
                          reads=["ysorted", "idx_all"], writes=[f"Gk{gi}"], semkey=f"gk{gi}")
                    pg = nxt("pj", 2)
                    A("pe", lambda e, pg=pg, sidx=sidx: e.transpose(out=PS[pg][0:NEXP, 0:128], in_=gd_all[:, sidx, :], identity=ident[:]), reads=["gd_all", "ident"], writes=[pg])
                    A("act", lambda e, pg=pg, bi=bi: e.copy(out=gTt[bi][:], in_=PS[pg][0:NEXP, 0:128]), reads=[pg], writes=[f"gTt{bi}"])
                    A("dve", lambda e, X=X, bi=bi: e.scalar_tensor_tensor(out=X[:], in0=X[:], scalar=ALPHA, in1=Gk[bi * 4][:], op0=ALU.mult, op1=ALU.add), reads=[Xk, f"Gk{bi * 4}"], writes=[Xk])
                    A("pool", lambda e, bi=bi: e.tensor_tensor(out=Gk[bi * 4 + 1][:], in0=Gk[bi * 4 + 1][:], in1=Gk[bi * 4 + 2][:], op=ALU.add), reads=[f"Gk{bi * 4 + 1}", f"Gk{bi * 4 + 2}"], writes=[f"Gk{bi * 4 + 1}"])
                    A("pool", lambda e, bi=bi: e.tensor_tensor(out=Gk[bi * 4 + 1][:], in0=Gk[bi * 4 + 1][:], in1=Gk[bi * 4 + 3][:], op=ALU.add), reads=[f"Gk{bi * 4 + 1}", f"Gk{bi * 4 + 3}"], writes=[f"Gk{bi * 4 + 1}"])
                    A("dve", lambda e, X=X, bi=bi: e.tensor_tensor(out=X[:], in0=X[:], in1=Gk[bi * 4 + 1][:], op=ALU.add), reads=[Xk, f"Gk{bi * 4 + 1}"], writes=[Xk])
                    for ct in range(4):
                        pb_ = nxt("fb", 2)
                        A("pe", lambda e, pb_=pb_, ct=ct, bi=bi: e.matmul(PS[pb_][:], lhsT=gTt[bi][:], rhs=bdn[:, ct * 512:(ct + 1) * 512], start=True, stop=True), reads=[f"gTt{bi}", "bdn"], writes=[pb_])
                        A("dve", lambda e, pb_=pb_, ct=ct, X=X: e.tensor_tensor(out=X[:, ct * 512:(ct + 1) * 512], in0=X[:, ct * 512:(ct + 1) * 512], in1=PS[pb_][:], op=ALU.add), reads=[pb_, Xk], writes=[Xk])
                    for q in range(4):
                        A("dve", lambda e, q=q, X=X: e.bn_stats(out=bst2[:, q, :], in_=X[:, q * 512:(q + 1) * 512]), reads=[Xk], writes=["bst2"])
                    A("dve", lambda e: e.bn_aggr(out=mv2[:], in_=bst2[:].rearrange("p a b -> p (a b)")), reads=["bst2"], writes=["mv2"])
                    rsqrt(rstd2[:], mv2[:, 1:2], 0, ["mv2"], ["rstd2"])
                    A("dve", lambda e, X=X: e.tensor_scalar(out=X[:], in0=X[:], scalar1=mv2[:, 0:1], scalar2=rstd2[:, 0:1], op0=ALU.subtract, op1=ALU.mult), reads=[Xk, "mv2", "rstd2"], writes=[Xk])
                    A("pool", lambda e, X=X: e.tensor_tensor(out=X[:], in0=X[:], in1=lnv2[:, 0, :], op=ALU.mult), reads=[Xk, "lnv2"], writes=[Xk])
                    A("pool", lambda e, X=X: e.tensor_tensor(out=X[:], in0=X[:], in1=lnv2[:, 1, :], op=ALU.add), reads=[Xk, "lnv2"], writes=[Xk])
                    A("sp", lambda e, X=X, sidx=sidx: e.dma_start(out=y_own[sidx * 128:(sidx + 1) * 128, :], in_=X[:]), reads=[Xk], semkey="out")
                P.flush()
        P.finish()
    return nc


def _consts():
    p = np.arange(128)
    h, t = p // 64, p % 64
    same = (h[:, None] == h[None, :])
    c = {}
    c["ident"] = np.eye(128, dtype=np.float32)
    c["maskh"] = (h[:, None] == np.arange(2)[None, :]).astype(np.float32)
    nt = same & (t[:, None] > t[None, :])
    n_ = same & (t[:, None] < t[None, :])
    c["m1"] = np.concatenate([nt, n_], axis=1).astype(np.float32)
    incl = (t[:, None] <= np.arange(64)[None, :])
    c["m2"] = np.concatenate([incl, n_, incl], axis=1).astype(np.float32)
    c["onesbd"] = same.astype(np.float32)
    c["tri"] = (p[:, None] < p[None, :]).astype(np.float32)
    c["rvt"] = np.stack([(p < 16).astype(np.float32), (NROWS + p).astype(np.float32)], axis=1)
    c["ecap"] = np.broadcast_to((np.arange(NEXP) * CAP).astype(np.float32)[None, :], (128, NEXP)).copy()
    return c


def _colmajor(v, ntile):
    out = np.zeros((ntile * 128,), np.float32)
    out[:v.shape[0]] = v
    return np.ascontiguousarray(out.reshape(ntile, 128).T)


def _shared_inputs(inp, stage):
    g = lambda k: np.asarray(inp[k], dtype=np.float32)[0]
    sh = dict(_consts())
    wi_ = np.zeros((D, 43 * 128), np.float32)
    wi_[:, :P_IN] = g("w_in")
    sh["w_in"] = np.ascontiguousarray(wi_.reshape(16, 128, 43, 128).transpose(2, 1, 0, 3)).reshape(43, 128, D)
    sh["binT"] = _colmajor(g("b_in"), 43)
    sh["muT"] = _colmajor(g("mu_shift"), 27)
    sh["cwT"] = np.ascontiguousarray(g("conv_w").reshape(31, 8, 128).transpose(2, 1, 0))
    sh["cvec"] = np.ascontiguousarray(np.stack([_colmajor(g("conv_b"), 8), _colmajor(g("conv_ln_g"), 8), _colmajor(g("conv_ln_b"), 8)], axis=1))
    pv = [g("rwkv_w0"), g("rwkv_a0"), g("rwkv_k_k"), g("rwkv_k_a"), g("rwkv_r_k").reshape(-1), g("rwkv_ln_g"), g("rwkv_ln_b")]
    sh["pvec"] = np.ascontiguousarray(np.stack([_colmajor(v, 8) for v in pv], axis=1))
    sh["w2"] = g("rwkv_w2"); sh["a2"] = g("rwkv_a2"); sh["g2"] = g("rwkv_g2")
    sh["w_out"] = np.ascontiguousarray(g("w_out").reshape(16, 128, 16, 128).transpose(2, 1, 0, 3)).reshape(16, 128, D)
    sh["lnv"] = np.ascontiguousarray(np.stack([g("ln1_g"), g("ln1_b"), g("ln2_g"), g("ln2_b")], axis=0))
    sh["rw"] = g("router_w"); sh["rb"] = g("router_b").reshape(1, NEXP)
    if stage >= 4:
        lay = lambda w: np.ascontiguousarray(w.reshape(NEXP, 16, 128, 8, 256).transpose(0, 3, 2, 1, 4)).reshape(NEXP, 8, 128, 4096)
        sh["w_gate"] = lay(g("w_gate")); sh["w_up"] = lay(g("w_up")); sh["w_down"] = lay(g("w_down"))
        bg = g("b_gate").reshape(NEXP, 16, 128).transpose(2, 0, 1)
        bu = g("b_up").reshape(NEXP, 16, 128).transpose(2, 0, 1)
        sh["bgu"] = np.ascontiguousarray(np.stack([bg, bu], axis=1))
        sh["bdn"] = g("b_down")
    return sh


def _core_inputs(c, inp, sh):
    b, half = c // 2, c % 2
    xpr = np.asarray(inp["x_prompt"], dtype=np.float32)
    m = dict(sh)
    m["xo"] = np.ascontiguousarray(xpr[b, half * NOWN:(half + 1) * NOWN])
    m["xp"] = np.ascontiguousarray(xpr[b, 0:NOWN])
    m["xs"] = np.ascontiguousarray(np.asarray(inp["x_sample"], dtype=np.float32)[c])
    m["flag"] = np.full((128, 1), float(half), np.float32)
    m["sconv"] = np.ascontiguousarray(np.asarray(inp["state_conv"], dtype=np.float32)[0, c])
    m["sshT"] = _colmajor(np.asarray(inp["state_shift"], dtype=np.float32)[0, c, 0], 27)
    sw = np.asarray(inp["state_wkv"], dtype=np.float32)[0, c]
    m["swkv"] = np.ascontiguousarray(sw.reshape(8, 2, 64, 64).transpose(0, 1, 3, 2).reshape(8, 128, 64))
    return m


_NC_CACHE = {}


def run_cores(inp, stage=99):
    if stage not in _NC_CACHE:
        _NC_CACHE[stage] = build_nc(stage)
    nc = _NC_CACHE[stage]
    sh = _shared_inputs(inp, stage)
    in_maps = [_core_inputs(c, inp, sh) for c in range(8)]
    res = run_bass_kernel_spmd(nc, in_maps, core_ids=list(range(8)))
    return res.results


def assemble(rs):
    y_p = np.zeros((4, 8192, D), np.float32); y_s = np.zeros((8, 16, D), np.float32)
    conv_p = np.zeros((1, 4, 30, C_CONV), np.float32); shift_p = np.zeros((1, 4, 1, NSH), np.float32); wkv_p = np.zeros((1, 4, 16, 64, 64), np.float32)
    conv_s = np.zeros((1, 8, 30, C_CONV), np.float32); shift_s = np.zeros((1, 8, 1, NSH), np.float32); wkv_s = np.zeros((1, 8, 16, 64, 64), np.float32)
    unshift = lambda a: np.ascontiguousarray(a.T).reshape(-1)[:NSH]
    unwkv = lambda a: a.reshape(8, 2, 64, 64).transpose(0, 1, 3, 2).reshape(16, 64, 64)
    for c in range(8):
        r = rs[c]
        b, half = c // 2, c % 2
        y_p[b, half * NOWN:(half + 1) * NOWN] = r["y_own"][0:NOWN]
        y_s[c] = r["y_own"][NOWN:NOWN + 16]
        if half == 1:
            conv_p[0, b] = r["conv_o"]; shift_p[0, b, 0] = unshift(r["shift_o"]); wkv_p[0, b] = unwkv(r["wkv_o"])
        conv_s[0, c] = r["conv_so"]; shift_s[0, c, 0] = unshift(r["shift_so"]); wkv_s[0, c] = unwkv(r["wkv_so"])
    return (y_p, y_s, conv_p, shift_p, wkv_p, conv_s, shift_s, wkv_s)


def kernel(**inputs):
    return assemble(run_cores(inputs, 99))
```
